# Optimizing a Trainium2 kernel written in Bass

```python
import math
import jax, jax.numpy as jnp
from jax import lax
import numpy as np

D_MODEL = 1024
BATCH = 8
SEQ = 4096
DEPTH = 2

GRID_W = 64
CTX_LEN = 256
N_MIXERS = 2
N_LRU_LAYERS = (DEPTH + N_MIXERS - 1) // N_MIXERS
N_ATTN_LAYERS = DEPTH // N_MIXERS
D_RNN = D_MODEL
N_LRU_BLOCKS = 8
LRU_BLOCK = D_RNN // N_LRU_BLOCKS
CONV_W = 4
LRU_C = 8.0
HEAD_DIM = 64
N_HEADS = D_MODEL // HEAD_DIM
N_KV_HEADS = 4
KV_GROUP = N_HEADS // N_KV_HEADS
ROPE_THETA = 10000.0
Q_BLOCK = 128
N_EXPERTS = 16
N_EXPERT_GROUPS = 4
EXPERTS_PER_GROUP = N_EXPERTS // N_EXPERT_GROUPS
TOP_K = 2
EXPERT_FF = 512
N_MOD = 6
EPS = 1e-6

kernel_name = "hybrid_rglru_gqa_grouped_moe_dit"


def rms_norm(x, w):
    xf = x.astype(jnp.float32)
    y = xf * lax.rsqrt(jnp.mean(xf * xf, axis=-1, keepdims=True) + EPS)
    return (y * w.astype(jnp.float32)).astype(x.dtype)


def adaln(cond, w, b):
    m = jax.nn.silu(cond) @ w + b
    m = m.reshape((-1, 1, N_MOD * D_MODEL))
    return jnp.split(m, N_MOD, axis=-1)


def modulate(x, g, shift, scale):
    return rms_norm(x, g) * (1 + scale) + shift


def dwconv_centred(u, w, b):
    left = CONV_W // 2
    right = CONV_W - 1 - left
    L = u.shape[1]
    up = jnp.pad(u, ((0, 0), (left, right), (0, 0)))
    out = b
    for k in range(CONV_W):
        out = out + up[:, k:k + L] * w[k]
    return out


def rglru_coeffs(u, wa, ba, wx, bx, lam):
    B_, L, C = u.shape
    ub = u.reshape(B_, L, N_LRU_BLOCKS, LRU_BLOCK)
    r = jax.nn.sigmoid((jnp.einsum("blnd,nde->blne", ub, wa).reshape(B_, L, C) + ba).astype(jnp.float32))
    ig = jax.nn.sigmoid((jnp.einsum("blnd,nde->blne", ub, wx).reshape(B_, L, C) + bx).astype(jnp.float32))
    log_a = -LRU_C * r * jax.nn.softplus(-lam.astype(jnp.float32))
    a = jnp.exp(log_a)
    bterm = jnp.sqrt(-jnp.expm1(2.0 * log_a)) * (ig * u.astype(jnp.float32))
    return a, bterm


def linear_scan(a, b, h0, reverse):
    if h0 is not None:
        if reverse:
            b = b.at[:, -1].add(a[:, -1] * h0)
        else:
            b = b.at[:, 0].add(a[:, 0] * h0)

    def combine(e1, e2):
        a1, b1 = e1
        a2, b2 = e2
        return a1 * a2, a2 * b1 + b2

    _, h = lax.associative_scan(combine, (a, b), reverse=reverse, axis=1)
    return h


def rglru_mixer(hx, hc, in_w, conv_w, conv_b, ga_w, ga_b, gx_w, gx_b, lam, out_w, ctx_out):
    w_gate, w_in = in_w[:, :D_RNN], in_w[:, D_RNN:]
    gx_, ux = jnp.split(hx @ in_w, 2, axis=-1)
    uc = hc @ w_in
    ux = dwconv_centred(ux, conv_w, conv_b)
    uc = dwconv_centred(uc, conv_w, conv_b)
    yx = 0.0
    yc = 0.0
    for d in range(2):
        rev = d == 1
        a_c, b_c = rglru_coeffs(uc, ga_w[d], ga_b[d], gx_w[d], gx_b[d], lam[d])
        h_c = linear_scan(a_c, b_c, None, rev)
        h0 = h_c[:, 0] if rev else h_c[:, -1]
        a_x, b_x = rglru_coeffs(ux, ga_w[d], ga_b[d], gx_w[d], gx_b[d], lam[d])
        h_x = linear_scan(a_x, b_x, h0, rev)
        yx = yx + h_x
        if ctx_out:
            yc = yc + h_c
    out_x = (jax.nn.gelu(gx_) * yx.astype(hx.dtype)) @ out_w
    out_c = None
    if ctx_out:
        out_c = (jax.nn.gelu(hc @ w_gate) * yc.astype(hc.dtype)) @ out_w
    return out_x, out_c


def rope_1d(t, pos):
    half = t.shape[-1] // 2
    inv = ROPE_THETA ** (-jnp.arange(half, dtype=jnp.float32) / half)
    ang = pos.astype(jnp.float32)[:, None] * inv
    cos = jnp.cos(ang)[None, :, None, :]
    sin = jnp.sin(ang)[None, :, None, :]
    tf = t.astype(jnp.float32)
    t1, t2 = tf[..., :half], tf[..., half:]
    return jnp.concatenate([t1 * cos - t2 * sin, t2 * cos + t1 * sin], axis=-1).astype(t.dtype)


def rope_2d(t):
    L = t.shape[1]
    rows = L // GRID_W
    pos_r = jnp.repeat(jnp.arange(rows, dtype=jnp.int32), GRID_W)
    pos_c = jnp.tile(jnp.arange(GRID_W, dtype=jnp.int32), rows)
    half = HEAD_DIM // 2
    return jnp.concatenate([rope_1d(t[..., :half], pos_r), rope_1d(t[..., half:], pos_c)], axis=-1)


def attention_mixer(hx, hc, qkv_w, qn_w, kn_w, o_w, ctx_out):
    B_, L, _ = hx.shape
    C = hc.shape[1]
    q_cols = N_HEADS * HEAD_DIM
    kv_cols = N_KV_HEADS * HEAD_DIM
    scale = 1.0 / math.sqrt(HEAD_DIM)

    qx, kx, vx = jnp.split(hx @ qkv_w, [q_cols, q_cols + kv_cols], axis=-1)
    qx = rope_2d(rms_norm(qx.reshape(B_, L, N_HEADS, HEAD_DIM), qn_w))
    kx = rope_2d(rms_norm(kx.reshape(B_, L, N_KV_HEADS, HEAD_DIM), kn_w))
    vx = vx.reshape(B_, L, N_KV_HEADS, HEAD_DIM)

    if ctx_out:
        qc, kc, vc = jnp.split(hc @ qkv_w, [q_cols, q_cols + kv_cols], axis=-1)
    else:
        kc, vc = jnp.split(hc @ qkv_w[:, q_cols:], [kv_cols], axis=-1)
    kc = rms_norm(kc.reshape(B_, C, N_KV_HEADS, HEAD_DIM), kn_w)
    vc = vc.reshape(B_, C, N_KV_HEADS, HEAD_DIM)

    def attend(q, k, v):
        s = jnp.einsum("bqkgd,bskd->bkgqs", q, k).astype(jnp.float32) * scale
        p = jax.nn.softmax(s, axis=-1).astype(v.dtype)
        return jnp.einsum("bkgqs,bskd->bqkgd", p, v)

    k_all = jnp.concatenate([kc, kx], axis=1)
    v_all = jnp.concatenate([vc, vx], axis=1)
    n_blk = L // Q_BLOCK
    qb = qx.reshape(B_, n_blk, Q_BLOCK, N_KV_HEADS, KV_GROUP, HEAD_DIM).swapaxes(0, 1)
    ob = lax.map(lambda q: attend(q, k_all, v_all), qb)
    out_x = ob.swapaxes(0, 1).reshape(B_, L, q_cols) @ o_w

    out_c = None
    if ctx_out:
        qc = rms_norm(qc.reshape(B_, C, N_HEADS, HEAD_DIM), qn_w).reshape(B_, C, N_KV_HEADS, KV_GROUP, HEAD_DIM)
        out_c = attend(qc, kc, vc).reshape(B_, C, q_cols) @ o_w
    return out_x, out_c


def grouped_moe(h, router_w, router_b, w1, w3, w2):
    logits = (h @ router_w).astype(jnp.float32) + router_b.astype(jnp.float32)
    probs = jax.nn.softmax(logits, axis=-1)
    pg = probs.reshape(probs.shape[:-1] + (N_EXPERT_GROUPS, EXPERTS_PER_GROUP))
    group_score = lax.top_k(pg, TOP_K)[0].sum(-1)
    sel = jnp.argmax(group_score, axis=-1)
    in_group = (sel[..., None] == jnp.arange(N_EXPERT_GROUPS))[..., None]
    masked = jnp.where(in_group, pg, -1.0).reshape(probs.shape)
    top_w, top_i = lax.top_k(masked, TOP_K)
    top_w = top_w / jnp.sum(top_w, axis=-1, keepdims=True)
    gates = jnp.sum(jax.nn.one_hot(top_i, N_EXPERTS, dtype=jnp.float32) * top_w[..., None], axis=-2).astype(h.dtype)
    y = jnp.zeros_like(h)
    for e in range(N_EXPERTS):
        y = y + gates[..., e:e + 1] * ((jax.nn.silu(h @ w1[e]) * (h @ w3[e])) @ w2[e])
    return y


def setup_inputs(seed: int = 0) -> dict:
    key = jax.random.key(seed)
    ks = jax.random.split(key, 28)
    f32 = jnp.float32

    def nrm(k, shape, fan_in):
        return jax.random.normal(k, shape, f32) * fan_in ** -0.5

    def gain(k, shape):
        return 1.0 + 0.05 * jax.random.normal(k, shape, f32)

    def small(k, shape, s=0.02):
        return s * jax.random.normal(k, shape, f32)

    p_a = jax.random.uniform(ks[14], (N_LRU_LAYERS, 2, D_RNN), f32, minval=0.9, maxval=0.999) ** (1.0 / LRU_C)
    lam = jnp.log(p_a) - jnp.log1p(-p_a)
    return {
        "x": jax.random.normal(ks[0], (BATCH, SEQ, D_MODEL), f32),
        "c": jax.random.normal(ks[1], (BATCH, D_MODEL), f32),
        "ctx": jax.random.normal(ks[2], (BATCH, CTX_LEN, D_MODEL), f32),
        "c_ctx": jax.random.normal(ks[3], (D_MODEL,), f32),
        "ada_w": nrm(ks[4], (DEPTH, D_MODEL, N_MOD * D_MODEL), D_MODEL),
        "ada_b": small(ks[5], (DEPTH, N_MOD * D_MODEL)),
        "norm_mix_w": gain(ks[6], (DEPTH, D_MODEL)),
        "norm_ffn_w": gain(ks[7], (DEPTH, D_MODEL)),
        "lru_in_w": nrm(ks[8], (N_LRU_LAYERS, D_MODEL, 2 * D_RNN), D_MODEL),
        "lru_conv_w": nrm(ks[9], (N_LRU_LAYERS, CONV_W, D_RNN), CONV_W),
        "lru_conv_b": small(ks[10], (N_LRU_LAYERS, D_RNN)),
        "lru_gate_a_w": nrm(ks[11], (N_LRU_LAYERS, 2, N_LRU_BLOCKS, LRU_BLOCK, LRU_BLOCK), LRU_BLOCK),
        "lru_gate_a_b": small(ks[12], (N_LRU_LAYERS, 2, D_RNN)),
        "lru_gate_x_w": nrm(ks[13], (N_LRU_LAYERS, 2, N_LRU_BLOCKS, LRU_BLOCK, LRU_BLOCK), LRU_BLOCK),
        "lru_gate_x_b": small(ks[15], (N_LRU_LAYERS, 2, D_RNN)),
        "lru_lambda": lam,
        "lru_out_w": nrm(ks[16], (N_LRU_LAYERS, D_RNN, D_MODEL), D_RNN),
        "attn_qkv_w": nrm(ks[17], (N_ATTN_LAYERS, D_MODEL, (N_HEADS + 2 * N_KV_HEADS) * HEAD_DIM), D_MODEL),
        "attn_q_norm_w": gain(ks[18], (N_ATTN_LAYERS, HEAD_DIM)),
        "attn_k_norm_w": gain(ks[19], (N_ATTN_LAYERS, HEAD_DIM)),
        "attn_o_w": nrm(ks[20], (N_ATTN_LAYERS, N_HEADS * HEAD_DIM, D_MODEL), N_HEADS * HEAD_DIM),
        "router_w": nrm(ks[21], (D_MODEL, N_EXPERTS), D_MODEL),
        "router_b": small(ks[22], (N_EXPERTS,), 0.01),
        "moe_w1": nrm(ks[23], (DEPTH, N_EXPERTS, D_MODEL, EXPERT_FF), D_MODEL),
        "moe_w3": nrm(ks[24], (DEPTH, N_EXPERTS, D_MODEL, EXPERT_FF), D_MODEL),
        "moe_w2": nrm(ks[25], (DEPTH, N_EXPERTS, EXPERT_FF, D_MODEL), EXPERT_FF),
    }


def reference(x, c, ctx, c_ctx, ada_w, ada_b, norm_mix_w, norm_ffn_w,
              lru_in_w, lru_conv_w, lru_conv_b, lru_gate_a_w, lru_gate_a_b, lru_gate_x_w, lru_gate_x_b,
              lru_lambda, lru_out_w, attn_qkv_w, attn_q_norm_w, attn_k_norm_w, attn_o_w,
              router_w, router_b, moe_w1, moe_w3, moe_w2):
    C = ctx.shape[1]
    for i in range(DEPTH):
        ctx_out = i < DEPTH - 1
        j = i // N_MIXERS
        sh_m, sc_m, g_m, sh_f, sc_f, g_f = adaln(c, ada_w[i], ada_b[i])
        csh_m, csc_m, cg_m, csh_f, csc_f, cg_f = adaln(c_ctx, ada_w[i], ada_b[i])

        hx = modulate(x, norm_mix_w[i], sh_m, sc_m)
        hc = modulate(ctx, norm_mix_w[i], csh_m, csc_m)
        if i % N_MIXERS == 0:
            dx, dc = rglru_mixer(hx, hc, lru_in_w[j], lru_conv_w[j], lru_conv_b[j],
                                 lru_gate_a_w[j], lru_gate_a_b[j], lru_gate_x_w[j], lru_gate_x_b[j],
                                 lru_lambda[j], lru_out_w[j], ctx_out)
        else:
            dx, dc = attention_mixer(hx, hc, attn_qkv_w[j], attn_q_norm_w[j], attn_k_norm_w[j],
                                     attn_o_w[j], ctx_out)
        x = x + g_m * dx
        if ctx_out:
            ctx = ctx + cg_m * dc

        hx = modulate(x, norm_ffn_w[i], sh_f, sc_f)
        if ctx_out:
            hc = modulate(ctx, norm_ffn_w[i], csh_f, csc_f)
            y = grouped_moe(jnp.concatenate([hc, hx], axis=1), router_w, router_b,
                            moe_w1[i], moe_w3[i], moe_w2[i])
            ctx = ctx + cg_f * y[:, :C]
            x = x + g_f * y[:, C:]
        else:
            x = x + g_f * grouped_moe(hx, router_w, router_b, moe_w1[i], moe_w3[i], moe_w2[i])
    return x
```

```python
import numpy as np
from contextlib import ExitStack
import concourse.bass as bass
import concourse.mybir as mybir
from concourse.bass_utils import run_bass_kernel_spmd

F32 = mybir.dt.float32
BF16 = mybir.dt.bfloat16
U8 = mybir.dt.uint8
AF = mybir.ActivationFunctionType
ALU = mybir.AluOpType
AX = mybir.AxisListType

D = 1024
T = 4096
C = 256
TS = T + C
NXT = T // 128
NCT = C // 128
NE = 16
FF = 512
EPS = 1e-6
ENGS = ("sync", "gpsimd", "scalar", "vector", "tensor")
NRING = {"sync": 28, "gpsimd": 24, "scalar": 8}


class Buf:
    __slots__ = ("name", "w", "r", "excl")

    def __init__(self, name="", excl=False):
        self.name = name
        self.w = None
        self.r = {}
        self.excl = excl


class Op:
    __slots__ = ("eng", "fn", "deps", "needed", "sig", "dma", "ring", "val", "prev")


class V:
    __slots__ = ("ap", "bufs")

    def __init__(self, ap, bufs):
        self.ap = ap
        self.bufs = tuple(bufs)

    def __getitem__(self, k):
        return V(self.ap[k], self.bufs)

    def rearrange(self, s, **kw):
        return V(self.ap.rearrange(s, **kw), self.bufs)

    def bitcast(self, dt):
        return V(self.ap.bitcast(dt), self.bufs)

    def with_bufs(self, *bufs):
        return V(self.ap, bufs)

    @property
    def shape(self):
        return self.ap.shape


def _ap(x):
    return x.ap if isinstance(x, V) else x


class Prog:
    def __init__(self, nc):
        self.nc = nc
        self.ops = {e: [] for e in ENGS}
        self.last_compute = {e: None for e in ENGS}
        self.dma_since = []
        self.pending = {e: None for e in ENGS}
        self.ring_pos = {e: 0 for e in ENGS}
        self.ring_last = {}
        self.ring_cnt = {}

    def add(self, eng, fn, reads=(), writes=(), dma=False):
        op = Op()
        op.eng = eng
        op.fn = fn
        op.dma = dma
        op.needed = False
        op.sig = 0
        op.deps = {}
        op.prev = None
        for b in reads:
            if b.w is not None:
                op.deps[b.w] = "raw"
            if b.excl:
                for key, o in b.r.items():
                    if key != eng:
                        op.deps.setdefault(o, "raw")
        for b in writes:
            if b.w is not None:
                op.deps.setdefault(b.w, "waw")
            for o in b.r.values():
                if o is not op:
                    op.deps.setdefault(o, "war")
        pend = self.pending[eng]
        if pend:
            for o in pend:
                op.deps[o] = "raw"
            self.pending[eng] = None
        for b in writes:
            b.w = op
            b.r = {}
        for b in reads:
            b.r[("dma", id(op)) if dma else eng] = op
        if dma:
            n = NRING[eng]
            key = (eng, self.ring_pos[eng] % n)
            self.ring_pos[eng] += 1
            op.ring = key
            self.ring_cnt[key] = self.ring_cnt.get(key, 0) + 1
            op.val = 16 * self.ring_cnt[key]
            op.prev = self.ring_last.get(key)
            self.ring_last[key] = op
            self.dma_since.append(op)
        else:
            self.last_compute[eng] = op
        self.ops[eng].append(op)
        return op

    def barrier(self):
        deps = [o for o in self.last_compute.values() if o is not None]
        latest = {}
        for o in self.dma_since:
            latest[o.ring] = o
        deps += list(latest.values())
        self.dma_since = list(latest.values())
        for e in ENGS:
            cur = self.pending[e] or []
            self.pending[e] = list(cur) + deps

    @staticmethod
    def _keep(op, d, kind):
        if d.dma:
            return True
        if d.eng != op.eng:
            return True
        if op.dma:
            return True
        if op.eng == "tensor":
            return False
        return True

    def emit(self, stack):
        nc = self.nc
        for e in ENGS:
            for op in self.ops[e]:
                for d, kind in op.deps.items():
                    if (not d.dma) and self._keep(op, d, kind):
                        d.needed = True
        for e in ENGS:
            cnt = 0
            for op in self.ops[e]:
                if (not op.dma) and op.needed:
                    cnt += 1
                    op.sig = cnt
        esem = {e: stack.enter_context(nc.semaphore("es_" + e)) for e in ENGS if e != "sync"}
        rsem = {}
        for e, n in NRING.items():
            for i in range(n):
                rsem[(e, i)] = stack.enter_context(nc.semaphore(f"rs_{e}_{i}"))
        block = stack.enter_context(nc.Block())
        prog = self

        def run_engine(e, h):
            waited = {}

            def wait(sem_key, sem, val):
                if waited.get(sem_key, 0) < val:
                    h.wait_ge(sem, val)
                    waited[sem_key] = val

            for op in prog.ops[e]:
                for d, kind in op.deps.items():
                    if not prog._keep(op, d, kind):
                        continue
                    if d.dma:
                        wait(d.ring, rsem[d.ring], d.val)
                    else:
                        wait(d.eng, esem[d.eng], d.sig)
                if op.dma and op.prev is not None:
                    wait(op.ring, rsem[op.ring], op.prev.val)
                ins = op.fn(h)
                if op.dma:
                    ins.then_inc(rsem[op.ring], 16)
                elif op.needed:
                    ins.then_inc(esem[e], 1)

        @block.sync
        def _(h):
            run_engine("sync", h)

        @block.gpsimd
        def _(h):
            run_engine("gpsimd", h)

        @block.scalar
        def _(h):
            run_engine("scalar", h)

        @block.vector
        def _(h):
            run_engine("vector", h)

        @block.tensor
        def _(h):
            run_engine("tensor", h)


def _bufs(*xs):
    out = []
    for x in xs:
        if isinstance(x, V):
            out.extend(x.bufs)
    return out


class K:
    def __init__(self, nc, P, arena, arena_bytes, banks):
        self.nc = nc
        self.P = P
        self.arena = arena
        self.arena_bytes = arena_bytes
        self.off = 0
        self.banks = banks
        self.uid = 0

    def reset(self, keep=0):
        self.P.barrier()
        self.off = keep

    def alloc(self, name, shape, dtype, npart=128):
        esz = {F32: 4, BF16: 2, U8: 1, mybir.dt.uint32: 4, mybir.dt.int32: 4}[dtype]
        n = 1
        for s in shape[1:]:
            n *= s
        nbytes = (n * esz + 63) // 64 * 64
        assert self.off + nbytes <= self.arena_bytes, (name, self.off, nbytes, self.arena_bytes)
        ap = self.arena[0:shape[0], self.off:self.off + n * esz].bitcast(dtype)
        self.off += nbytes
        if len(shape) == 3:
            ap = ap.rearrange("p (a b) -> p a b", a=shape[1])
        elif len(shape) == 4:
            ap = ap.rearrange("p (a b c) -> p a b c", a=shape[1], b=shape[2])
        self.uid += 1
        return V(ap, (Buf(f"{name}_{self.uid}"),))

    def dma(self, q, out, in_, **kw):
        o, i = _ap(out), _ap(in_)
        return self.P.add(q, lambda h: h.dma_start(out=o, in_=i, **kw),
                          reads=_bufs(in_), writes=_bufs(out), dma=True)

    def mm(self, out, lhsT, rhs, start, stop, extra_reads=()):
        o, l, r = _ap(out), _ap(lhsT), _ap(rhs)
        return self.P.add("tensor", lambda h: h.matmul(o, l, r, start=start, stop=stop),
                          reads=_bufs(lhsT, rhs) + list(extra_reads), writes=_bufs(out))

    def transpose(self, out, in_, ident):
        o, i, d = _ap(out), _ap(in_), _ap(ident)
        return self.P.add("tensor", lambda h: h.transpose(o, i, d),
                          reads=_bufs(in_, ident), writes=_bufs(out))

    def act(self, out, in_, func, bias=None, scale=1.0, accum_out=None, eng="scalar"):
        o, i = _ap(out), _ap(in_)
        kw = {}
        if bias is not None:
            kw["bias"] = _ap(bias)
        if accum_out is not None:
            kw["accum_out"] = _ap(accum_out)
        sc = _ap(scale)
        return self.P.add(eng, lambda h: h.activation(out=o, in_=i, func=func, scale=sc, **kw),
                          reads=_bufs(in_, bias, scale), writes=_bufs(out, accum_out))

    def vop(self, name, out, *ins, eng="vector", accum_out=None, **kw):
        o = _ap(out)
        args = [_ap(x) for x in ins]
        kws = {k: _ap(v) for k, v in kw.items()}
        if accum_out is not None:
            kws["accum_out"] = _ap(accum_out)
        return self.P.add(eng, lambda h: getattr(h, name)(o, *args, **kws),
                          reads=_bufs(*ins, *kw.values()), writes=_bufs(out, accum_out))


def run_interleaved(gens, width=2):
    it = iter(gens)
    active = []
    done = False
    while True:
        while not done and len(active) < width:
            g = next(it, None)
            if g is None:
                done = True
                break
            active.append(g)
        if not active:
            break
        for g in list(active):
            try:
                next(g)
            except StopIteration:
                active.remove(g)


def load_bc(k, q, dst, src_ap):
    return k.dma(q, dst, src_ap.partition_broadcast(128))


def load_mod_tiles(k, dr, layer, src, which, norm_w_ap, need_gate=True):
    base = 0 if which == "m" else 3
    mods = dr["mods_d"]
    A = k.alloc("A", [128, D], F32)
    B = k.alloc("B", [128, D], F32)
    tmp = k.alloc("gn", [128, D], F32)
    load_bc(k, "sync", B, mods[src, layer, (base + 0) * D:(base + 1) * D])
    load_bc(k, "sync", A, mods[src, layer, (base + 1) * D:(base + 2) * D])
    load_bc(k, "sync", tmp, norm_w_ap)
    k.vop("scalar_tensor_tensor", A, A, 1.0, tmp, op0=ALU.add, op1=ALU.mult)
    G = None
    if need_gate:
        G = k.alloc("G", [128, D], F32)
        load_bc(k, "sync", G, mods[src, layer, (base + 2) * D:(base + 3) * D])
    return A, B, G


def norm_mod(k, xt, A, B, h_out, junk, st, t32, ss_eng="vector"):
    ss, ms, sq, rs = st[:, 0:1], st[:, 1:2], st[:, 2:3], st[:, 3:4]
    if ss_eng == "vector":
        k.vop("scalar_tensor_tensor", junk, xt, 1.0, xt, op0=ALU.mult, op1=ALU.mult, accum_out=ss)
    else:
        k.act(junk, xt, AF.Square, accum_out=ss)
    k.vop("tensor_scalar", ms, ss, 1.0 / D, EPS, op0=ALU.mult, op1=ALU.add)
    k.act(sq, ms, AF.Sqrt)
    k.vop("reciprocal", rs, sq)
    k.vop("scalar_tensor_tensor", t32, xt, rs, A, op0=ALU.mult, op1=ALU.mult)
    k.vop("tensor_tensor", h_out, t32, B, op=ALU.add)


def norm_mod_g(k, xt, A, B, h_out, junk, st, t32, ss_eng="vector"):
    ss, ms, sq, rs = st[:, 0:1], st[:, 1:2], st[:, 2:3], st[:, 3:4]
    if ss_eng == "vector":
        k.vop("scalar_tensor_tensor", junk, xt, 1.0, xt, op0=ALU.mult, op1=ALU.mult, accum_out=ss)
    else:
        k.act(junk, xt, AF.Square, accum_out=ss)
    yield
    k.vop("tensor_scalar", ms, ss, 1.0 / D, EPS, op0=ALU.mult, op1=ALU.add)
    k.act(sq, ms, AF.Sqrt)
    yield
    k.vop("reciprocal", rs, sq)
    k.vop("scalar_tensor_tensor", t32, xt, rs, A, op0=ALU.mult, op1=ALU.mult)
    k.vop("tensor_tensor", h_out, t32, B, op=ALU.add)
    yield


def transpose_tile(k, h, dst, bank_bf, ident, evac_eng="scalar"):
    for c in range(8):
        k.transpose(bank_bf[:, c * 128:(c + 1) * 128], h[:, c * 128:(c + 1) * 128], ident)
    src = bank_bf.rearrange("p (a b) -> p a b", a=8)
    if evac_eng == "scalar":
        k.act(dst, src, AF.Copy)
    else:
        k.vop("tensor_copy", dst, src)


def phase_adaln(k, dr):
    k.reset()
    cT = k.alloc("cT", [128, 2, 8], F32)
    sT = k.alloc("sT", [128, 2, 8], F32)
    wt = [k.alloc(f"adaw{i}", [128, 8, 512], F32) for i in range(3)]
    bias = k.alloc("adab", [2, 2, 6 * D], F32)
    mods = k.alloc("mods", [2, 2, 6 * D], F32)
    k.dma("sync", cT[:, 0, :], dr["c"][0].rearrange("(p k) -> p k", k=8))
    k.dma("sync", cT[:, 1, :], dr["c_ctx"].rearrange("(p k) -> p k", k=8))
    for j in range(2):
        k.dma("sync", bias[j:j + 1, :, :], dr["ada_b"].rearrange("(o l) n -> o l n", o=1))
    k.act(sT, cT, AF.Silu)
    n = 0
    for layer in range(2):
        wv = dr["ada_w"][layer].rearrange("(p k) n -> p k n", k=8)
        for nchunk in range(12):
            w = wt[n % 3]
            k.dma("sync" if n % 2 == 0 else "scalar", w, wv[:, :, nchunk * 512:(nchunk + 1) * 512])
            ps = k.banks[n % 2]
            for kk in range(8):
                k.mm(ps[0:2, :], sT[:, :, kk], w[:, kk, :], kk == 0, kk == 7)
            sl = slice(nchunk * 512, (nchunk + 1) * 512)
            k.vop("tensor_tensor", mods[0:2, layer, sl], ps[0:2, :], bias[0:2, layer, sl], op=ALU.add)
            n += 1
    k.dma("sync", dr["mods_d"], mods)


GELU = AF.Gelu_apprx_tanh
LRU_BLOCKS = [("ctx", 0, 2, 0)] + [("x", 4 * i, 4, C + 512 * i) for i in range(8)]


def slow_dma(k, q, out, in_):
    return k.dma(q, out, in_, allow_slow_non_contiguous=True)


def phase_lru_in(k, dr):
    k.reset()
    ident = k.alloc("ident", [128, 128], BF16)
    k.dma("gpsimd", ident, dr["ident"])
    wv = dr["lru_in_w"][0].rearrange("(k p) n -> p k n", p=128)
    in_w = []
    for kk in range(8):
        w = k.alloc(f"in_w{kk}", [128, 2048], BF16)
        k.dma("gpsimd", w, wv[:, kk, :], max_dma_last_dim=4096)
        in_w.append(w)
    Ax, Bx, _ = load_mod_tiles(k, dr, 0, 0, "m", dr["norm_mix_w"][0], need_gate=False)
    Ac, Bc, _ = load_mod_tiles(k, dr, 0, 1, "m", dr["norm_mix_w"][0], need_gate=False)
    xt = [k.alloc("xt", [128, D], F32) for _ in range(3)]
    junk = k.alloc("junk", [128, D], BF16)
    t32s = [k.alloc("t32", [128, D], F32) for _ in range(2)]
    hb = [[k.alloc("hb", [128, D], BF16) for _ in range(4)] for _ in range(2)]
    st = [k.alloc("st", [128, 4], F32) for _ in range(4)]
    hT = [k.alloc("hT", [128, 8, 512], BF16) for _ in range(2)]
    gg = [k.alloc("gg", [128, 8, 512], BF16) for _ in range(2)]
    uu = [k.alloc("uu", [128, 8, 512], F32) for _ in range(2)]
    bankbf = [k.banks[6].bitcast(BF16), k.banks[7].bitcast(BF16)]
    cnt = {"tn": 0, "tp": 0}

    def norms(bi):
        src, t0, nt, tok0 = LRU_BLOCKS[bi]
        srcap = dr["ctx"] if src == "ctx" else dr["x"]
        A_, B_ = (Ac, Bc) if src == "ctx" else (Ax, Bx)
        for j in range(nt):
            tn = cnt["tn"]
            cnt["tn"] += 1
            x_ = xt[tn % 3]
            k.dma("sync", x_, srcap[(t0 + j) * 128:(t0 + j + 1) * 128, :])
            norm_mod(k, x_, A_, B_, hb[bi % 2][j], junk, st[tn % 4], t32s[tn % 2], ss_eng="scalar")

    def transposes(bi):
        src, t0, nt, tok0 = LRU_BLOCKS[bi]
        for j in range(nt):
            tp = cnt["tp"]
            cnt["tp"] += 1
            transpose_tile(k, hb[bi % 2][j], hT[bi % 2][:, :, j * 128:(j + 1) * 128], bankbf[tp % 2], ident)

    norms(0)
    transposes(0)
    for bi, (src, t0, nt, tok0) in enumerate(LRU_BLOCKS):
        hTb = hT[bi % 2]
        if bi + 1 < len(LRU_BLOCKS):
            norms(bi + 1)
        ntok = nt * 128
        for oc in range(16):
            ps = k.banks[oc % 6]
            for kk in range(8):
                k.mm(ps[:, :ntok], in_w[kk][:, oc * 128:(oc + 1) * 128], hTb[:, kk, :ntok], kk == 0, kk == 7)
            if oc < 8:
                k.act(gg[bi % 2][:, oc, :ntok], ps[:, :ntok], GELU)
            else:
                k.vop("tensor_copy", uu[bi % 2][:, oc - 8, :ntok], ps[:, :ntok])
        if bi + 1 < len(LRU_BLOCKS):
            transposes(bi + 1)
        k.dma("gpsimd", dr["GG"][:, :, tok0:tok0 + ntok].rearrange("c p t -> p c t"), gg[bi % 2][:, :, :ntok])
        k.dma("gpsimd", dr["UU"][:, :, tok0:tok0 + ntok].rearrange("c p t -> p c t"), uu[bi % 2][:, :, :ntok])


def phase_lru_scan(k, dr):
    k.reset()
    cw = k.alloc("cw", [128, 4, 8], F32)
    cb = k.alloc("cb", [128, 8], F32)
    gab = k.alloc("gab", [128, 2, 8], F32)
    gxb = k.alloc("gxb", [128, 2, 8], F32)
    lam = k.alloc("lam", [128, 2, 8], F32)
    cp = k.alloc("cp", [128, 2, 8], F32)
    for tap in range(4):
        slow_dma(k, "sync", cw[:, tap, :], dr["lru_conv_w"][0, tap].rearrange("(c p) -> p c", p=128))
    slow_dma(k, "sync", cb, dr["lru_conv_b"][0].rearrange("(c p) -> p c", p=128))
    for d in range(2):
        slow_dma(k, "sync", gab[:, d, :], dr["lru_gate_a_b"][0, d].rearrange("(c p) -> p c", p=128))
        slow_dma(k, "sync", gxb[:, d, :], dr["lru_gate_x_b"][0, d].rearrange("(c p) -> p c", p=128))
        slow_dma(k, "sync", lam[:, d, :], dr["lru_lambda"][0, d].rearrange("(c p) -> p c", p=128))
    k.act(cp, lam, AF.Exp, scale=-1.0)
    k.act(cp, cp, AF.Ln, bias=1.0)
    k.vop("tensor_scalar", cp, cp, -8.0, None, op0=ALU.mult)
    gw = k.alloc("gw", [128, 32, 128], BF16)
    k.dma("gpsimd", gw[:, 0:16, :], dr["lru_gate_a_w"][0].rearrange("d n i e -> i (d n) e"))
    k.dma("gpsimd", gw[:, 16:32, :], dr["lru_gate_x_w"][0].rearrange("d n i e -> i (d n) e"))
    XB = 259
    up = [k.alloc("up", [128, TS + 6], F32) for _ in range(1)]
    for u_ in up:
        k.vop("memset", u_, 0.0)
    uconv = k.alloc("uconv", [128, TS], F32)
    ub = k.alloc("ub", [128, TS], BF16)
    rrs = [k.alloc("rr", [128, TS], F32) for _ in range(2)]
    igs = [k.alloc("ig", [128, TS], F32) for _ in range(2)]
    sbs = [k.alloc("sb", [128, TS], F32) for _ in range(2)]
    y = k.alloc("y", [128, TS], F32)
    ggc = [k.alloc("ggc", [128, TS], BF16) for _ in range(1)]
    z = [k.alloc("z", [128, TS], BF16) for _ in range(1)]
    pieces = [(0, 256)] + [(C + 512 * j, 512) for j in range(8)]
    segs = [(2, 0, C), (XB + 2, C, T)]
    nb = 0
    for c in range(8):
        u_ = up[0]
        k.dma("sync", u_[:, 2:2 + C], dr["UU"][c, :, 0:C])
        k.dma("sync", u_[:, XB + 2:XB + 2 + T], dr["UU"][c, :, C:TS])
        k.dma("sync", ggc[0], dr["GG"][c])
        for (sbase, dbase, L) in segs:
            dst = uconv[:, dbase:dbase + L]
            k.vop("tensor_scalar", dst, u_[:, sbase - 2:sbase - 2 + L], cw[:, 0, c:c + 1], cb[:, c:c + 1],
                  op0=ALU.mult, op1=ALU.add)
            for tap in range(1, 4):
                k.vop("scalar_tensor_tensor", dst, u_[:, sbase - 2 + tap:sbase - 2 + tap + L],
                      cw[:, tap, c:c + 1], dst, op0=ALU.mult, op1=ALU.add)
        k.act(ub, uconv, AF.Copy)
        def dir_gen(d, c=c):
            nonlocal nb
            rr, ig, sb = rrs[d], igs[d], sbs[d]
            for pi, (p0, pl) in enumerate(pieces):
                psr = k.banks[nb % 6]
                psi = k.banks[(nb + 1) % 6]
                nb += 2
                k.mm(psr[:, :pl], gw[:, d * 8 + c, :], ub[:, p0:p0 + pl], True, True)
                k.mm(psi[:, :pl], gw[:, 16 + d * 8 + c, :], ub[:, p0:p0 + pl], True, True)
                k.act(rr[:, p0:p0 + pl], psr[:, :pl], AF.Sigmoid, bias=gab[:, d, c:c + 1])
                k.act(ig[:, p0:p0 + pl], psi[:, :pl], AF.Sigmoid, bias=gxb[:, d, c:c + 1])
                if pi % 3 == 2:
                    yield
            yield
            k.act(rr, rr, AF.Exp, scale=cp[:, d, c:c + 1])
            k.vop("tensor_tensor", ig, ig, uconv, op=ALU.mult)
            yield
            k.act(sb, rr, AF.Square)
            yield
            k.act(sb, sb, AF.Sqrt, bias=1.0, scale=-1.0)
            yield
            k.vop("tensor_tensor", ig, ig, sb, op=ALU.mult)
            yield
            hd = y if d == 0 else sb
            if d == 0:
                k.vop("tensor_tensor_scan", hd[:, 0:C], rr[:, 0:C], ig[:, 0:C], 0.0, op0=ALU.mult, op1=ALU.add)
                yield
                k.vop("tensor_tensor_scan", hd[:, C:TS], rr[:, C:TS], ig[:, C:TS], hd[:, C - 1:C],
                      op0=ALU.mult, op1=ALU.add)
            else:
                k.vop("tensor_tensor_scan", hd[:, 0:C][:, ::-1], rr[:, 0:C][:, ::-1], ig[:, 0:C][:, ::-1], 0.0,
                      op0=ALU.mult, op1=ALU.add)
                yield
                k.vop("tensor_tensor_scan", hd[:, C:TS][:, ::-1], rr[:, C:TS][:, ::-1], ig[:, C:TS][:, ::-1],
                      hd[:, 0:1], op0=ALU.mult, op1=ALU.add)

        run_interleaved([dir_gen(0), dir_gen(1)], width=2)
        k.vop("tensor_tensor", y, y, sbs[1], op=ALU.add)
        k.vop("tensor_tensor", z[0], y, ggc[0], op=ALU.mult)
        k.dma("gpsimd", dr["ZZ"][c], z[0])


def phase_lru_out(k, dr):
    k.reset()
    wv = dr["lru_out_w"][0].rearrange("(k p) n -> p k n", p=128)
    ow = []
    for kk in range(8):
        w = k.alloc(f"ow{kk}", [128, D], BF16)
        k.dma("gpsimd", w, wv[:, kk, :], max_dma_last_dim=4096)
        ow.append(w)
    Gx = k.alloc("Gx", [128, D], F32)
    Gc = k.alloc("Gc", [128, D], F32)
    load_bc(k, "sync", Gx, dr["mods_d"][0, 0, 2 * D:3 * D])
    load_bc(k, "sync", Gc, dr["mods_d"][1, 0, 2 * D:3 * D])
    zb = [k.alloc("zb", [128, 8, 512], BF16) for _ in range(2)]
    xt = [k.alloc("xt", [128, D], F32) for _ in range(3)]
    tmp = k.alloc("tmp", [128, D], F32)
    tn = 0
    nb = 0
    for bi, (src, t0, nt, tok0) in enumerate(LRU_BLOCKS):
        ntok = nt * 128
        zt = zb[bi % 2]
        k.dma("sync", zt[:, :, :ntok], dr["ZZ"][:, :, tok0:tok0 + ntok].rearrange("c p t -> p c t"))
        srcap = dr["ctx"] if src == "ctx" else dr["x"]
        dstap = dr["CR1"] if src == "ctx" else dr["XR1"]
        G = Gc if src == "ctx" else Gx
        for j in range(nt):
            x_ = xt[tn % 3]
            tn += 1
            rows = slice((t0 + j) * 128, (t0 + j + 1) * 128)
            k.dma("sync", x_, srcap[rows, :])
            for half in range(2):
                cs = slice(half * 512, (half + 1) * 512)
                ps = k.banks[nb % 4]
                nb += 1
                for kk in range(8):
                    k.mm(ps, zt[:, kk, j * 128:(j + 1) * 128], ow[kk][:, cs], kk == 0, kk == 7)
                k.vop("tensor_tensor", tmp[:, cs], ps, G[:, cs], op=ALU.mult)
                k.vop("tensor_tensor", x_[:, cs], x_[:, cs], tmp[:, cs], op=ALU.add)
            k.dma("gpsimd", dstap[rows, :], x_)


def bcast_last(v, n):
    a = v.ap
    dims = [list(d) for d in a.ap]
    return V(bass.AP(a.tensor, a.offset, dims + [[0, n]]), v.bufs)


def phase_moe(k, dr, layer, srcs, dsts, blocks):
    import os
    dbg_mode = os.environ.get("MOE_DBG", "")
    if dbg_mode:
        blocks = blocks[:1]
    n_exp = {"": NE, "pro": 0, "pro0": 0, "e1": 1, "e2": 2}[dbg_mode]
    k.reset()
    ident = k.alloc("ident32", [128, 128], F32)
    k.dma("sync", ident, dr["ident"])
    rw = k.alloc("rw", [128, 8, NE], F32)
    k.dma("sync", rw, dr["router_w"].rearrange("(k p) e -> p k e", p=128))
    rb = k.alloc("rb", [128, NE], F32)
    load_bc(k, "sync", rb, dr["router_b"])
    mod = {}
    for kind, si in (("x", 0), ("ctx", 1)):
        if kind in srcs:
            mod[kind] = load_mod_tiles(k, dr, layer, si, "f", dr["norm_ffn_w"][layer])
    maxnt = max(len(b) for b in blocks)
    xt = [k.alloc("xt", [128, D], F32) for _ in range(2)]
    h32 = [k.alloc("h32", [128, D], F32) for _ in range(2)]
    junk = k.alloc("junk", [128, D], BF16)
    t32 = k.alloc("t32", [128, D], F32)
    st = [k.alloc("st", [128, 4], F32) for _ in range(2)]
    hT32 = [k.alloc("hT32", [128, 8, 128], F32) for _ in range(2)]
    hTb = k.alloc("hTb", [128, 8, maxnt * 128], BF16)
    gates = k.alloc("gates", [128, maxnt, NE], F32)
    rs = [k.alloc("rs", [128, 160], F32) for _ in range(2)]
    w1 = [k.alloc("w1", [128, 8, FF], BF16) for _ in range(2)]
    w3 = [k.alloc("w3", [128, 8, FF], BF16) for _ in range(2)]
    w2 = [k.alloc("w2", [128, 4, D], BF16) for _ in range(2)]
    ssb = [k.alloc("ssb", [128, 512], F32) for _ in range(2)]
    actT = [k.alloc("actT", [128, 4, maxnt * 128], BF16) for _ in range(2)]
    yacc = k.alloc("yacc", [128, maxnt, D], F32)
    tmp = k.alloc("tmp", [128, D], F32)
    tn = 0
    ne = 0
    nh = 0
    ny = 0
    for blk in blocks:
        nt = len(blk)
        ntok = nt * 128
        for j, (kind, ti) in enumerate(blk):
            x_ = xt[tn % 2]
            h_ = h32[tn % 2]
            r_ = rs[tn % 2]
            A_, B_, _ = mod[kind]
            rows = slice(ti * 128, (ti + 1) * 128)
            k.dma("sync", x_, srcs[kind][rows, :])
            norm_mod(k, x_, A_, B_, h_, junk, st[tn % 2], t32)
            for hb_ in range(2):
                bank = k.banks[6 + hb_]
                for c4 in range(4):
                    c = hb_ * 4 + c4
                    k.transpose(bank[:, c4 * 128:(c4 + 1) * 128], h_[:, c * 128:(c + 1) * 128], ident)
                src = bank.rearrange("p (a b) -> p a b", a=4)
                k.vop("tensor_copy", hT32[tn % 2][:, hb_ * 4:hb_ * 4 + 4, :], src)
            k.act(hTb[:, :, j * 128:(j + 1) * 128], hT32[tn % 2], AF.Copy)
            lps = k.banks[5]
            for kk in range(8):
                k.mm(lps[:, 0:NE], hT32[tn % 2][:, kk, :], rw[:, kk, :], kk == 0, kk == 7)
            if dbg_mode == "pro0":
                k.vop("tensor_copy", gates[:, j, :], lps[:, 0:NE])
                tn += 1
                continue
            lg = r_[:, 0:16]
            pg = r_[:, 16:32]
            p6 = r_[:, 32:56].rearrange("p (a b) -> p a b", a=4)
            msk = r_[:, 56:72]
            eq1 = r_[:, 72:88]
            sel = r_[:, 88:104]
            gsel = r_[:, 104:120]
            gs = r_[:, 120:124]
            oh = r_[:, 124:128]
            ohm = r_[:, 128:132]
            mx, nmx, gm, v1, v2, den, rden = (r_[:, 132 + i:133 + i] for i in range(7))
            pgv = pg.rearrange("p (a b) -> p a b", a=4)
            mskv = msk.rearrange("p (a b) -> p a b", a=4)
            k.vop("tensor_tensor", lg, lps[:, 0:NE], rb, op=ALU.add)
            k.vop("tensor_reduce", mx, lg, axis=AX.X, op=ALU.max)
            k.vop("tensor_scalar", nmx, mx, -1.0, None, op0=ALU.mult)
            k.act(pg, lg, AF.Exp, bias=nmx)
            k.vop("tensor_tensor", p6[:, :, 0:3], pgv[:, :, 0:3], pgv[:, :, 1:4], op=ALU.add)
            k.vop("tensor_tensor", p6[:, :, 3:5], pgv[:, :, 0:2], pgv[:, :, 2:4], op=ALU.add)
            k.vop("tensor_tensor", p6[:, :, 5:6], pgv[:, :, 0:1], pgv[:, :, 3:4], op=ALU.add)
            k.vop("tensor_reduce", gs, p6, axis=AX.X, op=ALU.max)
            k.vop("tensor_reduce", gm, gs, axis=AX.X, op=ALU.max)
            k.vop("tensor_scalar", oh, gs, gm, None, op0=ALU.is_equal)
            k.vop("tensor_scalar", ohm, oh, -1.0, None, op0=ALU.add)
            k.vop("tensor_tensor", mskv, pgv, bcast_last(oh, 4), op=ALU.mult)
            k.vop("tensor_tensor", mskv, mskv, bcast_last(ohm, 4), op=ALU.add)
            k.vop("tensor_reduce", v1, msk, axis=AX.X, op=ALU.max)
            k.vop("tensor_scalar", eq1, msk, v1, None, op0=ALU.is_equal)
            k.vop("scalar_tensor_tensor", eq1, eq1, -2.0, msk, op0=ALU.mult, op1=ALU.add)
            k.vop("tensor_reduce", v2, eq1, axis=AX.X, op=ALU.max)
            k.vop("tensor_scalar", sel, msk, v2, None, op0=ALU.is_ge)
            k.vop("scalar_tensor_tensor", gsel, msk, 1.0, sel, op0=ALU.mult, op1=ALU.mult, accum_out=den)
            k.vop("reciprocal", rden, den)
            k.vop("tensor_scalar", gates[:, j, :], gsel, rden, None, op0=ALU.mult)
            tn += 1
        hs = ntok // 2
        for e in range(n_exp):
            wb = ne % 2
            ne += 1
            k.dma("gpsimd", w1[wb], dr["moe_w1"][layer, e].rearrange("(k p) f -> p k f", p=128))
            k.dma("gpsimd", w3[wb], dr["moe_w3"][layer, e].rearrange("(k p) f -> p k f", p=128))
            k.dma("gpsimd", w2[wb], dr["moe_w2"][layer, e].rearrange("(k p) n -> p k n", p=128),
                  max_dma_last_dim=4096)
            aT = actT[e % 2]
            for half in range(2):
                cs = slice(half * hs, (half + 1) * hs)
                for fc in range(4):
                    ps1 = k.banks[(nh % 2) * 2]
                    ps3 = k.banks[(nh % 2) * 2 + 1]
                    s_ = ssb[nh % 2]
                    nh += 1
                    for kk in range(8):
                        k.mm(ps1[:, :hs], w1[wb][:, kk, fc * 128:(fc + 1) * 128], hTb[:, kk, cs], kk == 0, kk == 7)
                    for kk in range(8):
                        k.mm(ps3[:, :hs], w3[wb][:, kk, fc * 128:(fc + 1) * 128], hTb[:, kk, cs], kk == 0, kk == 7)
                    k.act(s_[:, :hs], ps1[:, :hs], AF.Silu)
                    k.vop("tensor_tensor", aT[:, fc, cs], s_[:, :hs], ps3[:, :hs], op=ALU.mult)
            for j in range(nt):
                for dh in range(2):
                    psy = k.banks[4 + ny % 2]
                    ny += 1
                    ds_ = slice(dh * 512, (dh + 1) * 512)
                    for fc in range(4):
                        k.mm(psy, aT[:, fc, j * 128:(j + 1) * 128], w2[wb][:, fc, ds_], fc == 0, fc == 3)
                    if e == 0:
                        k.vop("tensor_scalar", yacc[:, j, ds_], psy, gates[:, j, e:e + 1], None, op0=ALU.mult)
                    else:
                        k.vop("scalar_tensor_tensor", yacc[:, j, ds_], psy, gates[:, j, e:e + 1], yacc[:, j, ds_],
                              op0=ALU.mult, op1=ALU.add)
        for j, (kind, ti) in enumerate(blk):
            x_ = xt[tn % 2]
            tn += 1
            rows = slice(ti * 128, (ti + 1) * 128)
            G = mod[kind][2]
            k.dma("sync", x_, srcs[kind][rows, :])
            if n_exp == 0:
                k.vop("tensor_copy", yacc[:, j, 0:NE], gates[:, j, :])
                k.vop("tensor_copy", yacc[:, j, NE:D], h32[0][:, NE:D])
            k.vop("tensor_tensor", tmp, yacc[:, j, :], G, op=ALU.mult)
            k.vop("tensor_tensor", x_, x_, tmp, op=ALU.add)
            k.dma("sync", dsts[kind][rows, :], x_)


NB = 12
NSLOT = NB * 512
U32 = mybir.dt.uint32
I32 = mybir.dt.int32


def phase_moe_sorted(k, dr, layer, srcs, dsts, tiles):
    nt = len(tiles)
    nblk = (nt * 128 + 4 * 511) // 512
    assert nblk <= NB
    k.reset()
    Hs, Ys, Gs = dr["Hs"], dr["Ys"], dr["Gs"]
    slot_u = k.alloc("slot_u", [128, nt], U32)
    g512 = k.alloc("g512", [128, NB], F32)
    g2048 = k.alloc("g2048", [128, NB], F32)
    mark = k.off
    ident = k.alloc("ident32", [128, 128], F32)
    k.dma("sync", ident, dr["ident"])
    ltri = k.alloc("ltri", [128, 128], F32)
    k.dma("sync", ltri, dr["ltri"])
    ones = k.alloc("ones", [128, 128], F32)
    k.vop("memset", ones, 1.0)
    rw = k.alloc("rw", [128, 8, NE], F32)
    k.dma("sync", rw, dr["router_w"].rearrange("(k p) e -> p k e", p=128))
    rb = k.alloc("rb", [128, NE], F32)
    load_bc(k, "sync", rb, dr["router_b"])
    mod = {}
    for kind, si in (("x", 0), ("ctx", 1)):
        if kind in srcs:
            mod[kind] = load_mod_tiles(k, dr, layer, si, "f", dr["norm_ffn_w"][layer], need_gate=False)
    xt = [k.alloc("xt", [128, D], F32) for _ in range(3)]
    h32 = [k.alloc("h32", [128, D], F32) for _ in range(2)]
    junk = k.alloc("junk", [128, D], BF16)
    t32s = [k.alloc("t32", [128, D], F32) for _ in range(2)]
    st = [k.alloc("st", [128, 4], F32) for _ in range(3)]
    hT32 = [k.alloc("hT32", [128, 8, 128], F32) for _ in range(2)]
    hb_all = k.alloc("hb_all", [128, nt, D], BF16)
    lgall = k.alloc("lgall", [128, nt, NE], F32)
    junks = [junk, k.alloc("junk2", [128, D], BF16)]

    def m1_gen(j, kind, ti):
        x_ = xt[j % 3]
        h_ = h32[j % 2]
        A_, B_, _ = mod[kind]
        rows = slice(ti * 128, (ti + 1) * 128)
        k.dma("sync", x_, srcs[kind][rows, :])
        yield from norm_mod_g(k, x_, A_, B_, h_, junks[j % 2], st[j % 3], t32s[j % 2], ss_eng="scalar")
        k.act(hb_all[:, j, :], h_, AF.Copy)
        for hb_ in range(2):
            bank = k.banks[6 + hb_]
            for c4 in range(4):
                c = hb_ * 4 + c4
                k.transpose(bank[:, c4 * 128:(c4 + 1) * 128], h_[:, c * 128:(c + 1) * 128], ident)
            k.vop("tensor_copy", hT32[j % 2][:, hb_ * 4:hb_ * 4 + 4, :], bank.rearrange("p (a b) -> p a b", a=4))
        yield
        lps = k.banks[4 + j % 2]
        for kk in range(8):
            k.mm(lps[:, 0:NE], hT32[j % 2][:, kk, :], rw[:, kk, :], kk == 0, kk == 7)
        k.vop("tensor_tensor", lgall[:, j, :], lps[:, 0:NE], rb, op=ALU.add)

    run_interleaved((m1_gen(j, kind, ti) for j, (kind, ti) in enumerate(tiles)), width=2)
    def al(name, n):
        return k.alloc(name, [128, n], F32)
    pg = al("pg", nt * 16); p6 = al("p6", nt * 24); msk = al("msk", nt * 16); eq1 = al("eq1", nt * 16)
    sel = al("sel", nt * 16); gates = al("gates", nt * 16); glp = al("glp", nt * 16)
    gs = al("gs", nt * 4); oh = al("oh", nt * 4); ohm = al("ohm", nt * 4); t4 = al("t4", nt * 4)
    tot = al("tot", nt * 4); cum = al("cum", nt * 4)
    mx = al("mx", nt); gm = al("gm", nt); v1 = al("v1", nt); v2 = al("v2", nt); den = al("den", nt)
    slot_f = al("slot_f", nt); onesr = al("onesr", nt)
    ng = al("ng", 4); cnt = al("cnt", 4); base = al("base", 4); endg = al("endg", 4)
    thr = al("thr", 9); cmp_ = al("cmp", 36); blk0 = al("blk0", NB); gidf = al("gidf", NB); tmpb = al("tmpb", NB)

    def v3(v, a, b):
        return v.rearrange("p (a b) -> p a b", a=a)
    lg3 = lgall
    k.vop("tensor_reduce", mx, lg3, axis=AX.X, op=ALU.max)
    k.vop("tensor_tensor", v3(pg, nt, 16), lg3, bcast_last(mx, 16), op=ALU.subtract)
    k.act(pg, pg, AF.Exp)
    pgv = v3(pg, nt * 4, 4)
    p6v = v3(p6, nt * 4, 6)
    k.vop("tensor_tensor", p6v[:, :, 0:3], pgv[:, :, 0:3], pgv[:, :, 1:4], op=ALU.add)
    k.vop("tensor_tensor", p6v[:, :, 3:5], pgv[:, :, 0:2], pgv[:, :, 2:4], op=ALU.add)
    k.vop("tensor_tensor", p6v[:, :, 5:6], pgv[:, :, 0:1], pgv[:, :, 3:4], op=ALU.add)
    k.vop("tensor_reduce", gs, p6v, axis=AX.X, op=ALU.max)
    k.vop("tensor_reduce", gm, v3(gs, nt, 4), axis=AX.X, op=ALU.max)
    k.vop("tensor_tensor", v3(oh, nt, 4), v3(gs, nt, 4), bcast_last(gm, 4), op=ALU.is_equal)
    k.vop("tensor_scalar", ohm, oh, -1.0, None, op0=ALU.add)
    mskv = v3(msk, nt * 4, 4)
    k.vop("tensor_tensor", mskv, pgv, bcast_last(oh, 4), op=ALU.mult)
    k.vop("tensor_tensor", mskv, mskv, bcast_last(ohm, 4), op=ALU.add)
    msk3 = v3(msk, nt, 16)
    k.vop("tensor_reduce", v1, msk3, axis=AX.X, op=ALU.max)
    k.vop("tensor_tensor", v3(eq1, nt, 16), msk3, bcast_last(v1, 16), op=ALU.is_equal)
    k.vop("scalar_tensor_tensor", eq1, eq1, -2.0, msk, op0=ALU.mult, op1=ALU.add)
    k.vop("tensor_reduce", v2, v3(eq1, nt, 16), axis=AX.X, op=ALU.max)
    k.vop("tensor_tensor", v3(sel, nt, 16), msk3, bcast_last(v2, 16), op=ALU.is_ge)
    k.vop("tensor_tensor", sel, sel, msk, op=ALU.mult)
    k.vop("tensor_reduce", den, v3(sel, nt, 16), axis=AX.X, op=ALU.add)
    k.vop("reciprocal", den, den)
    k.vop("tensor_tensor", v3(gates, nt, 16), v3(sel, nt, 16), bcast_last(den, 16), op=ALU.mult)
    k.vop("memset", glp, 0.0)
    k.vop("tensor_reduce", v3(glp, nt, 16)[:, :, 0:4], apv(gates, [[16, nt], [1, 4], [4, 4]]), axis=AX.X, op=ALU.add)
    pre_ps = k.banks[4]
    tot_ps = k.banks[5]
    k.mm(pre_ps[:, 0:nt * 4], ltri, oh, True, True)
    k.mm(tot_ps[:, 0:nt * 4], ones, oh, True, True)
    k.vop("tensor_copy", tot, tot_ps[:, 0:nt * 4])
    k.vop("memset", onesr, 1.0)
    for g in range(4):
        k.vop("tensor_tensor_scan", apv(cum, [[4, nt]], extra_off=g), onesr, apv(tot, [[4, nt]], extra_off=g), 0.0,
              op0=ALU.mult, op1=ALU.add)
    k.vop("tensor_copy", ng, cum[:, (nt - 1) * 4:nt * 4])
    k.vop("tensor_tensor", cum, cum, tot, op=ALU.subtract)
    for m in range(9):
        k.vop("memset", thr[:, m:m + 1], float(512 * m))
    k.vop("tensor_tensor", v3(cmp_, 4, 9), bcast_last(ng, 9), apv(thr, [[0, 4], [1, 9]]), op=ALU.is_gt)
    k.vop("tensor_reduce", cnt, v3(cmp_, 4, 9), axis=AX.X, op=ALU.add)
    k.vop("tensor_scalar", cnt, cnt, 512.0, None, op0=ALU.mult)
    k.vop("memset", base[:, 0:1], 0.0)
    for g in range(1, 4):
        k.vop("tensor_tensor", base[:, g:g + 1], base[:, g - 1:g], cnt[:, g - 1:g], op=ALU.add)
    k.vop("tensor_tensor", endg, base, cnt, op=ALU.add)
    k.vop("tensor_tensor", t4, cum, pre_ps[:, 0:nt * 4], op=ALU.add)
    k.vop("tensor_tensor", v3(t4, nt, 4), v3(t4, nt, 4), apv(base, [[0, nt], [1, 4]]), op=ALU.add)
    k.vop("tensor_tensor", t4, t4, oh, op=ALU.mult)
    k.vop("tensor_reduce", slot_f, v3(t4, nt, 4), axis=AX.X, op=ALU.add)
    k.vop("tensor_copy", slot_u, slot_f)
    for i in range(NB):
        k.vop("memset", blk0[:, i:i + 1], float(512 * i))
    k.vop("tensor_scalar", gidf, blk0, endg[:, 0:1], None, op0=ALU.is_ge)
    for g in (1, 2):
        k.vop("tensor_scalar", tmpb, blk0, endg[:, g:g + 1], None, op0=ALU.is_ge)
        k.vop("tensor_tensor", gidf, gidf, tmpb, op=ALU.add)
    k.vop("tensor_scalar", g512, gidf, 512.0, None, op0=ALU.mult)
    k.vop("tensor_scalar", g2048, gidf, 2048.0, None, op0=ALU.mult)
    zt = k.alloc("zt", [128, 4096], BF16)
    k.vop("memset", zt, 0.0)
    zg = k.alloc("zg", [128, NSLOT * 16 // 128], F32)
    k.vop("memset", zg, 0.0)
    for i in range(NB):
        k.dma("sync", Hs[i * 512:(i + 1) * 512, :].rearrange("(p r) c -> p (r c)", p=128), zt)
    k.dma("sync", Gs.rearrange("(p r) c -> p (r c)", p=128), zg)
    k.P.barrier()
    for j in range(nt):
        idx = slot_u[:, j:j + 1]
        src_h = hb_all[:, j, :]
        src_g = v3(glp, nt, 16)[:, j, :]

        def sc_h(h, idx=idx, src=src_h):
            return h.indirect_dma_start(out=Hs, out_offset=bass.IndirectOffsetOnAxis(ap=idx.ap, axis=0),
                                        in_=src.ap, in_offset=None)

        def sc_g(h, idx=idx, src=src_g):
            return h.indirect_dma_start(out=Gs, out_offset=bass.IndirectOffsetOnAxis(ap=idx.ap, axis=0),
                                        in_=src.ap, in_offset=None)
        k.P.add("gpsimd", sc_h, reads=list(slot_u.bufs) + list(hb_all.bufs), dma=True)
        k.P.add("gpsimd", sc_g, reads=list(slot_u.bufs) + list(glp.bufs), dma=True)
    k.P.barrier()
    k.off = mark
    identb = k.alloc("identb", [128, 128], BF16)
    k.dma("gpsimd", identb, dr["ident"])
    ht4 = [k.alloc("ht4", [128, 4, D], BF16) for _ in range(2)]
    gt4 = [k.alloc("gt4", [128, 4, 16], F32) for _ in range(2)]
    hTb = [k.alloc("hTb", [128, 8, 512], BF16) for _ in range(2)]
    w1 = [k.alloc("w1", [128, 8 * FF], BF16) for _ in range(2)]
    w3 = [k.alloc("w3", [128, 8 * FF], BF16) for _ in range(2)]
    wst = [k.alloc("wst", [128, 8 * FF], F32) for _ in range(4)]
    w2 = [[k.alloc("w2", [128, D], BF16) for _ in range(4)] for _ in range(2)]
    ssb = [k.alloc("ssb", [128, 512], F32) for _ in range(2)]
    actT = [k.alloc("actT", [128, 4, 512], BF16) for _ in range(2)]
    yacc = [k.alloc("yacc", [128, 4, D], F32) for _ in range(2)]
    bankbf = [k.banks[6].bitcast(BF16), k.banks[7].bitcast(BF16)]
    cA = k.alloc("cA", [128, 32], F32)
    cB = k.alloc("cB", [128, 16], F32)
    k.dma("sync", cA, dr["idxA"])
    k.dma("sync", cB, dr["idxB"])
    idxA = [k.alloc("idxA", [128, 32], U32) for _ in range(2)]
    idxB = [k.alloc("idxB", [128, 16], U32) for _ in range(2)]
    W1f = dr["moe_w1"].rearrange("l e (p k) c -> (l e p) (k c)", k=8)
    W3f = dr["moe_w3"].rearrange("l e (p k) c -> (l e p) (k c)", k=8)
    W2f = dr["moe_w2"].rearrange("l e f c -> (l e f) c")
    ne = 0
    nh = 0
    ny = 0
    tpc = {"n": 0}

    def do_transposes(bi):
        h4_ = ht4[bi % 2]
        hTd = hTb[bi % 2]
        for a in range(4):
            bb = bankbf[tpc["n"] % 2]
            tpc["n"] += 1
            hv = h4_[:, a, :].rearrange("p (f k) -> p k f", k=8)
            for c in range(8):
                k.transpose(bb[:, c * 128:(c + 1) * 128], hv[:, c, :], identb)
            k.vop("tensor_copy", hTd[:, :, a * 128:(a + 1) * 128], bb.rearrange("p (a b) -> p a b", a=8))
    regs = {}
    for i in range(nblk):
        rows = slice(i * 512, (i + 1) * 512)
        h4 = ht4[i % 2]
        g4 = gt4[i % 2]
        hT_ = hTb[i % 2]
        ya = yacc[i % 2]
        if i == 0:
            k.dma("sync", h4, Hs[rows, :].rearrange("(a p) c -> p a c", p=128))
            k.dma("sync", g4, Gs[rows, :].rearrange("(a p) c -> p a c", p=128))
        if i + 1 < nblk:
            nrows = slice((i + 1) * 512, (i + 2) * 512)
            k.dma("sync", ht4[(i + 1) % 2], Hs[nrows, :].rearrange("(a p) c -> p a c", p=128))
            k.dma("sync", gt4[(i + 1) % 2], Gs[nrows, :].rearrange("(a p) c -> p a c", p=128))
        if i == 0:
            do_transposes(0)
        for el in range(4):
            wb = ne % 2
            ne += 1
            if el == 0:
                k.vop("tensor_scalar", idxA[i % 2][:, 0:4], cA[:, 0:4], g512[:, i:i + 1], float(layer * NE * 128),
                      op0=ALU.add, op1=ALU.add)
                k.vop("tensor_scalar", idxB[i % 2], cB, g2048[:, i:i + 1], float(layer * NE * FF), op0=ALU.add, op1=ALU.add)

            def gath(dst, srcW, idxv):
                def f(h):
                    return h.indirect_dma_start(out=dst.ap, out_offset=None, in_=srcW,
                                                in_offset=bass.IndirectOffsetOnAxis(ap=idxv.ap, axis=0))
                k.P.add("gpsimd", f, reads=list(idxv.bufs), writes=list(dst.bufs), dma=True)
            s1 = wst[(2 * ne) % 4]
            s3 = wst[(2 * ne + 1) % 4]
            gath(s1, W1f, idxA[i % 2][:, el:el + 1])
            gath(s3, W3f, idxA[i % 2][:, el:el + 1])
            k.act(w1[wb], s1, AF.Copy)
            k.act(w3[wb], s3, AF.Copy)
            for kk in range(4):
                gath(w2[wb][kk], W2f, idxB[i % 2][:, el * 4 + kk:el * 4 + kk + 1])
            aT = actT[ne % 2]
            for fc in range(4):
                ps1 = k.banks[(nh % 2) * 2]
                ps3 = k.banks[(nh % 2) * 2 + 1]
                s_ = ssb[nh % 2]
                nh += 1
                for kk in range(8):
                    k.mm(ps1, w1[wb][:, kk * FF + fc * 128:kk * FF + (fc + 1) * 128], hT_[:, kk, :], kk == 0, kk == 7)
                for kk in range(8):
                    k.mm(ps3, w3[wb][:, kk * FF + fc * 128:kk * FF + (fc + 1) * 128], hT_[:, kk, :], kk == 0, kk == 7)
                k.act(s_, ps1, AF.Silu)
                k.vop("tensor_tensor", aT[:, fc, :], s_, ps3, op=ALU.mult)
            if el == 1 and i + 1 < nblk:
                do_transposes(i + 1)
            for a in range(4):
                for dh in range(2):
                    psy = k.banks[4 + ny % 2]
                    ny += 1
                    ds_ = slice(dh * 512, (dh + 1) * 512)
                    for fc in range(4):
                        k.mm(psy, aT[:, fc, a * 128:(a + 1) * 128], w2[wb][fc][:, ds_], fc == 0, fc == 3)
                    if el == 0:
                        k.vop("tensor_scalar", ya[:, a, ds_], psy, g4[:, a, el:el + 1], None, op0=ALU.mult)
                    else:
                        k.vop("scalar_tensor_tensor", ya[:, a, ds_], psy, g4[:, a, el:el + 1], ya[:, a, ds_],
                              op0=ALU.mult, op1=ALU.add)
        k.dma("sync", Ys[rows, :].rearrange("(a p) c -> p a c", p=128), ya)
    k.P.barrier()
    k.off = mark
    Gt = {}
    base_i = 3
    for kind, si in (("x", 0), ("ctx", 1)):
        if kind in srcs:
            Gt[kind] = k.alloc("G", [128, D], F32)
            load_bc(k, "sync", Gt[kind], dr["mods_d"][si, layer, (base_i + 2) * D:(base_i + 3) * D])
    xt = [k.alloc("xt", [128, D], F32) for _ in range(6)]
    yt = [k.alloc("yt", [128, D], F32) for _ in range(6)]
    for j, (kind, ti) in enumerate(tiles):
        x_ = xt[j % 6]
        y_ = yt[j % 6]
        rows = slice(ti * 128, (ti + 1) * 128)
        idx = slot_u[:, j:j + 1]

        def ga(h, idx=idx, dst=y_):
            return h.indirect_dma_start(out=dst.ap, out_offset=None, in_=Ys,
                                        in_offset=bass.IndirectOffsetOnAxis(ap=idx.ap, axis=0))
        k.P.add("gpsimd", ga, reads=list(slot_u.bufs), writes=list(y_.bufs), dma=True)
        k.dma("scalar", x_, srcs[kind][rows, :])
        k.vop("tensor_tensor", y_, y_, Gt[kind], op=ALU.mult)
        k.vop("tensor_tensor", x_, x_, y_, op=ALU.add)
        k.dma("sync", dsts[kind][rows, :], x_)


def phase_moe0(k, dr):
    tiles = [("ctx", 0), ("ctx", 1)] + [("x", i) for i in range(NXT)]
    blocks = [tiles[0:7], tiles[7:14], tiles[14:21], tiles[21:28], tiles[28:34]]
    import os
    if os.environ.get("MOE_SRC_X"):
        srcs = {"x": dr["x"], "ctx": dr["ctx"]}
    else:
        srcs = {"x": dr["XR1"], "ctx": dr["CR1"]}
    if os.environ.get("MOE_DENSE"):
        phase_moe(k, dr, 0, srcs, {"x": dr["XR2"], "ctx": dr["CR2"]}, blocks)
    else:
        phase_moe_sorted(k, dr, 0, srcs, {"x": dr["XR2"], "ctx": dr["CR2"]}, tiles)


def phase_moe1(k, dr):
    tiles = [("x", i) for i in range(NXT)]
    blocks = [tiles[8 * i:8 * i + 8] for i in range(4)]
    import os
    if os.environ.get("MOE_DENSE"):
        phase_moe(k, dr, 1, {"x": dr["XR3"]}, {"x": dr["out"]}, blocks)
    else:
        phase_moe_sorted(k, dr, 1, {"x": dr["XR3"]}, {"x": dr["out"]}, tiles)


QPAIRS = [(0, 4), (1, 5), (2, 6), (3, 7), (8, 12), (9, 13), (10, 14), (11, 15)]
NKT = TS // 128
ATT = {}


def apv(v, dims, extra_off=0):
    a = v.ap
    return V(bass.AP(a.tensor, a.offset + extra_off, [list(a.ap[0])] + [list(d) for d in dims]), v.bufs)


def phase_attn_proj(k, dr):
    k.reset()
    kT = k.alloc("kT", [128, 2, TS], BF16)
    Vaug = k.alloc("Vaug", [128, NKT * 4, 128], BF16)
    ATT["kT"], ATT["Vaug"], ATT["keep"] = kT, Vaug, k.off
    k.vop("memset", Vaug[:, :, 64:128], 1.0)
    ident = k.alloc("ident", [128, 128], BF16)
    k.dma("gpsimd", ident, dr["ident"])
    wv = dr["attn_qkv_w"][0].rearrange("(k p) n -> p k n", p=128)
    qkvw = []
    for kk in range(8):
        w = k.alloc(f"qkvw{kk}", [128, 1536], BF16)
        for G_ in range(2):
            for a_ in range(2):
                src = wv[:, kk, (8 * G_ + 4 * a_) * 64:(8 * G_ + 4 * a_ + 4) * 64].rearrange("p (i d) -> p i d", i=4)
                dst = w[:, 8 * G_ * 64:8 * G_ * 64 + 512].rearrange("p (i a d) -> p i a d", i=4, a=2)[:, :, a_, :]
                k.dma("gpsimd", dst, src)
        k.dma("gpsimd", w[:, 1024:1536], wv[:, kk, 1024:1536], max_dma_last_dim=2048)
        qkvw.append(w)
    Ax, Bx, _ = load_mod_tiles(k, dr, 1, 0, "m", dr["norm_mix_w"][1], need_gate=False)
    Ac, Bc, _ = load_mod_tiles(k, dr, 1, 1, "m", dr["norm_mix_w"][1], need_gate=False)
    gq = k.alloc("gq", [128, 20, 64], F32)
    g64 = k.alloc("g64", [128, 2, 64], F32)
    load_bc(k, "sync", g64[:, 0, :], dr["attn_q_norm_w"][0])
    load_bc(k, "sync", g64[:, 1, :], dr["attn_k_norm_w"][0])
    k.vop("tensor_copy", gq[:, 0:16, :], apv(g64[:, 0, :], [[0, 16], [1, 64]]))
    k.vop("tensor_copy", gq[:, 16:20, :], apv(g64[:, 1, :], [[0, 4], [1, 64]]))
    rp = k.alloc("rp", [128, NXT, 64], F32)
    k.dma("sync", rp, dr["rope"].rearrange("(n p) c -> p n c", p=128))
    xt = [k.alloc("xt", [128, D], F32) for _ in range(2)]
    junks = [k.alloc("junk", [128, D], BF16) for _ in range(2)]
    t32s = [k.alloc("t32", [128, D], F32) for _ in range(2)]
    hb = [k.alloc("hb", [128, D], BF16) for _ in range(2)]
    st = [k.alloc("st", [128, 4], F32) for _ in range(2)]
    hT = [k.alloc("hT", [128, 8, 128], BF16) for _ in range(2)]
    qk32 = [k.alloc("qk32", [128, 20, 64], F32) for _ in range(2)]
    sqs = [k.alloc("sq", [128, 20, 64], F32) for _ in range(2)]
    rst = [k.alloc("rst", [128, 64], F32) for _ in range(2)]
    tcs = [[k.alloc("tc", [128, 20, 32], F32) for _ in range(2)] for _ in range(2)]
    tss = [[k.alloc("ts", [128, 20, 32], F32) for _ in range(2)] for _ in range(2)]
    qr = [k.alloc("qr", [128, 20, 64], BF16) for _ in range(2)]
    qst = [k.alloc("qst", [128, 8, 512], BF16) for _ in range(2)]
    tiles = [("ctx", j) for j in range(NCT)] + [("x", i) for i in range(NXT)]

    def tile_gen(kind, ti, tn):
        isx = kind == "x"
        kt = ti if not isx else NCT + ti
        x_ = xt[tn % 2]
        A_, B_ = (Ax, Bx) if isx else (Ac, Bc)
        rows = slice(ti * 128, (ti + 1) * 128)
        k.dma("sync", x_, (dr["XR2"] if isx else dr["CR2"])[rows, :])
        yield from norm_mod_g(k, x_, A_, B_, hb[tn % 2], junks[tn % 2], st[tn % 2], t32s[tn % 2], ss_eng="scalar")
        hbank = k.banks[4 + tn % 2].bitcast(BF16)
        hT_ = hT[tn % 2]
        transpose_tile(k, hb[tn % 2], hT_, hbank, ident)
        yield
        sq = sqs[tn % 2]
        q32 = qk32[tn % 2]
        q32f = q32.rearrange("p a b -> p (a b)")
        if isx:
            for nbk in range(3):
                for kk in range(8):
                    k.mm(k.banks[nbk], hT_[:, kk, :], qkvw[kk][:, nbk * 512:(nbk + 1) * 512], kk == 0, kk == 7)
            k.act(q32f[:, 0:512], k.banks[0], AF.Copy)
            k.act(q32f[:, 512:1024], k.banks[1], AF.Copy)
            k.act(q32f[:, 1024:1280], k.banks[2][:, 0:256], AF.Copy)
            vsrc = k.banks[2][:, 256:512]
            h0, nh = 0, 20
        else:
            for kk in range(8):
                k.mm(k.banks[2], hT_[:, kk, :], qkvw[kk][:, 1024:1536], kk == 0, kk == 7)
            k.act(q32f[:, 1024:1280], k.banks[2][:, 0:256], AF.Copy)
            vsrc = k.banks[2][:, 256:512]
            h0, nh = 16, 4
        k.act(Vaug[:, kt * 4:kt * 4 + 4, 0:64], vsrc.rearrange("p (g d) -> p g d", g=4), AF.Copy)
        yield
        qh = q32[:, h0:h0 + nh, :]
        r_ = rst[tn % 2]
        k.vop("tensor_tensor", sq[:, h0:h0 + nh, :], qh, qh, op=ALU.mult)
        k.vop("tensor_reduce", r_[:, 0:nh], sq[:, h0:h0 + nh, :], axis=AX.X, op=ALU.add)
        k.vop("tensor_scalar", r_[:, 20:20 + nh], r_[:, 0:nh], 1.0 / 64, EPS, op0=ALU.mult, op1=ALU.add)
        k.act(r_[:, 40:40 + nh], r_[:, 20:20 + nh], AF.Sqrt)
        yield
        k.vop("reciprocal", r_[:, 0:nh], r_[:, 40:40 + nh])
        k.vop("tensor_tensor", qh, qh, bcast_last(r_[:, 0:nh], 64), op=ALU.mult)
        q_ = qr[tn % 2]
        if isx:
            k.vop("tensor_tensor", qh, qh, gq[:, h0:h0 + nh, :], op=ALU.mult)
            for rc in range(2):
                qv = apv(q32, [[64, 20], [16, 2], [1, 16]], extra_off=rc * 32)
                ov = apv(q_, [[64, 20], [16, 2], [1, 16]], extra_off=rc * 32)
                cosb = apv(rp[:, ti, rc * 32:rc * 32 + 16], [[0, 20], [0, 2], [1, 16]])
                sinb = apv(rp[:, ti, rc * 32 + 16:rc * 32 + 32], [[0, 20], [0, 2], [1, 16]])
                tcv = tcs[tn % 2][rc].rearrange("p h (t j) -> p h t j", t=2)
                tsv = tss[tn % 2][rc].rearrange("p h (t j) -> p h t j", t=2)
                k.vop("tensor_tensor", tcv, qv, cosb, op=ALU.mult)
                k.vop("tensor_tensor", tsv, qv, sinb, op=ALU.mult)
                k.vop("tensor_tensor", ov[:, :, 0, :], tcv[:, :, 0, :], tsv[:, :, 1, :], op=ALU.subtract)
                k.vop("tensor_tensor", ov[:, :, 1, :], tcv[:, :, 1, :], tsv[:, :, 0, :], op=ALU.add)
        else:
            k.vop("tensor_tensor", q_[:, h0:h0 + nh, :], qh, gq[:, h0:h0 + nh, :], op=ALU.mult)
        yield
        kbank = k.banks[3].bitcast(BF16)
        for m in range(2):
            k.transpose(kbank[:, m * 128:(m + 1) * 128], q_[:, 16 + 2 * m:18 + 2 * m, :].rearrange("p a b -> p (a b)"), ident)
        k.act(kT[:, :, kt * 128:(kt + 1) * 128], kbank[:, 0:256].rearrange("p (a b) -> p a b", a=2), AF.Copy)
        if isx:
            qbank = k.banks[6 + tn % 2].bitcast(BF16)
            qf = q_.rearrange("p a b -> p (a b)")
            for s_i in range(8):
                k.transpose(qbank[:, s_i * 128:(s_i + 1) * 128], qf[:, s_i * 128:(s_i + 1) * 128], ident)
            qs_ = qst[(ti // 4) % 2]
            k.vop("tensor_copy", qs_[:, :, (ti % 4) * 128:(ti % 4 + 1) * 128],
                  qbank.rearrange("p (a b) -> p a b", a=8))
            if ti % 4 == 3:
                t0 = (ti // 4) * 512
                k.dma("gpsimd", dr["QT"][:, :, t0:t0 + 512].rearrange("s p t -> p s t"), qs_)


    run_interleaved((tile_gen(kind, ti, n) for n, (kind, ti) in enumerate(tiles)), width=2)


def bank2(k, i):
    return V(k.psum[:, i:i + 2, :].rearrange("p a b -> p (a b)"), k.banks[i].bufs + k.banks[i + 1].bufs)


def phase_attn_core(k, dr):
    k.reset(keep=ATT["keep"])
    kT, Vaug = ATT["kT"], ATT["Vaug"]
    wv = dr["attn_o_w"][0].rearrange("(k p) n -> p k n", p=128)
    ow = []
    for kk in range(8):
        w = k.alloc(f"aow{kk}", [128, D], BF16)
        k.dma("gpsimd", w, wv[:, kk, :], max_dma_last_dim=4096)
        ow.append(w)
    G = k.alloc("G", [128, D], F32)
    load_bc(k, "sync", G, dr["mods_d"][0, 1, 2 * D:3 * D])
    qz = [k.alloc("qz", [128, 16, 512], BF16) for _ in range(2)]
    for q_ in qz:
        k.vop("memset", q_, 0.0)
    aT = [k.alloc("aT", [128, 8, 512], BF16) for _ in range(2)]
    NP = 4
    pT = [k.alloc("pT", [128, 1024], BF16) for _ in range(NP)]
    rl = [k.alloc("rl", [128, 512], F32) for _ in range(2)]
    rl0 = [k.alloc("rl0", [128, 512], F32) for _ in range(2)]
    osb = [k.alloc("osb", [128, 512], F32) for _ in range(2)]
    xt = [k.alloc("xt", [128, D], F32) for _ in range(3)]
    tmp = k.alloc("tmp", [128, D], F32)
    sb2 = [bank2(k, 2), bank2(k, 4), bank2(k, 6)]
    LOOK = 2
    nu = 0
    nr = 0
    tn = 0
    pending = []
    tnc = {"n": 0}

    def flush_oproj():
        while pending:
            qb_, a_o = pending.pop(0)
            for j in range(4):
                ti = qb_ * 4 + j
                x_ = xt[tnc["n"] % 3]
                tnc["n"] += 1
                rows = slice(ti * 128, (ti + 1) * 128)
                k.dma("sync", x_, dr["XR2"][rows, :])
                for half in range(2):
                    cs = slice(half * 512, (half + 1) * 512)
                    ps = k.banks[2 + half]
                    for c in range(8):
                        k.mm(ps, a_o[:, c, j * 128:(j + 1) * 128], ow[c][:, cs], c == 0, c == 7)
                    k.vop("tensor_tensor", tmp[:, cs], ps, G[:, cs], op=ALU.mult)
                    k.vop("tensor_tensor", x_[:, cs], x_[:, cs], tmp[:, cs], op=ALU.add)
                k.dma("gpsimd", dr["XR3"][rows, :], x_)

    for qb in range(T // 512):
        q_ = qz[qb % 2]
        a_ = aT[qb % 2]
        qsl = slice(qb * 512, (qb + 1) * 512)
        for G_ in range(2):
            k.dma("sync", q_[0:64, 8 * G_:8 * G_ + 4, :],
                  dr["QT"][4 * G_:4 * G_ + 4, 0:64, qsl].rearrange("s p t -> p s t"))
            k.dma("sync", q_[64:128, 8 * G_ + 4:8 * G_ + 8, :],
                  dr["QT"][4 * G_:4 * G_ + 4, 64:128, qsl].rearrange("s p t -> p s t"))
        for g in range(4):
            for pr in range(2):
                if g == 0 and pr == 1:
                    flush_oproj()
                heads = [4 * g + 2 * pr, 4 * g + 2 * pr + 1]
                sbk = {}

                def emit_S(n):
                    bank = sb2[(nu + n) % 3]
                    sbk[n] = bank
                    for i, h in enumerate(heads):
                        k.mm(bank[:, i * 512:(i + 1) * 512], kT[:, g // 2, n * 128:(n + 1) * 128],
                             q_[:, h, :], True, True)

                def emit_PV(n):
                    p_ = pT[(nu + n) % NP]
                    k.act(p_, sbk[n], AF.Exp, scale=0.125)
                    for i in range(2):
                        k.mm(k.banks[i], Vaug[:, n * 4 + g, :], p_[:, i * 512:(i + 1) * 512],
                             n == 0, n == NKT - 1)

                for n in range(LOOK):
                    emit_S(n)
                for n in range(NKT):
                    if n + LOOK < NKT:
                        emit_S(n + LOOK)
                    emit_PV(n)
                nu += NKT
                for i, h in enumerate(heads):
                    k.vop("tensor_copy", osb[i], k.banks[i])
                for i, h in enumerate(heads):
                    r_ = rl[nr % 2]
                    r0 = rl0[nr % 2]
                    nr += 1
                    ov_ = osb[i]
                    k.vop("reciprocal", r_[64:128, :], ov_[64:128, :])
                    k.vop("tensor_copy", r0[0:64, :], r_[64:128, :])
                    dp = (h % 2) * 64
                    k.vop("tensor_tensor", a_[dp:dp + 64, h // 2, :], ov_[0:64, :], r0[0:64, :], op=ALU.mult)
        pending.append((qb, a_))
    flush_oproj()


PHASES = [("adaln", phase_adaln), ("lru_in", phase_lru_in), ("lru_scan", phase_lru_scan),
          ("lru_out", phase_lru_out), ("moe0", phase_moe0),
          ("attn_proj", phase_attn_proj), ("attn_core", phase_attn_core), ("moe1", phase_moe1)]


IN_SPECS = {
    "x": [T, D], "c": [1, D], "ctx": [C, D], "c_ctx": [D],
    "ada_w": [2, D, 6 * D], "ada_b": [2, 6 * D], "norm_mix_w": [2, D], "norm_ffn_w": [2, D],
    "lru_in_w": [1, D, 2 * D], "lru_conv_w": [1, 4, D], "lru_conv_b": [1, D],
    "lru_gate_a_w": [1, 2, 8, 128, 128], "lru_gate_a_b": [1, 2, D],
    "lru_gate_x_w": [1, 2, 8, 128, 128], "lru_gate_x_b": [1, 2, D],
    "lru_lambda": [1, 2, D], "lru_out_w": [1, D, D],
    "attn_qkv_w": [1, D, 1536], "attn_q_norm_w": [1, 64], "attn_k_norm_w": [1, 64],
    "attn_o_w": [1, D, D], "router_w": [D, NE], "router_b": [NE],
    "moe_w1": [2, NE, D, FF], "moe_w3": [2, NE, D, FF], "moe_w2": [2, NE, FF, D],
    "ident": [128, 128], "rope": [T, 64], "ltri": [128, 128], "idxA": [128, 32], "idxB": [128, 16],
}
SCRATCH_SPECS = {
    "mods_d": ([2, 2, 6 * D], F32),
    "GG": ([8, 128, TS], BF16), "UU": ([8, 128, TS], F32), "ZZ": ([8, 128, TS], BF16),
    "XR1": ([T, D], F32), "CR1": ([C, D], F32), "XR2": ([T, D], F32), "CR2": ([C, D], F32),
    "XR3": ([T, D], F32), "QT": ([8, 128, T], BF16),
    "Hs": ([NSLOT, D], BF16), "Ys": ([NSLOT, D], F32), "Gs": ([NSLOT, 16], F32),
}


class DR(dict):
    def __init__(self, nc, dbg):
        super().__init__()
        self.nc = nc
        self.dbg = dbg
        self.used_inputs = []

    def __missing__(self, name):
        if not self.dbg:
            if name in ("XR1", "XR2", "XR3"):
                return self["out"]
            if name == "CR2":
                return self["CR1"]
            if name == "ZZ":
                return self["GG"]
            if name == "QT":
                return self["GG"][:, :, 0:T]
        if name in IN_SPECS:
            ap = self.nc.dram_tensor(name, list(IN_SPECS[name]), F32, kind="ExternalInput").ap()
            self.used_inputs.append(name)
        else:
            shape, dt = SCRATCH_SPECS[name]
            kind = "ExternalOutput" if self.dbg else "Internal"
            ap = self.nc.dram_tensor(name, list(shape), dt, kind=kind).ap()
        self[name] = ap
        return ap


def build(stop_after=None, dbg=False):
    nc = bass.Bass("TRN2", target_bir_lowering=False)
    dr = DR(nc, dbg)
    dr["out"] = nc.dram_tensor("out", [T, D], F32, kind="ExternalOutput").ap()
    with ExitStack() as stack:
        arena_bytes = 204 * 1024
        arena_t = stack.enter_context(nc.sbuf_tensor("arena", [128, arena_bytes], U8))
        pt = stack.enter_context(nc.psum_tensor("psum", [128, 8, 512], F32))
        banks = [V(pt[:, i, :], (Buf(f"bank{i}", excl=True),)) for i in range(8)]
        P = Prog(nc)
        k = K(nc, P, arena_t[:], arena_bytes, banks)
        k.psum = pt
        import os
        only = os.environ.get("PHASE_ONLY", "")
        for name, fn in PHASES:
            if only and name not in only.split(","):
                continue
            fn(k, dr)
            if stop_after == name:
                break
        P.barrier()
        P.add("sync", lambda h: h.nop(), dma=False)
        P.emit(stack)
    nc._used_inputs = list(dr.used_inputs)
    return nc


def make_in_maps(inputs, used=None):
    f = lambda a: np.ascontiguousarray(np.asarray(a, dtype=np.float32))
    shared = {n: f(inputs[n]) for n in (
        "c_ctx", "ada_w", "ada_b", "norm_mix_w", "norm_ffn_w", "lru_in_w", "lru_conv_w", "lru_conv_b",
        "lru_gate_a_w", "lru_gate_a_b", "lru_gate_x_w", "lru_gate_x_b", "lru_lambda", "lru_out_w",
        "attn_qkv_w", "attn_q_norm_w", "attn_k_norm_w", "attn_o_w", "router_w", "router_b",
        "moe_w1", "moe_w3", "moe_w2")}
    shared["ident"] = np.eye(128, dtype=np.float32)
    shared["ltri"] = np.triu(np.ones((128, 128), dtype=np.float32), k=1)
    p_ = np.arange(128, dtype=np.float32)[:, None, None]
    ia = np.zeros((128, 32), dtype=np.float32)
    ia[:, 0:4] = np.arange(4, dtype=np.float32)[None, :] * 128 + np.arange(128, dtype=np.float32)[:, None]
    shared["idxA"] = ia
    shared["idxB"] = np.ascontiguousarray((np.arange(4, dtype=np.float32)[None, :, None] * 512
                                           + np.arange(4, dtype=np.float32)[None, None, :] * 128 + p_).reshape(128, 16))
    inv = (np.float32(10000.0) ** (-np.arange(16, dtype=np.float32) / np.float32(16))).astype(np.float32)
    t = np.arange(T)
    pr = (t // 64).astype(np.float32)[:, None] * inv
    pc = (t % 64).astype(np.float32)[:, None] * inv
    rope = np.concatenate([np.cos(pr), np.sin(pr), np.cos(pc), np.sin(pc)], axis=1).astype(np.float32)
    shared["rope"] = np.ascontiguousarray(rope)
    x = f(inputs["x"]); c = f(inputs["c"]); ctx = f(inputs["ctx"])
    maps = []
    for b in range(8):
        m = dict(shared)
        m["x"] = x[b]; m["c"] = c[b:b + 1]; m["ctx"] = ctx[b]
        if used is not None:
            m = {n: v for n, v in m.items() if n in used}
        maps.append(m)
    return maps


def kernel(**inputs):
    nc = build()
    res = run_bass_kernel_spmd(nc, make_in_maps(inputs, nc._used_inputs), core_ids=list(range(8)))
    return np.stack([r["out"] for r in res.results], axis=0).astype(np.float32)
```

```python
import numpy as np
from contextlib import ExitStack
import concourse.bass as bass
import concourse.mybir as mybir
from concourse.bass_utils import run_bass_kernel_spmd

F32 = mybir.dt.float32
BF16 = mybir.dt.bfloat16
U8 = mybir.dt.uint8
AF = mybir.ActivationFunctionType
ALU = mybir.AluOpType
AX = mybir.AxisListType

D = 1024
T = 4096
C = 256
TS = T + C
NXT = T // 128
NCT = C // 128
NE = 16
FF = 512
EPS = 1e-6
ENGS = ("sync", "gpsimd", "scalar", "vector", "tensor")
NRING = {"sync": 28, "gpsimd": 24, "scalar": 8}


class Buf:
    __slots__ = ("name", "w", "r", "excl")

    def __init__(self, name="", excl=False):
        self.name = name
        self.w = None
        self.r = {}
        self.excl = excl


class Op:
    __slots__ = ("eng", "fn", "deps", "needed", "sig", "dma", "ring", "val", "prev")


class V:
    __slots__ = ("ap", "bufs")

    def __init__(self, ap, bufs):
        self.ap = ap
        self.bufs = tuple(bufs)

    def __getitem__(self, k):
        return V(self.ap[k], self.bufs)

    def rearrange(self, s, **kw):
        return V(self.ap.rearrange(s, **kw), self.bufs)

    def bitcast(self, dt):
        return V(self.ap.bitcast(dt), self.bufs)

    def with_bufs(self, *bufs):
        return V(self.ap, bufs)

    @property
    def shape(self):
        return self.ap.shape


def _ap(x):
    return x.ap if isinstance(x, V) else x


class Prog:
    def __init__(self, nc):
        self.nc = nc
        self.ops = {e: [] for e in ENGS}
        self.last_compute = {e: None for e in ENGS}
        self.dma_since = []
        self.pending = {e: None for e in ENGS}
        self.ring_pos = {e: 0 for e in ENGS}
        self.ring_last = {}
        self.ring_cnt = {}

    def add(self, eng, fn, reads=(), writes=(), dma=False):
        op = Op()
        op.eng = eng
        op.fn = fn
        op.dma = dma
        op.needed = False
        op.sig = 0
        op.deps = {}
        op.prev = None
        for b in reads:
            if b.w is not None:
                op.deps[b.w] = "raw"
            if b.excl:
                for key, o in b.r.items():
                    if key != eng:
                        op.deps.setdefault(o, "raw")
        for b in writes:
            if b.w is not None:
                op.deps.setdefault(b.w, "waw")
            for o in b.r.values():
                if o is not op:
                    op.deps.setdefault(o, "war")
        pend = self.pending[eng]
        if pend:
            for o in pend:
                op.deps[o] = "raw"
            self.pending[eng] = None
        for b in writes:
            b.w = op
            b.r = {}
        for b in reads:
            b.r[("dma", id(op)) if dma else eng] = op
        if dma:
            n = NRING[eng]
            key = (eng, self.ring_pos[eng] % n)
            self.ring_pos[eng] += 1
            op.ring = key
            self.ring_cnt[key] = self.ring_cnt.get(key, 0) + 1
            op.val = 16 * self.ring_cnt[key]
            op.prev = self.ring_last.get(key)
            self.ring_last[key] = op
            self.dma_since.append(op)
        else:
            self.last_compute[eng] = op
        self.ops[eng].append(op)
        return op

    def barrier(self):
        deps = [o for o in self.last_compute.values() if o is not None]
        latest = {}
        for o in self.dma_since:
            latest[o.ring] = o
        deps += list(latest.values())
        self.dma_since = list(latest.values())
        for e in ENGS:
            cur = self.pending[e] or []
            self.pending[e] = list(cur) + deps

    @staticmethod
    def _keep(op, d, kind):
        if d.dma:
            return True
        if d.eng != op.eng:
            return True
        if op.dma:
            return True
        if op.eng == "tensor":
            return False
        return True

    def emit(self, stack):
        nc = self.nc
        for e in ENGS:
            for op in self.ops[e]:
                for d, kind in op.deps.items():
                    if (not d.dma) and self._keep(op, d, kind):
                        d.needed = True
        for e in ENGS:
            cnt = 0
            for op in self.ops[e]:
                if (not op.dma) and op.needed:
                    cnt += 1
                    op.sig = cnt
        esem = {e: stack.enter_context(nc.semaphore("es_" + e)) for e in ENGS if e != "sync"}
        rsem = {}
        for e, n in NRING.items():
            for i in range(n):
                rsem[(e, i)] = stack.enter_context(nc.semaphore(f"rs_{e}_{i}"))
        block = stack.enter_context(nc.Block())
        prog = self

        def run_engine(e, h):
            waited = {}

            def wait(sem_key, sem, val):
                if waited.get(sem_key, 0) < val:
                    h.wait_ge(sem, val)
                    waited[sem_key] = val

            for op in prog.ops[e]:
                for d, kind in op.deps.items():
                    if not prog._keep(op, d, kind):
                        continue
                    if d.dma:
                        wait(d.ring, rsem[d.ring], d.val)
                    else:
                        wait(d.eng, esem[d.eng], d.sig)
                if op.dma and op.prev is not None:
                    wait(op.ring, rsem[op.ring], op.prev.val)
                ins = op.fn(h)
                if op.dma:
                    ins.then_inc(rsem[op.ring], 16)
                elif op.needed:
                    ins.then_inc(esem[e], 1)

        @block.sync
        def _(h):
            run_engine("sync", h)

        @block.gpsimd
        def _(h):
            run_engine("gpsimd", h)

        @block.scalar
        def _(h):
            run_engine("scalar", h)

        @block.vector
        def _(h):
            run_engine("vector", h)

        @block.tensor
        def _(h):
            run_engine("tensor", h)


def _bufs(*xs):
    out = []
    for x in xs:
        if isinstance(x, V):
            out.extend(x.bufs)
    return out


class K:
    def __init__(self, nc, P, arena, arena_bytes, banks):
        self.nc = nc
        self.P = P
        self.arena = arena
        self.arena_bytes = arena_bytes
        self.off = 0
        self.banks = banks
        self.uid = 0

    def reset(self, keep=0):
        self.P.barrier()
        self.off = keep

    def alloc(self, name, shape, dtype, npart=128):
        esz = {F32: 4, BF16: 2, U8: 1, mybir.dt.uint32: 4, mybir.dt.int32: 4}[dtype]
        n = 1
        for s in shape[1:]:
            n *= s
        nbytes = (n * esz + 63) // 64 * 64
        assert self.off + nbytes <= self.arena_bytes, (name, self.off, nbytes, self.arena_bytes)
        ap = self.arena[0:shape[0], self.off:self.off + n * esz].bitcast(dtype)
        self.off += nbytes
        if len(shape) == 3:
            ap = ap.rearrange("p (a b) -> p a b", a=shape[1])
        elif len(shape) == 4:
            ap = ap.rearrange("p (a b c) -> p a b c", a=shape[1], b=shape[2])
        self.uid += 1
        return V(ap, (Buf(f"{name}_{self.uid}"),))

    def dma(self, q, out, in_, **kw):
        o, i = _ap(out), _ap(in_)
        return self.P.add(q, lambda h: h.dma_start(out=o, in_=i, **kw),
                          reads=_bufs(in_), writes=_bufs(out), dma=True)

    def mm(self, out, lhsT, rhs, start, stop, extra_reads=()):
        o, l, r = _ap(out), _ap(lhsT), _ap(rhs)
        return self.P.add("tensor", lambda h: h.matmul(o, l, r, start=start, stop=stop),
                          reads=_bufs(lhsT, rhs) + list(extra_reads), writes=_bufs(out))

    def transpose(self, out, in_, ident):
        o, i, d = _ap(out), _ap(in_), _ap(ident)
        return self.P.add("tensor", lambda h: h.transpose(o, i, d),
                          reads=_bufs(in_, ident), writes=_bufs(out))

    def act(self, out, in_, func, bias=None, scale=1.0, accum_out=None, eng="scalar"):
        o, i = _ap(out), _ap(in_)
        kw = {}
        if bias is not None:
            kw["bias"] = _ap(bias)
        if accum_out is not None:
            kw["accum_out"] = _ap(accum_out)
        sc = _ap(scale)
        return self.P.add(eng, lambda h: h.activation(out=o, in_=i, func=func, scale=sc, **kw),
                          reads=_bufs(in_, bias, scale), writes=_bufs(out, accum_out))

    def vop(self, name, out, *ins, eng="vector", accum_out=None, **kw):
        o = _ap(out)
        args = [_ap(x) for x in ins]
        kws = {k: _ap(v) for k, v in kw.items()}
        if accum_out is not None:
            kws["accum_out"] = _ap(accum_out)
        return self.P.add(eng, lambda h: getattr(h, name)(o, *args, **kws),
                          reads=_bufs(*ins, *kw.values()), writes=_bufs(out, accum_out))


def run_interleaved(gens, width=2):
    it = iter(gens)
    active = []
    done = False
    while True:
        while not done and len(active) < width:
            g = next(it, None)
            if g is None:
                done = True
                break
            active.append(g)
        if not active:
            break
        for g in list(active):
            try:
                next(g)
            except StopIteration:
                active.remove(g)


def load_bc(k, q, dst, src_ap):
    return k.dma(q, dst, src_ap.partition_broadcast(128))


def load_mod_tiles(k, dr, layer, src, which, norm_w_ap, need_gate=True):
    base = 0 if which == "m" else 3
    mods = dr["mods_d"]
    A = k.alloc("A", [128, D], F32)
    B = k.alloc("B", [128, D], F32)
    tmp = k.alloc("gn", [128, D], F32)
    load_bc(k, "sync", B, mods[src, layer, (base + 0) * D:(base + 1) * D])
    load_bc(k, "sync", A, mods[src, layer, (base + 1) * D:(base + 2) * D])
    load_bc(k, "sync", tmp, norm_w_ap)
    k.vop("scalar_tensor_tensor", A, A, 1.0, tmp, op0=ALU.add, op1=ALU.mult)
    G = None
    if need_gate:
        G = k.alloc("G", [128, D], F32)
        load_bc(k, "sync", G, mods[src, layer, (base + 2) * D:(base + 3) * D])
    return A, B, G


def norm_mod(k, xt, A, B, h_out, junk, st, t32, ss_eng="vector"):
    ss, ms, sq, rs = st[:, 0:1], st[:, 1:2], st[:, 2:3], st[:, 3:4]
    if ss_eng == "vector":
        k.vop("scalar_tensor_tensor", junk, xt, 1.0, xt, op0=ALU.mult, op1=ALU.mult, accum_out=ss)
    else:
        k.act(junk, xt, AF.Square, accum_out=ss)
    k.vop("tensor_scalar", ms, ss, 1.0 / D, EPS, op0=ALU.mult, op1=ALU.add)
    k.act(sq, ms, AF.Sqrt)
    k.vop("reciprocal", rs, sq)
    k.vop("scalar_tensor_tensor", t32, xt, rs, A, op0=ALU.mult, op1=ALU.mult)
    k.vop("tensor_tensor", h_out, t32, B, op=ALU.add)


def norm_mod_g(k, xt, A, B, h_out, junk, st, t32, ss_eng="vector"):
    ss, ms, sq, rs = st[:, 0:1], st[:, 1:2], st[:, 2:3], st[:, 3:4]
    if ss_eng == "vector":
        k.vop("scalar_tensor_tensor", junk, xt, 1.0, xt, op0=ALU.mult, op1=ALU.mult, accum_out=ss)
    else:
        k.act(junk, xt, AF.Square, accum_out=ss)
    yield
    k.vop("tensor_scalar", ms, ss, 1.0 / D, EPS, op0=ALU.mult, op1=ALU.add)
    k.act(sq, ms, AF.Sqrt)
    yield
    k.vop("reciprocal", rs, sq)
    k.vop("scalar_tensor_tensor", t32, xt, rs, A, op0=ALU.mult, op1=ALU.mult)
    k.vop("tensor_tensor", h_out, t32, B, op=ALU.add)
    yield


def transpose_tile(k, h, dst, bank_bf, ident, evac_eng="scalar"):
    for c in range(8):
        k.transpose(bank_bf[:, c * 128:(c + 1) * 128], h[:, c * 128:(c + 1) * 128], ident)
    src = bank_bf.rearrange("p (a b) -> p a b", a=8)
    if evac_eng == "scalar":
        k.act(dst, src, AF.Copy)
    else:
        k.vop("tensor_copy", dst, src)


def phase_adaln(k, dr):
    k.reset()
    cT = k.alloc("cT", [128, 2, 8], F32)
    sT = k.alloc("sT", [128, 2, 8], F32)
    wt = [k.alloc(f"adaw{i}", [128, 8, 512], F32) for i in range(3)]
    bias = k.alloc("adab", [2, 2, 6 * D], F32)
    mods = k.alloc("mods", [2, 2, 6 * D], F32)
    k.dma("sync", cT[:, 0, :], dr["c"][0].rearrange("(p k) -> p k", k=8))
    k.dma("sync", cT[:, 1, :], dr["c_ctx"].rearrange("(p k) -> p k", k=8))
    for j in range(2):
        k.dma("sync", bias[j:j + 1, :, :], dr["ada_b"].rearrange("(o l) n -> o l n", o=1))
    k.act(sT, cT, AF.Silu)
    n = 0
    for layer in range(2):
        wv = dr["ada_w"][layer].rearrange("(p k) n -> p k n", k=8)
        for nchunk in range(12):
            w = wt[n % 3]
            k.dma("sync" if n % 2 == 0 else "scalar", w, wv[:, :, nchunk * 512:(nchunk + 1) * 512])
            ps = k.banks[n % 2]
            for kk in range(8):
                k.mm(ps[0:2, :], sT[:, :, kk], w[:, kk, :], kk == 0, kk == 7)
            sl = slice(nchunk * 512, (nchunk + 1) * 512)
            k.vop("tensor_tensor", mods[0:2, layer, sl], ps[0:2, :], bias[0:2, layer, sl], op=ALU.add)
            n += 1
    k.dma("sync", dr["mods_d"], mods)


GELU = AF.Gelu_apprx_tanh
LRU_BLOCKS = [("ctx", 0, 2, 0)] + [("x", 4 * i, 4, C + 512 * i) for i in range(8)]


def slow_dma(k, q, out, in_):
    return k.dma(q, out, in_, allow_slow_non_contiguous=True)


def phase_lru_in(k, dr):
    k.reset()
    ident = k.alloc("ident", [128, 128], BF16)
    k.dma("gpsimd", ident, dr["ident"])
    wv = dr["lru_in_w"][0].rearrange("(k p) n -> p k n", p=128)
    in_w = []
    for kk in range(8):
        w = k.alloc(f"in_w{kk}", [128, 2048], BF16)
        k.dma("gpsimd", w, wv[:, kk, :], max_dma_last_dim=4096)
        in_w.append(w)
    Ax, Bx, _ = load_mod_tiles(k, dr, 0, 0, "m", dr["norm_mix_w"][0], need_gate=False)
    Ac, Bc, _ = load_mod_tiles(k, dr, 0, 1, "m", dr["norm_mix_w"][0], need_gate=False)
    xt = [k.alloc("xt", [128, D], F32) for _ in range(3)]
    junk = k.alloc("junk", [128, D], BF16)
    t32s = [k.alloc("t32", [128, D], F32) for _ in range(2)]
    hb = [[k.alloc("hb", [128, D], BF16) for _ in range(4)] for _ in range(2)]
    st = [k.alloc("st", [128, 4], F32) for _ in range(4)]
    hT = [k.alloc("hT", [128, 8, 512], BF16) for _ in range(2)]
    gg = [k.alloc("gg", [128, 8, 512], BF16) for _ in range(2)]
    uu = [k.alloc("uu", [128, 8, 512], F32) for _ in range(2)]
    bankbf = [k.banks[6].bitcast(BF16), k.banks[7].bitcast(BF16)]
    cnt = {"tn": 0, "tp": 0}

    def norms(bi):
        src, t0, nt, tok0 = LRU_BLOCKS[bi]
        srcap = dr["ctx"] if src == "ctx" else dr["x"]
        A_, B_ = (Ac, Bc) if src == "ctx" else (Ax, Bx)
        for j in range(nt):
            tn = cnt["tn"]
            cnt["tn"] += 1
            x_ = xt[tn % 3]
            k.dma("sync", x_, srcap[(t0 + j) * 128:(t0 + j + 1) * 128, :])
            norm_mod(k, x_, A_, B_, hb[bi % 2][j], junk, st[tn % 4], t32s[tn % 2], ss_eng="scalar")

    def transposes(bi):
        src, t0, nt, tok0 = LRU_BLOCKS[bi]
        for j in range(nt):
            tp = cnt["tp"]
            cnt["tp"] += 1
            transpose_tile(k, hb[bi % 2][j], hT[bi % 2][:, :, j * 128:(j + 1) * 128], bankbf[tp % 2], ident)

    norms(0)
    transposes(0)
    for bi, (src, t0, nt, tok0) in enumerate(LRU_BLOCKS):
        hTb = hT[bi % 2]
        if bi + 1 < len(LRU_BLOCKS):
            norms(bi + 1)
        ntok = nt * 128
        for oc in range(16):
            ps = k.banks[oc % 6]
            for kk in range(8):
                k.mm(ps[:, :ntok], in_w[kk][:, oc * 128:(oc + 1) * 128], hTb[:, kk, :ntok], kk == 0, kk == 7)
            if oc < 8:
                k.act(gg[bi % 2][:, oc, :ntok], ps[:, :ntok], GELU)
            else:
                k.vop("tensor_copy", uu[bi % 2][:, oc - 8, :ntok], ps[:, :ntok])
        if bi + 1 < len(LRU_BLOCKS):
            transposes(bi + 1)
        k.dma("gpsimd", dr["GG"][:, :, tok0:tok0 + ntok].rearrange("c p t -> p c t"), gg[bi % 2][:, :, :ntok])
        k.dma("gpsimd", dr["UU"][:, :, tok0:tok0 + ntok].rearrange("c p t -> p c t"), uu[bi % 2][:, :, :ntok])


def phase_lru_scan(k, dr):
    k.reset()
    cw = k.alloc("cw", [128, 4, 8], F32)
    cb = k.alloc("cb", [128, 8], F32)
    gab = k.alloc("gab", [128, 2, 8], F32)
    gxb = k.alloc("gxb", [128, 2, 8], F32)
    lam = k.alloc("lam", [128, 2, 8], F32)
    cp = k.alloc("cp", [128, 2, 8], F32)
    for tap in range(4):
        slow_dma(k, "sync", cw[:, tap, :], dr["lru_conv_w"][0, tap].rearrange("(c p) -> p c", p=128))
    slow_dma(k, "sync", cb, dr["lru_conv_b"][0].rearrange("(c p) -> p c", p=128))
    for d in range(2):
        slow_dma(k, "sync", gab[:, d, :], dr["lru_gate_a_b"][0, d].rearrange("(c p) -> p c", p=128))
        slow_dma(k, "sync", gxb[:, d, :], dr["lru_gate_x_b"][0, d].rearrange("(c p) -> p c", p=128))
        slow_dma(k, "sync", lam[:, d, :], dr["lru_lambda"][0, d].rearrange("(c p) -> p c", p=128))
    k.act(cp, lam, AF.Exp, scale=-1.0)
    k.act(cp, cp, AF.Ln, bias=1.0)
    k.vop("tensor_scalar", cp, cp, -8.0, None, op0=ALU.mult)
    gw = k.alloc("gw", [128, 32, 128], BF16)
    k.dma("gpsimd", gw[:, 0:16, :], dr["lru_gate_a_w"][0].rearrange("d n i e -> i (d n) e"))
    k.dma("gpsimd", gw[:, 16:32, :], dr["lru_gate_x_w"][0].rearrange("d n i e -> i (d n) e"))
    XB = 259
    up = [k.alloc("up", [128, TS + 6], F32) for _ in range(1)]
    for u_ in up:
        k.vop("memset", u_, 0.0)
    uconv = k.alloc("uconv", [128, TS], F32)
    ub = k.alloc("ub", [128, TS], BF16)
    rrs = [k.alloc("rr", [128, TS], F32) for _ in range(2)]
    igs = [k.alloc("ig", [128, TS], F32) for _ in range(2)]
    sbs = [k.alloc("sb", [128, TS], F32) for _ in range(2)]
    y = k.alloc("y", [128, TS], F32)
    ggc = [k.alloc("ggc", [128, TS], BF16) for _ in range(1)]
    z = [k.alloc("z", [128, TS], BF16) for _ in range(1)]
    pieces = [(0, 256)] + [(C + 512 * j, 512) for j in range(8)]
    segs = [(2, 0, C), (XB + 2, C, T)]
    nb = 0
    for c in range(8):
        u_ = up[0]
        k.dma("sync", u_[:, 2:2 + C], dr["UU"][c, :, 0:C])
        k.dma("sync", u_[:, XB + 2:XB + 2 + T], dr["UU"][c, :, C:TS])
        k.dma("sync", ggc[0], dr["GG"][c])
        for (sbase, dbase, L) in segs:
            dst = uconv[:, dbase:dbase + L]
            k.vop("tensor_scalar", dst, u_[:, sbase - 2:sbase - 2 + L], cw[:, 0, c:c + 1], cb[:, c:c + 1],
                  op0=ALU.mult, op1=ALU.add)
            for tap in range(1, 4):
                k.vop("scalar_tensor_tensor", dst, u_[:, sbase - 2 + tap:sbase - 2 + tap + L],
                      cw[:, tap, c:c + 1], dst, op0=ALU.mult, op1=ALU.add)
        k.act(ub, uconv, AF.Copy)
        def dir_gen(d, c=c):
            nonlocal nb
            rr, ig, sb = rrs[d], igs[d], sbs[d]
            for pi, (p0, pl) in enumerate(pieces):
                psr = k.banks[nb % 6]
                psi = k.banks[(nb + 1) % 6]
                nb += 2
                k.mm(psr[:, :pl], gw[:, d * 8 + c, :], ub[:, p0:p0 + pl], True, True)
                k.mm(psi[:, :pl], gw[:, 16 + d * 8 + c, :], ub[:, p0:p0 + pl], True, True)
                k.act(rr[:, p0:p0 + pl], psr[:, :pl], AF.Sigmoid, bias=gab[:, d, c:c + 1])
                k.act(ig[:, p0:p0 + pl], psi[:, :pl], AF.Sigmoid, bias=gxb[:, d, c:c + 1])
                if pi % 3 == 2:
                    yield
            yield
            k.act(rr, rr, AF.Exp, scale=cp[:, d, c:c + 1])
            k.vop("tensor_tensor", ig, ig, uconv, op=ALU.mult, eng="gpsimd")
            yield
            k.act(sb, rr, AF.Square)
            yield
            k.act(sb, sb, AF.Sqrt, bias=1.0, scale=-1.0)
            yield
            k.vop("tensor_tensor", ig, ig, sb, op=ALU.mult)
            yield
            hd = y if d == 0 else sb
            if d == 0:
                k.vop("tensor_tensor_scan", hd[:, 0:C], rr[:, 0:C], ig[:, 0:C], 0.0, op0=ALU.mult, op1=ALU.add)
                yield
                k.vop("tensor_tensor_scan", hd[:, C:TS], rr[:, C:TS], ig[:, C:TS], hd[:, C - 1:C],
                      op0=ALU.mult, op1=ALU.add)
            else:
                k.vop("tensor_tensor_scan", hd[:, 0:C][:, ::-1], rr[:, 0:C][:, ::-1], ig[:, 0:C][:, ::-1], 0.0,
                      op0=ALU.mult, op1=ALU.add)
                yield
                k.vop("tensor_tensor_scan", hd[:, C:TS][:, ::-1], rr[:, C:TS][:, ::-1], ig[:, C:TS][:, ::-1],
                      hd[:, 0:1], op0=ALU.mult, op1=ALU.add)

        run_interleaved([dir_gen(0), dir_gen(1)], width=2)
        k.vop("tensor_tensor", y, y, sbs[1], op=ALU.add)
        k.vop("tensor_tensor", z[0], y, ggc[0], op=ALU.mult)
        k.dma("gpsimd", dr["ZZ"][c], z[0])


def phase_lru_out(k, dr):
    k.reset()
    wv = dr["lru_out_w"][0].rearrange("(k p) n -> p k n", p=128)
    ow = []
    for kk in range(8):
        w = k.alloc(f"ow{kk}", [128, D], BF16)
        k.dma("gpsimd", w, wv[:, kk, :], max_dma_last_dim=4096)
        ow.append(w)
    Gx = k.alloc("Gx", [128, D], F32)
    Gc = k.alloc("Gc", [128, D], F32)
    load_bc(k, "sync", Gx, dr["mods_d"][0, 0, 2 * D:3 * D])
    load_bc(k, "sync", Gc, dr["mods_d"][1, 0, 2 * D:3 * D])
    zb = [k.alloc("zb", [128, 8, 512], BF16) for _ in range(2)]
    xt = [k.alloc("xt", [128, D], F32) for _ in range(3)]
    tmp = k.alloc("tmp", [128, D], F32)
    tn = 0
    nb = 0
    for bi, (src, t0, nt, tok0) in enumerate(LRU_BLOCKS):
        ntok = nt * 128
        zt = zb[bi % 2]
        k.dma("sync", zt[:, :, :ntok], dr["ZZ"][:, :, tok0:tok0 + ntok].rearrange("c p t -> p c t"))
        srcap = dr["ctx"] if src == "ctx" else dr["x"]
        dstap = dr["CR1"] if src == "ctx" else dr["XR1"]
        G = Gc if src == "ctx" else Gx
        for j in range(nt):
            x_ = xt[tn % 3]
            tn += 1
            rows = slice((t0 + j) * 128, (t0 + j + 1) * 128)
            k.dma("sync", x_, srcap[rows, :])
            for half in range(2):
                cs = slice(half * 512, (half + 1) * 512)
                ps = k.banks[nb % 4]
                nb += 1
                for kk in range(8):
                    k.mm(ps, zt[:, kk, j * 128:(j + 1) * 128], ow[kk][:, cs], kk == 0, kk == 7)
                k.vop("tensor_tensor", tmp[:, cs], ps, G[:, cs], op=ALU.mult)
                k.vop("tensor_tensor", x_[:, cs], x_[:, cs], tmp[:, cs], op=ALU.add)
            k.dma("gpsimd", dstap[rows, :], x_)


def bcast_last(v, n):
    a = v.ap
    dims = [list(d) for d in a.ap]
    return V(bass.AP(a.tensor, a.offset, dims + [[0, n]]), v.bufs)


def phase_moe(k, dr, layer, srcs, dsts, blocks):
    import os
    dbg_mode = os.environ.get("MOE_DBG", "")
    if dbg_mode:
        blocks = blocks[:1]
    n_exp = {"": NE, "pro": 0, "pro0": 0, "e1": 1, "e2": 2}[dbg_mode]
    k.reset()
    ident = k.alloc("ident32", [128, 128], F32)
    k.dma("sync", ident, dr["ident"])
    rw = k.alloc("rw", [128, 8, NE], F32)
    k.dma("sync", rw, dr["router_w"].rearrange("(k p) e -> p k e", p=128))
    rb = k.alloc("rb", [128, NE], F32)
    load_bc(k, "sync", rb, dr["router_b"])
    mod = {}
    for kind, si in (("x", 0), ("ctx", 1)):
        if kind in srcs:
            mod[kind] = load_mod_tiles(k, dr, layer, si, "f", dr["norm_ffn_w"][layer])
    maxnt = max(len(b) for b in blocks)
    xt = [k.alloc("xt", [128, D], F32) for _ in range(2)]
    h32 = [k.alloc("h32", [128, D], F32) for _ in range(2)]
    junk = k.alloc("junk", [128, D], BF16)
    t32 = k.alloc("t32", [128, D], F32)
    st = [k.alloc("st", [128, 4], F32) for _ in range(2)]
    hT32 = [k.alloc("hT32", [128, 8, 128], F32) for _ in range(2)]
    hTb = k.alloc("hTb", [128, 8, maxnt * 128], BF16)
    gates = k.alloc("gates", [128, maxnt, NE], F32)
    rs = [k.alloc("rs", [128, 160], F32) for _ in range(2)]
    w1 = [k.alloc("w1", [128, 8, FF], BF16) for _ in range(2)]
    w3 = [k.alloc("w3", [128, 8, FF], BF16) for _ in range(2)]
    w2 = [k.alloc("w2", [128, 4, D], BF16) for _ in range(2)]
    ssb = [k.alloc("ssb", [128, 512], F32) for _ in range(2)]
    actT = [k.alloc("actT", [128, 4, maxnt * 128], BF16) for _ in range(2)]
    yacc = k.alloc("yacc", [128, maxnt, D], F32)
    tmp = k.alloc("tmp", [128, D], F32)
    tn = 0
    ne = 0
    nh = 0
    ny = 0
    for blk in blocks:
        nt = len(blk)
        ntok = nt * 128
        for j, (kind, ti) in enumerate(blk):
            x_ = xt[tn % 2]
            h_ = h32[tn % 2]
            r_ = rs[tn % 2]
            A_, B_, _ = mod[kind]
            rows = slice(ti * 128, (ti + 1) * 128)
            k.dma("sync", x_, srcs[kind][rows, :])
            norm_mod(k, x_, A_, B_, h_, junk, st[tn % 2], t32)
            for hb_ in range(2):
                bank = k.banks[6 + hb_]
                for c4 in range(4):
                    c = hb_ * 4 + c4
                    k.transpose(bank[:, c4 * 128:(c4 + 1) * 128], h_[:, c * 128:(c + 1) * 128], ident)
                src = bank.rearrange("p (a b) -> p a b", a=4)
                k.vop("tensor_copy", hT32[tn % 2][:, hb_ * 4:hb_ * 4 + 4, :], src)
            k.act(hTb[:, :, j * 128:(j + 1) * 128], hT32[tn % 2], AF.Copy)
            lps = k.banks[5]
            for kk in range(8):
                k.mm(lps[:, 0:NE], hT32[tn % 2][:, kk, :], rw[:, kk, :], kk == 0, kk == 7)
            if dbg_mode == "pro0":
                k.vop("tensor_copy", gates[:, j, :], lps[:, 0:NE])
                tn += 1
                continue
            lg = r_[:, 0:16]
            pg = r_[:, 16:32]
            p6 = r_[:, 32:56].rearrange("p (a b) -> p a b", a=4)
            msk = r_[:, 56:72]
            eq1 = r_[:, 72:88]
            sel = r_[:, 88:104]
            gsel = r_[:, 104:120]
            gs = r_[:, 120:124]
            oh = r_[:, 124:128]
            ohm = r_[:, 128:132]
            mx, nmx, gm, v1, v2, den, rden = (r_[:, 132 + i:133 + i] for i in range(7))
            pgv = pg.rearrange("p (a b) -> p a b", a=4)
            mskv = msk.rearrange("p (a b) -> p a b", a=4)
            k.vop("tensor_tensor", lg, lps[:, 0:NE], rb, op=ALU.add)
            k.vop("tensor_reduce", mx, lg, axis=AX.X, op=ALU.max)
            k.vop("tensor_scalar", nmx, mx, -1.0, None, op0=ALU.mult)
            k.act(pg, lg, AF.Exp, bias=nmx)
            k.vop("tensor_tensor", p6[:, :, 0:3], pgv[:, :, 0:3], pgv[:, :, 1:4], op=ALU.add)
            k.vop("tensor_tensor", p6[:, :, 3:5], pgv[:, :, 0:2], pgv[:, :, 2:4], op=ALU.add)
            k.vop("tensor_tensor", p6[:, :, 5:6], pgv[:, :, 0:1], pgv[:, :, 3:4], op=ALU.add)
            k.vop("tensor_reduce", gs, p6, axis=AX.X, op=ALU.max)
            k.vop("tensor_reduce", gm, gs, axis=AX.X, op=ALU.max)
            k.vop("tensor_scalar", oh, gs, gm, None, op0=ALU.is_equal)
            k.vop("tensor_scalar", ohm, oh, -1.0, None, op0=ALU.add)
            k.vop("tensor_tensor", mskv, pgv, bcast_last(oh, 4), op=ALU.mult)
            k.vop("tensor_tensor", mskv, mskv, bcast_last(ohm, 4), op=ALU.add)
            k.vop("tensor_reduce", v1, msk, axis=AX.X, op=ALU.max)
            k.vop("tensor_scalar", eq1, msk, v1, None, op0=ALU.is_equal)
            k.vop("scalar_tensor_tensor", eq1, eq1, -2.0, msk, op0=ALU.mult, op1=ALU.add)
            k.vop("tensor_reduce", v2, eq1, axis=AX.X, op=ALU.max)
            k.vop("tensor_scalar", sel, msk, v2, None, op0=ALU.is_ge)
            k.vop("scalar_tensor_tensor", gsel, msk, 1.0, sel, op0=ALU.mult, op1=ALU.mult, accum_out=den)
            k.vop("reciprocal", rden, den)
            k.vop("tensor_scalar", gates[:, j, :], gsel, rden, None, op0=ALU.mult)
            tn += 1
        hs = ntok // 2
        for e in range(n_exp):
            wb = ne % 2
            ne += 1
            k.dma("gpsimd", w1[wb], dr["moe_w1"][layer, e].rearrange("(k p) f -> p k f", p=128))
            k.dma("gpsimd", w3[wb], dr["moe_w3"][layer, e].rearrange("(k p) f -> p k f", p=128))
            k.dma("gpsimd", w2[wb], dr["moe_w2"][layer, e].rearrange("(k p) n -> p k n", p=128),
                  max_dma_last_dim=4096)
            aT = actT[e % 2]
            for half in range(2):
                cs = slice(half * hs, (half + 1) * hs)
                for fc in range(4):
                    ps1 = k.banks[(nh % 2) * 2]
                    ps3 = k.banks[(nh % 2) * 2 + 1]
                    s_ = ssb[nh % 2]
                    nh += 1
                    for kk in range(8):
                        k.mm(ps1[:, :hs], w1[wb][:, kk, fc * 128:(fc + 1) * 128], hTb[:, kk, cs], kk == 0, kk == 7)
                    for kk in range(8):
                        k.mm(ps3[:, :hs], w3[wb][:, kk, fc * 128:(fc + 1) * 128], hTb[:, kk, cs], kk == 0, kk == 7)
                    k.act(s_[:, :hs], ps1[:, :hs], AF.Silu)
                    k.vop("tensor_tensor", aT[:, fc, cs], s_[:, :hs], ps3[:, :hs], op=ALU.mult)
            for j in range(nt):
                for dh in range(2):
                    psy = k.banks[4 + ny % 2]
                    ny += 1
                    ds_ = slice(dh * 512, (dh + 1) * 512)
                    for fc in range(4):
                        k.mm(psy, aT[:, fc, j * 128:(j + 1) * 128], w2[wb][:, fc, ds_], fc == 0, fc == 3)
                    if e == 0:
                        k.vop("tensor_scalar", yacc[:, j, ds_], psy, gates[:, j, e:e + 1], None, op0=ALU.mult)
                    else:
                        k.vop("scalar_tensor_tensor", yacc[:, j, ds_], psy, gates[:, j, e:e + 1], yacc[:, j, ds_],
                              op0=ALU.mult, op1=ALU.add)
        for j, (kind, ti) in enumerate(blk):
            x_ = xt[tn % 2]
            tn += 1
            rows = slice(ti * 128, (ti + 1) * 128)
            G = mod[kind][2]
            k.dma("sync", x_, srcs[kind][rows, :])
            if n_exp == 0:
                k.vop("tensor_copy", yacc[:, j, 0:NE], gates[:, j, :])
                k.vop("tensor_copy", yacc[:, j, NE:D], h32[0][:, NE:D])
            k.vop("tensor_tensor", tmp, yacc[:, j, :], G, op=ALU.mult)
            k.vop("tensor_tensor", x_, x_, tmp, op=ALU.add)
            k.dma("sync", dsts[kind][rows, :], x_)


NB = 12
NSLOT = NB * 512
U32 = mybir.dt.uint32
I32 = mybir.dt.int32


def phase_moe_sorted(k, dr, layer, srcs, dsts, tiles):
    nt = len(tiles)
    nblk = (nt * 128 + 4 * 511) // 512
    assert nblk <= NB
    k.reset()
    Hs, Ys, Gs = dr["Hs"], dr["Ys"], dr["Gs"]
    slot_u = k.alloc("slot_u", [128, nt], U32)
    g512 = k.alloc("g512", [128, NB], F32)
    g2048 = k.alloc("g2048", [128, NB], F32)
    mark = k.off
    ident = k.alloc("ident32", [128, 128], F32)
    k.dma("sync", ident, dr["ident"])
    ltri = k.alloc("ltri", [128, 128], F32)
    k.dma("sync", ltri, dr["ltri"])
    ones = k.alloc("ones", [128, 128], F32)
    k.vop("memset", ones, 1.0)
    rw = k.alloc("rw", [128, 8, NE], F32)
    k.dma("sync", rw, dr["router_w"].rearrange("(k p) e -> p k e", p=128))
    rb = k.alloc("rb", [128, NE], F32)
    load_bc(k, "sync", rb, dr["router_b"])
    mod = {}
    for kind, si in (("x", 0), ("ctx", 1)):
        if kind in srcs:
            mod[kind] = load_mod_tiles(k, dr, layer, si, "f", dr["norm_ffn_w"][layer], need_gate=False)
    xt = [k.alloc("xt", [128, D], F32) for _ in range(3)]
    h32 = [k.alloc("h32", [128, D], F32) for _ in range(2)]
    junk = k.alloc("junk", [128, D], BF16)
    t32s = [k.alloc("t32", [128, D], F32) for _ in range(2)]
    st = [k.alloc("st", [128, 4], F32) for _ in range(3)]
    hT32 = [k.alloc("hT32", [128, 8, 128], F32) for _ in range(2)]
    hb_all = k.alloc("hb_all", [128, nt, D], BF16)
    lgall = k.alloc("lgall", [128, nt, NE], F32)
    junks = [junk, k.alloc("junk2", [128, D], BF16)]

    def m1_gen(j, kind, ti):
        x_ = xt[j % 3]
        h_ = h32[j % 2]
        A_, B_, _ = mod[kind]
        rows = slice(ti * 128, (ti + 1) * 128)
        k.dma("sync", x_, srcs[kind][rows, :])
        yield from norm_mod_g(k, x_, A_, B_, h_, junks[j % 2], st[j % 3], t32s[j % 2], ss_eng="scalar")
        k.act(hb_all[:, j, :], h_, AF.Copy)
        for hb_ in range(2):
            bank = k.banks[6 + hb_]
            for c4 in range(4):
                c = hb_ * 4 + c4
                k.transpose(bank[:, c4 * 128:(c4 + 1) * 128], h_[:, c * 128:(c + 1) * 128], ident)
            k.vop("tensor_copy", hT32[j % 2][:, hb_ * 4:hb_ * 4 + 4, :], bank.rearrange("p (a b) -> p a b", a=4))
        yield
        lps = k.banks[4 + j % 2]
        for kk in range(8):
            k.mm(lps[:, 0:NE], hT32[j % 2][:, kk, :], rw[:, kk, :], kk == 0, kk == 7)
        k.vop("tensor_tensor", lgall[:, j, :], lps[:, 0:NE], rb, op=ALU.add)

    run_interleaved((m1_gen(j, kind, ti) for j, (kind, ti) in enumerate(tiles)), width=2)
    def al(name, n):
        return k.alloc(name, [128, n], F32)
    pg = al("pg", nt * 16); p6 = al("p6", nt * 24); msk = al("msk", nt * 16); eq1 = al("eq1", nt * 16)
    sel = al("sel", nt * 16); gates = al("gates", nt * 16); glp = al("glp", nt * 16)
    gs = al("gs", nt * 4); oh = al("oh", nt * 4); ohm = al("ohm", nt * 4); t4 = al("t4", nt * 4)
    tot = al("tot", nt * 4); cum = al("cum", nt * 4)
    mx = al("mx", nt); gm = al("gm", nt); v1 = al("v1", nt); v2 = al("v2", nt); den = al("den", nt)
    slot_f = al("slot_f", nt); onesr = al("onesr", nt)
    ng = al("ng", 4); cnt = al("cnt", 4); base = al("base", 4); endg = al("endg", 4)
    thr = al("thr", 9); cmp_ = al("cmp", 36); blk0 = al("blk0", NB); gidf = al("gidf", NB); tmpb = al("tmpb", NB)

    def v3(v, a, b):
        return v.rearrange("p (a b) -> p a b", a=a)
    lg3 = lgall
    k.vop("tensor_reduce", mx, lg3, axis=AX.X, op=ALU.max)
    k.vop("tensor_tensor", v3(pg, nt, 16), lg3, bcast_last(mx, 16), op=ALU.subtract)
    k.act(pg, pg, AF.Exp)
    pgv = v3(pg, nt * 4, 4)
    p6v = v3(p6, nt * 4, 6)
    k.vop("tensor_tensor", p6v[:, :, 0:3], pgv[:, :, 0:3], pgv[:, :, 1:4], op=ALU.add)
    k.vop("tensor_tensor", p6v[:, :, 3:5], pgv[:, :, 0:2], pgv[:, :, 2:4], op=ALU.add)
    k.vop("tensor_tensor", p6v[:, :, 5:6], pgv[:, :, 0:1], pgv[:, :, 3:4], op=ALU.add)
    k.vop("tensor_reduce", gs, p6v, axis=AX.X, op=ALU.max)
    k.vop("tensor_reduce", gm, v3(gs, nt, 4), axis=AX.X, op=ALU.max)
    k.vop("tensor_tensor", v3(oh, nt, 4), v3(gs, nt, 4), bcast_last(gm, 4), op=ALU.is_equal)
    k.vop("tensor_scalar", ohm, oh, -1.0, None, op0=ALU.add)
    mskv = v3(msk, nt * 4, 4)
    k.vop("tensor_tensor", mskv, pgv, bcast_last(oh, 4), op=ALU.mult)
    k.vop("tensor_tensor", mskv, mskv, bcast_last(ohm, 4), op=ALU.add)
    msk3 = v3(msk, nt, 16)
    k.vop("tensor_reduce", v1, msk3, axis=AX.X, op=ALU.max)
    k.vop("tensor_tensor", v3(eq1, nt, 16), msk3, bcast_last(v1, 16), op=ALU.is_equal)
    k.vop("scalar_tensor_tensor", eq1, eq1, -2.0, msk, op0=ALU.mult, op1=ALU.add)
    k.vop("tensor_reduce", v2, v3(eq1, nt, 16), axis=AX.X, op=ALU.max)
    k.vop("tensor_tensor", v3(sel, nt, 16), msk3, bcast_last(v2, 16), op=ALU.is_ge)
    k.vop("tensor_tensor", sel, sel, msk, op=ALU.mult)
    k.vop("tensor_reduce", den, v3(sel, nt, 16), axis=AX.X, op=ALU.add)
    k.vop("reciprocal", den, den)
    k.vop("tensor_tensor", v3(gates, nt, 16), v3(sel, nt, 16), bcast_last(den, 16), op=ALU.mult)
    k.vop("memset", glp, 0.0)
    k.vop("tensor_reduce", v3(glp, nt, 16)[:, :, 0:4], apv(gates, [[16, nt], [1, 4], [4, 4]]), axis=AX.X, op=ALU.add)
    pre_ps = k.banks[4]
    tot_ps = k.banks[5]
    k.mm(pre_ps[:, 0:nt * 4], ltri, oh, True, True)
    k.mm(tot_ps[:, 0:nt * 4], ones, oh, True, True)
    k.vop("tensor_copy", tot, tot_ps[:, 0:nt * 4])
    k.vop("memset", onesr, 1.0)
    for g in range(4):
        k.vop("tensor_tensor_scan", apv(cum, [[4, nt]], extra_off=g), onesr, apv(tot, [[4, nt]], extra_off=g), 0.0,
              op0=ALU.mult, op1=ALU.add)
    k.vop("tensor_copy", ng, cum[:, (nt - 1) * 4:nt * 4])
    k.vop("tensor_tensor", cum, cum, tot, op=ALU.subtract)
    for m in range(9):
        k.vop("memset", thr[:, m:m + 1], float(512 * m))
    k.vop("tensor_tensor", v3(cmp_, 4, 9), bcast_last(ng, 9), apv(thr, [[0, 4], [1, 9]]), op=ALU.is_gt)
    k.vop("tensor_reduce", cnt, v3(cmp_, 4, 9), axis=AX.X, op=ALU.add)
    k.vop("tensor_scalar", cnt, cnt, 512.0, None, op0=ALU.mult)
    k.vop("memset", base[:, 0:1], 0.0)
    for g in range(1, 4):
        k.vop("tensor_tensor", base[:, g:g + 1], base[:, g - 1:g], cnt[:, g - 1:g], op=ALU.add)
    k.vop("tensor_tensor", endg, base, cnt, op=ALU.add)
    k.vop("tensor_tensor", t4, cum, pre_ps[:, 0:nt * 4], op=ALU.add)
    k.vop("tensor_tensor", v3(t4, nt, 4), v3(t4, nt, 4), apv(base, [[0, nt], [1, 4]]), op=ALU.add)
    k.vop("tensor_tensor", t4, t4, oh, op=ALU.mult)
    k.vop("tensor_reduce", slot_f, v3(t4, nt, 4), axis=AX.X, op=ALU.add)
    k.vop("tensor_copy", slot_u, slot_f)
    for i in range(NB):
        k.vop("memset", blk0[:, i:i + 1], float(512 * i))
    k.vop("tensor_scalar", gidf, blk0, endg[:, 0:1], None, op0=ALU.is_ge)
    for g in (1, 2):
        k.vop("tensor_scalar", tmpb, blk0, endg[:, g:g + 1], None, op0=ALU.is_ge)
        k.vop("tensor_tensor", gidf, gidf, tmpb, op=ALU.add)
    k.vop("tensor_scalar", g512, gidf, 512.0, None, op0=ALU.mult)
    k.vop("tensor_scalar", g2048, gidf, 2048.0, None, op0=ALU.mult)
    zt = k.alloc("zt", [128, 4096], BF16)
    k.vop("memset", zt, 0.0)
    zg = k.alloc("zg", [128, NSLOT * 16 // 128], F32)
    k.vop("memset", zg, 0.0)
    for i in range(NB):
        k.dma("sync", Hs[i * 512:(i + 1) * 512, :].rearrange("(p r) c -> p (r c)", p=128), zt)
    k.dma("sync", Gs.rearrange("(p r) c -> p (r c)", p=128), zg)
    k.P.barrier()
    for j in range(nt):
        idx = slot_u[:, j:j + 1]
        src_h = hb_all[:, j, :]
        src_g = v3(glp, nt, 16)[:, j, :]

        def sc_h(h, idx=idx, src=src_h):
            return h.indirect_dma_start(out=Hs, out_offset=bass.IndirectOffsetOnAxis(ap=idx.ap, axis=0),
                                        in_=src.ap, in_offset=None)

        def sc_g(h, idx=idx, src=src_g):
            return h.indirect_dma_start(out=Gs, out_offset=bass.IndirectOffsetOnAxis(ap=idx.ap, axis=0),
                                        in_=src.ap, in_offset=None)
        k.P.add("gpsimd", sc_h, reads=list(slot_u.bufs) + list(hb_all.bufs), dma=True)
        k.P.add("gpsimd", sc_g, reads=list(slot_u.bufs) + list(glp.bufs), dma=True)
    k.P.barrier()
    k.off = mark
    identb = k.alloc("identb", [128, 128], BF16)
    k.dma("gpsimd", identb, dr["ident"])
    ht4 = [k.alloc("ht4", [128, 4, D], BF16) for _ in range(2)]
    gt4 = [k.alloc("gt4", [128, 4, 16], F32) for _ in range(2)]
    hTb = [k.alloc("hTb", [128, 8, 512], BF16) for _ in range(2)]
    w1 = [k.alloc("w1", [128, 8 * FF], BF16) for _ in range(2)]
    w3 = [k.alloc("w3", [128, 8 * FF], BF16) for _ in range(2)]
    wst = [k.alloc("wst", [128, 8 * FF], F32) for _ in range(4)]
    w2 = [[k.alloc("w2", [128, D], BF16) for _ in range(4)] for _ in range(2)]
    ssb = [k.alloc("ssb", [128, 512], F32) for _ in range(2)]
    actT = [k.alloc("actT", [128, 4, 512], BF16) for _ in range(2)]
    yacc = [k.alloc("yacc", [128, 4, D], F32) for _ in range(2)]
    bankbf = [k.banks[6].bitcast(BF16), k.banks[7].bitcast(BF16)]
    cA = k.alloc("cA", [128, 32], F32)
    cB = k.alloc("cB", [128, 16], F32)
    k.dma("sync", cA, dr["idxA"])
    k.dma("sync", cB, dr["idxB"])
    idxA = [k.alloc("idxA", [128, 4], U32) for _ in range(nblk)]
    idxB = [k.alloc("idxB", [128, 16], U32) for _ in range(nblk)]
    W1f = dr["moe_w1"].rearrange("l e (p k) c -> (l e p) (k c)", k=8)
    W3f = dr["moe_w3"].rearrange("l e (p k) c -> (l e p) (k c)", k=8)
    W2f = dr["moe_w2"].rearrange("l e f c -> (l e f) c")
    for i in range(nblk):
        k.vop("tensor_scalar", idxA[i], cA[:, 0:4], g512[:, i:i + 1], float(layer * NE * 128), op0=ALU.add, op1=ALU.add)
        k.vop("tensor_scalar", idxB[i], cB, g2048[:, i:i + 1], float(layer * NE * FF), op0=ALU.add, op1=ALU.add)
    ne = 0
    nh = 0
    ny = 0
    tpc = {"n": 0}

    def do_transposes(bi):
        h4_ = ht4[bi % 2]
        hTd = hTb[bi % 2]
        for a in range(4):
            bb = bankbf[tpc["n"] % 2]
            tpc["n"] += 1
            hv = h4_[:, a, :].rearrange("p (f k) -> p k f", k=8)
            for c in range(8):
                k.transpose(bb[:, c * 128:(c + 1) * 128], hv[:, c, :], identb)
            k.vop("tensor_copy", hTd[:, :, a * 128:(a + 1) * 128], bb.rearrange("p (a b) -> p a b", a=8))
    regs = {}
    for i in range(nblk):
        rows = slice(i * 512, (i + 1) * 512)
        h4 = ht4[i % 2]
        g4 = gt4[i % 2]
        hT_ = hTb[i % 2]
        ya = yacc[i % 2]
        if i == 0:
            k.dma("sync", h4, Hs[rows, :].rearrange("(a p) c -> p a c", p=128))
            k.dma("sync", g4, Gs[rows, :].rearrange("(a p) c -> p a c", p=128))
        if i + 1 < nblk:
            nrows = slice((i + 1) * 512, (i + 2) * 512)
            k.dma("sync", ht4[(i + 1) % 2], Hs[nrows, :].rearrange("(a p) c -> p a c", p=128))
            k.dma("sync", gt4[(i + 1) % 2], Gs[nrows, :].rearrange("(a p) c -> p a c", p=128))
        if i == 0:
            do_transposes(0)
        for el in range(4):
            wb = ne % 2
            ne += 1

            def gath(dst, srcW, idxv):
                def f(h):
                    return h.indirect_dma_start(out=dst.ap, out_offset=None, in_=srcW,
                                                in_offset=bass.IndirectOffsetOnAxis(ap=idxv.ap, axis=0))
                k.P.add("gpsimd", f, reads=list(idxv.bufs), writes=list(dst.bufs), dma=True)
            s1 = wst[(2 * ne) % 4]
            s3 = wst[(2 * ne + 1) % 4]
            gath(s1, W1f, idxA[i][:, el:el + 1])
            gath(s3, W3f, idxA[i][:, el:el + 1])
            k.act(w1[wb], s1, AF.Copy)
            k.act(w3[wb], s3, AF.Copy)
            for kk in range(4):
                gath(w2[wb][kk], W2f, idxB[i][:, el * 4 + kk:el * 4 + kk + 1])
            aT = actT[ne % 2]
            for fc in range(4):
                ps1 = k.banks[(nh % 2) * 2]
                ps3 = k.banks[(nh % 2) * 2 + 1]
                s_ = ssb[nh % 2]
                nh += 1
                for kk in range(8):
                    k.mm(ps1, w1[wb][:, kk * FF + fc * 128:kk * FF + (fc + 1) * 128], hT_[:, kk, :], kk == 0, kk == 7)
                for kk in range(8):
                    k.mm(ps3, w3[wb][:, kk * FF + fc * 128:kk * FF + (fc + 1) * 128], hT_[:, kk, :], kk == 0, kk == 7)
                k.act(s_, ps1, AF.Silu)
                k.vop("tensor_tensor", aT[:, fc, :], s_, ps3, op=ALU.mult)
            if el == 1 and i + 1 < nblk:
                do_transposes(i + 1)
            for a in range(4):
                for dh in range(2):
                    psy = k.banks[4 + ny % 2]
                    ny += 1
                    ds_ = slice(dh * 512, (dh + 1) * 512)
                    for fc in range(4):
                        k.mm(psy, aT[:, fc, a * 128:(a + 1) * 128], w2[wb][fc][:, ds_], fc == 0, fc == 3)
                    if el == 0:
                        k.vop("tensor_scalar", ya[:, a, ds_], psy, g4[:, a, el:el + 1], None, op0=ALU.mult)
                    else:
                        k.vop("scalar_tensor_tensor", ya[:, a, ds_], psy, g4[:, a, el:el + 1], ya[:, a, ds_],
                              op0=ALU.mult, op1=ALU.add)
        k.dma("sync", Ys[rows, :].rearrange("(a p) c -> p a c", p=128), ya)
    k.P.barrier()
    k.off = mark
    Gt = {}
    base_i = 3
    for kind, si in (("x", 0), ("ctx", 1)):
        if kind in srcs:
            Gt[kind] = k.alloc("G", [128, D], F32)
            load_bc(k, "sync", Gt[kind], dr["mods_d"][si, layer, (base_i + 2) * D:(base_i + 3) * D])
    xt = [k.alloc("xt", [128, D], F32) for _ in range(6)]
    yt = [k.alloc("yt", [128, D], F32) for _ in range(6)]
    for j, (kind, ti) in enumerate(tiles):
        x_ = xt[j % 6]
        y_ = yt[j % 6]
        rows = slice(ti * 128, (ti + 1) * 128)
        idx = slot_u[:, j:j + 1]

        def ga(h, idx=idx, dst=y_):
            return h.indirect_dma_start(out=dst.ap, out_offset=None, in_=Ys,
                                        in_offset=bass.IndirectOffsetOnAxis(ap=idx.ap, axis=0))
        k.P.add("gpsimd", ga, reads=list(slot_u.bufs), writes=list(y_.bufs), dma=True)
        k.dma("scalar", x_, srcs[kind][rows, :])
        k.vop("tensor_tensor", y_, y_, Gt[kind], op=ALU.mult)
        k.vop("tensor_tensor", x_, x_, y_, op=ALU.add)
        k.dma("sync", dsts[kind][rows, :], x_)


def phase_moe0(k, dr):
    tiles = [("ctx", 0), ("ctx", 1)] + [("x", i) for i in range(NXT)]
    blocks = [tiles[0:7], tiles[7:14], tiles[14:21], tiles[21:28], tiles[28:34]]
    import os
    if os.environ.get("MOE_SRC_X"):
        srcs = {"x": dr["x"], "ctx": dr["ctx"]}
    else:
        srcs = {"x": dr["XR1"], "ctx": dr["CR1"]}
    if os.environ.get("MOE_DENSE"):
        phase_moe(k, dr, 0, srcs, {"x": dr["XR2"], "ctx": dr["CR2"]}, blocks)
    else:
        phase_moe_sorted(k, dr, 0, srcs, {"x": dr["XR2"], "ctx": dr["CR2"]}, tiles)


def phase_moe1(k, dr):
    tiles = [("x", i) for i in range(NXT)]
    blocks = [tiles[8 * i:8 * i + 8] for i in range(4)]
    import os
    if os.environ.get("MOE_DENSE"):
        phase_moe(k, dr, 1, {"x": dr["XR3"]}, {"x": dr["out"]}, blocks)
    else:
        phase_moe_sorted(k, dr, 1, {"x": dr["XR3"]}, {"x": dr["out"]}, tiles)


QPAIRS = [(0, 4), (1, 5), (2, 6), (3, 7), (8, 12), (9, 13), (10, 14), (11, 15)]
NKT = TS // 128
ATT = {}


def apv(v, dims, extra_off=0):
    a = v.ap
    return V(bass.AP(a.tensor, a.offset + extra_off, [list(a.ap[0])] + [list(d) for d in dims]), v.bufs)


def phase_attn_proj(k, dr):
    k.reset()
    kT = k.alloc("kT", [128, 2, TS], BF16)
    Vaug = k.alloc("Vaug", [128, NKT * 4, 128], BF16)
    ATT["kT"], ATT["Vaug"], ATT["keep"] = kT, Vaug, k.off
    k.vop("memset", Vaug[:, :, 64:128], 1.0)
    ident = k.alloc("ident", [128, 128], BF16)
    k.dma("gpsimd", ident, dr["ident"])
    wv = dr["attn_qkv_w"][0].rearrange("(k p) n -> p k n", p=128)
    qkvw = []
    for kk in range(8):
        w = k.alloc(f"qkvw{kk}", [128, 1536], BF16)
        for G_ in range(2):
            for a_ in range(2):
                src = wv[:, kk, (8 * G_ + 4 * a_) * 64:(8 * G_ + 4 * a_ + 4) * 64].rearrange("p (i d) -> p i d", i=4)
                dst = w[:, 8 * G_ * 64:8 * G_ * 64 + 512].rearrange("p (i a d) -> p i a d", i=4, a=2)[:, :, a_, :]
                k.dma("gpsimd", dst, src)
        k.dma("gpsimd", w[:, 1024:1536], wv[:, kk, 1024:1536], max_dma_last_dim=2048)
        qkvw.append(w)
    Ax, Bx, _ = load_mod_tiles(k, dr, 1, 0, "m", dr["norm_mix_w"][1], need_gate=False)
    Ac, Bc, _ = load_mod_tiles(k, dr, 1, 1, "m", dr["norm_mix_w"][1], need_gate=False)
    gq = k.alloc("gq", [128, 20, 64], F32)
    g64 = k.alloc("g64", [128, 2, 64], F32)
    load_bc(k, "sync", g64[:, 0, :], dr["attn_q_norm_w"][0])
    load_bc(k, "sync", g64[:, 1, :], dr["attn_k_norm_w"][0])
    k.vop("tensor_copy", gq[:, 0:16, :], apv(g64[:, 0, :], [[0, 16], [1, 64]]))
    k.vop("tensor_copy", gq[:, 16:20, :], apv(g64[:, 1, :], [[0, 4], [1, 64]]))
    rp = k.alloc("rp", [128, NXT, 64], F32)
    k.dma("sync", rp, dr["rope"].rearrange("(n p) c -> p n c", p=128))
    xt = [k.alloc("xt", [128, D], F32) for _ in range(2)]
    junks = [k.alloc("junk", [128, D], BF16) for _ in range(2)]
    t32s = [k.alloc("t32", [128, D], F32) for _ in range(2)]
    hb = [k.alloc("hb", [128, D], BF16) for _ in range(2)]
    st = [k.alloc("st", [128, 4], F32) for _ in range(2)]
    hT = [k.alloc("hT", [128, 8, 128], BF16) for _ in range(2)]
    qk32 = [k.alloc("qk32", [128, 20, 64], F32) for _ in range(2)]
    sqs = [k.alloc("sq", [128, 20, 64], F32) for _ in range(2)]
    rst = [k.alloc("rst", [128, 64], F32) for _ in range(2)]
    tcs = [[k.alloc("tc", [128, 20, 32], F32) for _ in range(2)] for _ in range(2)]
    tss = [[k.alloc("ts", [128, 20, 32], F32) for _ in range(2)] for _ in range(2)]
    qr = [k.alloc("qr", [128, 20, 64], BF16) for _ in range(2)]
    qst = [k.alloc("qst", [128, 8, 512], BF16) for _ in range(2)]
    tiles = [("ctx", j) for j in range(NCT)] + [("x", i) for i in range(NXT)]

    def tile_gen(kind, ti, tn):
        isx = kind == "x"
        kt = ti if not isx else NCT + ti
        x_ = xt[tn % 2]
        A_, B_ = (Ax, Bx) if isx else (Ac, Bc)
        rows = slice(ti * 128, (ti + 1) * 128)
        k.dma("sync", x_, (dr["XR2"] if isx else dr["CR2"])[rows, :])
        yield from norm_mod_g(k, x_, A_, B_, hb[tn % 2], junks[tn % 2], st[tn % 2], t32s[tn % 2], ss_eng="scalar")
        hbank = k.banks[4 + tn % 2].bitcast(BF16)
        hT_ = hT[tn % 2]
        transpose_tile(k, hb[tn % 2], hT_, hbank, ident)
        yield
        sq = sqs[tn % 2]
        q32 = qk32[tn % 2]
        q32f = q32.rearrange("p a b -> p (a b)")
        if isx:
            for nbk in range(3):
                for kk in range(8):
                    k.mm(k.banks[nbk], hT_[:, kk, :], qkvw[kk][:, nbk * 512:(nbk + 1) * 512], kk == 0, kk == 7)
            k.act(q32f[:, 0:512], k.banks[0], AF.Copy)
            k.act(q32f[:, 512:1024], k.banks[1], AF.Copy)
            k.act(q32f[:, 1024:1280], k.banks[2][:, 0:256], AF.Copy)
            vsrc = k.banks[2][:, 256:512]
            h0, nh = 0, 20
        else:
            for kk in range(8):
                k.mm(k.banks[2], hT_[:, kk, :], qkvw[kk][:, 1024:1536], kk == 0, kk == 7)
            k.act(q32f[:, 1024:1280], k.banks[2][:, 0:256], AF.Copy)
            vsrc = k.banks[2][:, 256:512]
            h0, nh = 16, 4
        k.act(Vaug[:, kt * 4:kt * 4 + 4, 0:64], vsrc.rearrange("p (g d) -> p g d", g=4), AF.Copy)
        yield
        qh = q32[:, h0:h0 + nh, :]
        r_ = rst[tn % 2]
        k.vop("tensor_tensor", sq[:, h0:h0 + nh, :], qh, qh, op=ALU.mult)
        k.vop("tensor_reduce", r_[:, 0:nh], sq[:, h0:h0 + nh, :], axis=AX.X, op=ALU.add)
        k.vop("tensor_scalar", r_[:, 20:20 + nh], r_[:, 0:nh], 1.0 / 64, EPS, op0=ALU.mult, op1=ALU.add)
        k.act(r_[:, 40:40 + nh], r_[:, 20:20 + nh], AF.Sqrt)
        yield
        k.vop("reciprocal", r_[:, 0:nh], r_[:, 40:40 + nh])
        k.vop("tensor_tensor", qh, qh, bcast_last(r_[:, 0:nh], 64), op=ALU.mult)
        q_ = qr[tn % 2]
        if isx:
            k.vop("tensor_tensor", qh, qh, gq[:, h0:h0 + nh, :], op=ALU.mult)
            for rc in range(2):
                qv = apv(q32, [[64, 20], [16, 2], [1, 16]], extra_off=rc * 32)
                ov = apv(q_, [[64, 20], [16, 2], [1, 16]], extra_off=rc * 32)
                cosb = apv(rp[:, ti, rc * 32:rc * 32 + 16], [[0, 20], [0, 2], [1, 16]])
                sinb = apv(rp[:, ti, rc * 32 + 16:rc * 32 + 32], [[0, 20], [0, 2], [1, 16]])
                tcv = tcs[tn % 2][rc].rearrange("p h (t j) -> p h t j", t=2)
                tsv = tss[tn % 2][rc].rearrange("p h (t j) -> p h t j", t=2)
                k.vop("tensor_tensor", tcv, qv, cosb, op=ALU.mult)
                k.vop("tensor_tensor", tsv, qv, sinb, op=ALU.mult)
                k.vop("tensor_tensor", ov[:, :, 0, :], tcv[:, :, 0, :], tsv[:, :, 1, :], op=ALU.subtract)
                k.vop("tensor_tensor", ov[:, :, 1, :], tcv[:, :, 1, :], tsv[:, :, 0, :], op=ALU.add)
        else:
            k.vop("tensor_tensor", q_[:, h0:h0 + nh, :], qh, gq[:, h0:h0 + nh, :], op=ALU.mult)
        yield
        kbank = k.banks[3].bitcast(BF16)
        for m in range(2):
            k.transpose(kbank[:, m * 128:(m + 1) * 128], q_[:, 16 + 2 * m:18 + 2 * m, :].rearrange("p a b -> p (a b)"), ident)
        k.act(kT[:, :, kt * 128:(kt + 1) * 128], kbank[:, 0:256].rearrange("p (a b) -> p a b", a=2), AF.Copy)
        if isx:
            qbank = k.banks[6 + tn % 2].bitcast(BF16)
            qf = q_.rearrange("p a b -> p (a b)")
            for s_i in range(8):
                k.transpose(qbank[:, s_i * 128:(s_i + 1) * 128], qf[:, s_i * 128:(s_i + 1) * 128], ident)
            qs_ = qst[(ti // 4) % 2]
            k.vop("tensor_copy", qs_[:, :, (ti % 4) * 128:(ti % 4 + 1) * 128],
                  qbank.rearrange("p (a b) -> p a b", a=8))
            if ti % 4 == 3:
                t0 = (ti // 4) * 512
                k.dma("gpsimd", dr["QT"][:, :, t0:t0 + 512].rearrange("s p t -> p s t"), qs_)


    run_interleaved((tile_gen(kind, ti, n) for n, (kind, ti) in enumerate(tiles)), width=2)


def bank2(k, i):
    return V(k.psum[:, i:i + 2, :].rearrange("p a b -> p (a b)"), k.banks[i].bufs + k.banks[i + 1].bufs)


def phase_attn_core(k, dr):
    k.reset(keep=ATT["keep"])
    kT, Vaug = ATT["kT"], ATT["Vaug"]
    wv = dr["attn_o_w"][0].rearrange("(k p) n -> p k n", p=128)
    ow = []
    for kk in range(8):
        w = k.alloc(f"aow{kk}", [128, D], BF16)
        k.dma("gpsimd", w, wv[:, kk, :], max_dma_last_dim=4096)
        ow.append(w)
    G = k.alloc("G", [128, D], F32)
    load_bc(k, "sync", G, dr["mods_d"][0, 1, 2 * D:3 * D])
    qz = [k.alloc("qz", [128, 16, 512], BF16) for _ in range(2)]
    for q_ in qz:
        k.vop("memset", q_, 0.0)
    aT = [k.alloc("aT", [128, 8, 512], BF16) for _ in range(2)]
    NP = 4
    pT = [k.alloc("pT", [128, 1024], BF16) for _ in range(NP)]
    rl = [k.alloc("rl", [128, 512], F32) for _ in range(2)]
    rl0 = [k.alloc("rl0", [128, 512], F32) for _ in range(2)]
    osb = [k.alloc("osb", [128, 512], F32) for _ in range(2)]
    xt = [k.alloc("xt", [128, D], F32) for _ in range(3)]
    tmp = k.alloc("tmp", [128, D], F32)
    sb2 = [bank2(k, 2), bank2(k, 4), bank2(k, 6)]
    LOOK = 2
    nu = 0
    nr = 0
    tn = 0
    pending = []
    tnc = {"n": 0}

    def flush_oproj():
        while pending:
            qb_, a_o = pending.pop(0)
            for j in range(4):
                ti = qb_ * 4 + j
                x_ = xt[tnc["n"] % 3]
                tnc["n"] += 1
                rows = slice(ti * 128, (ti + 1) * 128)
                k.dma("sync", x_, dr["XR2"][rows, :])
                for half in range(2):
                    cs = slice(half * 512, (half + 1) * 512)
                    ps = k.banks[2 + half]
                    for c in range(8):
                        k.mm(ps, a_o[:, c, j * 128:(j + 1) * 128], ow[c][:, cs], c == 0, c == 7)
                    k.vop("tensor_tensor", tmp[:, cs], ps, G[:, cs], op=ALU.mult)
                    k.vop("tensor_tensor", x_[:, cs], x_[:, cs], tmp[:, cs], op=ALU.add)
                k.dma("gpsimd", dr["XR3"][rows, :], x_)

    for qb in range(T // 512):
        q_ = qz[qb % 2]
        a_ = aT[qb % 2]
        qsl = slice(qb * 512, (qb + 1) * 512)
        for G_ in range(2):
            k.dma("sync", q_[0:64, 8 * G_:8 * G_ + 4, :],
                  dr["QT"][4 * G_:4 * G_ + 4, 0:64, qsl].rearrange("s p t -> p s t"))
            k.dma("sync", q_[64:128, 8 * G_ + 4:8 * G_ + 8, :],
                  dr["QT"][4 * G_:4 * G_ + 4, 64:128, qsl].rearrange("s p t -> p s t"))
        for g in range(4):
            for pr in range(2):
                if g == 0 and pr == 1:
                    flush_oproj()
                heads = [4 * g + 2 * pr, 4 * g + 2 * pr + 1]
                sbk = {}

                def emit_S(n):
                    bank = sb2[(nu + n) % 3]
                    sbk[n] = bank
                    for i, h in enumerate(heads):
                        k.mm(bank[:, i * 512:(i + 1) * 512], kT[:, g // 2, n * 128:(n + 1) * 128],
                             q_[:, h, :], True, True)

                def emit_PV(n):
                    p_ = pT[(nu + n) % NP]
                    k.act(p_, sbk[n], AF.Exp, scale=0.125)
                    for i in range(2):
                        k.mm(k.banks[i], Vaug[:, n * 4 + g, :], p_[:, i * 512:(i + 1) * 512],
                             n == 0, n == NKT - 1)

                for n in range(LOOK):
                    emit_S(n)
                for n in range(NKT):
                    if n + LOOK < NKT:
                        emit_S(n + LOOK)
                    emit_PV(n)
                nu += NKT
                for i, h in enumerate(heads):
                    k.vop("tensor_copy", osb[i], k.banks[i])
                for i, h in enumerate(heads):
                    r_ = rl[nr % 2]
                    r0 = rl0[nr % 2]
                    nr += 1
                    ov_ = osb[i]
                    k.vop("reciprocal", r_[64:128, :], ov_[64:128, :])
                    k.vop("tensor_copy", r0[0:64, :], r_[64:128, :])
                    dp = (h % 2) * 64
                    k.vop("tensor_tensor", a_[dp:dp + 64, h // 2, :], ov_[0:64, :], r0[0:64, :], op=ALU.mult)
        pending.append((qb, a_))
    flush_oproj()


PHASES = [("adaln", phase_adaln), ("lru_in", phase_lru_in), ("lru_scan", phase_lru_scan),
          ("lru_out", phase_lru_out), ("moe0", phase_moe0),
          ("attn_proj", phase_attn_proj), ("attn_core", phase_attn_core), ("moe1", phase_moe1)]


IN_SPECS = {
    "x": [T, D], "c": [1, D], "ctx": [C, D], "c_ctx": [D],
    "ada_w": [2, D, 6 * D], "ada_b": [2, 6 * D], "norm_mix_w": [2, D], "norm_ffn_w": [2, D],
    "lru_in_w": [1, D, 2 * D], "lru_conv_w": [1, 4, D], "lru_conv_b": [1, D],
    "lru_gate_a_w": [1, 2, 8, 128, 128], "lru_gate_a_b": [1, 2, D],
    "lru_gate_x_w": [1, 2, 8, 128, 128], "lru_gate_x_b": [1, 2, D],
    "lru_lambda": [1, 2, D], "lru_out_w": [1, D, D],
    "attn_qkv_w": [1, D, 1536], "attn_q_norm_w": [1, 64], "attn_k_norm_w": [1, 64],
    "attn_o_w": [1, D, D], "router_w": [D, NE], "router_b": [NE],
    "moe_w1": [2, NE, D, FF], "moe_w3": [2, NE, D, FF], "moe_w2": [2, NE, FF, D],
    "ident": [128, 128], "rope": [T, 64], "ltri": [128, 128], "idxA": [128, 32], "idxB": [128, 16],
}
SCRATCH_SPECS = {
    "mods_d": ([2, 2, 6 * D], F32),
    "GG": ([8, 128, TS], BF16), "UU": ([8, 128, TS], F32), "ZZ": ([8, 128, TS], BF16),
    "XR1": ([T, D], F32), "CR1": ([C, D], F32), "XR2": ([T, D], F32), "CR2": ([C, D], F32),
    "XR3": ([T, D], F32), "QT": ([8, 128, T], BF16),
    "Hs": ([NSLOT, D], BF16), "Ys": ([NSLOT, D], F32), "Gs": ([NSLOT, 16], F32),
}


class DR(dict):
    def __init__(self, nc, dbg):
        super().__init__()
        self.nc = nc
        self.dbg = dbg
        self.used_inputs = []

    def __missing__(self, name):
        if not self.dbg:
            if name in ("XR1", "XR2", "XR3"):
                return self["out"]
            if name == "CR2":
                return self["CR1"]
            if name == "ZZ":
                return self["GG"]
            if name == "QT":
                return self["GG"][:, :, 0:T]
        if name in IN_SPECS:
            ap = self.nc.dram_tensor(name, list(IN_SPECS[name]), F32, kind="ExternalInput").ap()
            self.used_inputs.append(name)
        else:
            shape, dt = SCRATCH_SPECS[name]
            kind = "ExternalOutput" if self.dbg else "Internal"
            ap = self.nc.dram_tensor(name, list(shape), dt, kind=kind).ap()
        self[name] = ap
        return ap


def build(stop_after=None, dbg=False):
    nc = bass.Bass("TRN2", target_bir_lowering=False)
    dr = DR(nc, dbg)
    dr["out"] = nc.dram_tensor("out", [T, D], F32, kind="ExternalOutput").ap()
    with ExitStack() as stack:
        arena_bytes = 204 * 1024
        arena_t = stack.enter_context(nc.sbuf_tensor("arena", [128, arena_bytes], U8))
        pt = stack.enter_context(nc.psum_tensor("psum", [128, 8, 512], F32))
        banks = [V(pt[:, i, :], (Buf(f"bank{i}", excl=True),)) for i in range(8)]
        P = Prog(nc)
        k = K(nc, P, arena_t[:], arena_bytes, banks)
        k.psum = pt
        import os
        only = os.environ.get("PHASE_ONLY", "")
        for name, fn in PHASES:
            if only and name not in only.split(","):
                continue
            fn(k, dr)
            if stop_after == name:
                break
        P.barrier()
        P.add("sync", lambda h: h.nop(), dma=False)
        P.emit(stack)
    nc._used_inputs = list(dr.used_inputs)
    return nc


def make_in_maps(inputs, used=None):
    f = lambda a: np.ascontiguousarray(np.asarray(a, dtype=np.float32))
    shared = {n: f(inputs[n]) for n in (
        "c_ctx", "ada_w", "ada_b", "norm_mix_w", "norm_ffn_w", "lru_in_w", "lru_conv_w", "lru_conv_b",
        "lru_gate_a_w", "lru_gate_a_b", "lru_gate_x_w", "lru_gate_x_b", "lru_lambda", "lru_out_w",
        "attn_qkv_w", "attn_q_norm_w", "attn_k_norm_w", "attn_o_w", "router_w", "router_b",
        "moe_w1", "moe_w3", "moe_w2")}
    shared["ident"] = np.eye(128, dtype=np.float32)
    shared["ltri"] = np.triu(np.ones((128, 128), dtype=np.float32), k=1)
    p_ = np.arange(128, dtype=np.float32)[:, None, None]
    ia = np.zeros((128, 32), dtype=np.float32)
    ia[:, 0:4] = np.arange(4, dtype=np.float32)[None, :] * 128 + np.arange(128, dtype=np.float32)[:, None]
    shared["idxA"] = ia
    shared["idxB"] = np.ascontiguousarray((np.arange(4, dtype=np.float32)[None, :, None] * 512
                                           + np.arange(4, dtype=np.float32)[None, None, :] * 128 + p_).reshape(128, 16))
    inv = (np.float32(10000.0) ** (-np.arange(16, dtype=np.float32) / np.float32(16))).astype(np.float32)
    t = np.arange(T)
    pr = (t // 64).astype(np.float32)[:, None] * inv
    pc = (t % 64).astype(np.float32)[:, None] * inv
    rope = np.concatenate([np.cos(pr), np.sin(pr), np.cos(pc), np.sin(pc)], axis=1).astype(np.float32)
    shared["rope"] = np.ascontiguousarray(rope)
    x = f(inputs["x"]); c = f(inputs["c"]); ctx = f(inputs["ctx"])
    maps = []
    for b in range(8):
        m = dict(shared)
        m["x"] = x[b]; m["c"] = c[b:b + 1]; m["ctx"] = ctx[b]
        if used is not None:
            m = {n: v for n, v in m.items() if n in used}
        maps.append(m)
    return maps


def kernel(**inputs):
    nc = build()
    res = run_bass_kernel_spmd(nc, make_in_maps(inputs, nc._used_inputs), core_ids=list(range(8)))
    return np.stack([r["out"] for r in res.results], axis=0).astype(np.float32)
```

```python
import numpy as np
from contextlib import ExitStack
import concourse.bass as bass
import concourse.mybir as mybir
from concourse.bass_utils import run_bass_kernel_spmd

F32 = mybir.dt.float32
BF16 = mybir.dt.bfloat16
U8 = mybir.dt.uint8
AF = mybir.ActivationFunctionType
ALU = mybir.AluOpType
AX = mybir.AxisListType

D = 1024
T = 4096
C = 256
TS = T + C
NXT = T // 128
NCT = C // 128
NE = 16
FF = 512
EPS = 1e-6
ENGS = ("sync", "gpsimd", "scalar", "vector", "tensor")
NRING = {"sync": 28, "gpsimd": 24, "scalar": 8}


class Buf:
    __slots__ = ("name", "w", "r", "excl")

    def __init__(self, name="", excl=False):
        self.name = name
        self.w = None
        self.r = {}
        self.excl = excl


class Op:
    __slots__ = ("eng", "fn", "deps", "needed", "sig", "dma", "ring", "val", "prev")


class V:
    __slots__ = ("ap", "bufs")

    def __init__(self, ap, bufs):
        self.ap = ap
        self.bufs = tuple(bufs)

    def __getitem__(self, k):
        return V(self.ap[k], self.bufs)

    def rearrange(self, s, **kw):
        return V(self.ap.rearrange(s, **kw), self.bufs)

    def bitcast(self, dt):
        return V(self.ap.bitcast(dt), self.bufs)

    def with_bufs(self, *bufs):
        return V(self.ap, bufs)

    @property
    def shape(self):
        return self.ap.shape


def _ap(x):
    return x.ap if isinstance(x, V) else x


class Prog:
    def __init__(self, nc):
        self.nc = nc
        self.ops = {e: [] for e in ENGS}
        self.last_compute = {e: None for e in ENGS}
        self.dma_since = []
        self.pending = {e: None for e in ENGS}
        self.ring_pos = {e: 0 for e in ENGS}
        self.ring_last = {}
        self.ring_cnt = {}

    def add(self, eng, fn, reads=(), writes=(), dma=False):
        op = Op()
        op.eng = eng
        op.fn = fn
        op.dma = dma
        op.needed = False
        op.sig = 0
        op.deps = {}
        op.prev = None
        for b in reads:
            if b.w is not None:
                op.deps[b.w] = "raw"
            if b.excl:
                for key, o in b.r.items():
                    if key != eng:
                        op.deps.setdefault(o, "raw")
        for b in writes:
            if b.w is not None:
                op.deps.setdefault(b.w, "waw")
            for o in b.r.values():
                if o is not op:
                    op.deps.setdefault(o, "war")
        pend = self.pending[eng]
        if pend:
            for o in pend:
                op.deps[o] = "raw"
            self.pending[eng] = None
        for b in writes:
            b.w = op
            b.r = {}
        for b in reads:
            b.r[("dma", id(op)) if dma else eng] = op
        if dma:
            n = NRING[eng]
            key = (eng, self.ring_pos[eng] % n)
            self.ring_pos[eng] += 1
            op.ring = key
            self.ring_cnt[key] = self.ring_cnt.get(key, 0) + 1
            op.val = 16 * self.ring_cnt[key]
            op.prev = self.ring_last.get(key)
            self.ring_last[key] = op
            self.dma_since.append(op)
        else:
            self.last_compute[eng] = op
        self.ops[eng].append(op)
        return op

    def barrier(self):
        deps = [o for o in self.last_compute.values() if o is not None]
        latest = {}
        for o in self.dma_since:
            latest[o.ring] = o
        deps += list(latest.values())
        self.dma_since = list(latest.values())
        for e in ENGS:
            cur = self.pending[e] or []
            self.pending[e] = list(cur) + deps

    @staticmethod
    def _keep(op, d, kind):
        if d.dma:
            return True
        if d.eng != op.eng:
            return True
        if op.dma:
            return True
        if op.eng == "tensor":
            return False
        return True

    def emit(self, stack):
        nc = self.nc
        for e in ENGS:
            for op in self.ops[e]:
                for d, kind in op.deps.items():
                    if (not d.dma) and self._keep(op, d, kind):
                        d.needed = True
        for e in ENGS:
            cnt = 0
            for op in self.ops[e]:
                if (not op.dma) and op.needed:
                    cnt += 1
                    op.sig = cnt
        esem = {e: stack.enter_context(nc.semaphore("es_" + e)) for e in ENGS if e != "sync"}
        rsem = {}
        for e, n in NRING.items():
            for i in range(n):
                rsem[(e, i)] = stack.enter_context(nc.semaphore(f"rs_{e}_{i}"))
        block = stack.enter_context(nc.Block())
        prog = self

        def run_engine(e, h):
            waited = {}

            def wait(sem_key, sem, val):
                if waited.get(sem_key, 0) < val:
                    h.wait_ge(sem, val)
                    waited[sem_key] = val

            for op in prog.ops[e]:
                for d, kind in op.deps.items():
                    if not prog._keep(op, d, kind):
                        continue
                    if d.dma:
                        wait(d.ring, rsem[d.ring], d.val)
                    else:
                        wait(d.eng, esem[d.eng], d.sig)
                if op.dma and op.prev is not None:
                    wait(op.ring, rsem[op.ring], op.prev.val)
                ins = op.fn(h)
                if op.dma:
                    ins.then_inc(rsem[op.ring], 16)
                elif op.needed:
                    ins.then_inc(esem[e], 1)

        @block.sync
        def _(h):
            run_engine("sync", h)

        @block.gpsimd
        def _(h):
            run_engine("gpsimd", h)

        @block.scalar
        def _(h):
            run_engine("scalar", h)

        @block.vector
        def _(h):
            run_engine("vector", h)

        @block.tensor
        def _(h):
            run_engine("tensor", h)


def _bufs(*xs):
    out = []
    for x in xs:
        if isinstance(x, V):
            out.extend(x.bufs)
    return out


class K:
    def __init__(self, nc, P, arena, arena_bytes, banks):
        self.nc = nc
        self.P = P
        self.arena = arena
        self.arena_bytes = arena_bytes
        self.off = 0
        self.banks = banks
        self.uid = 0

    def reset(self, keep=0):
        self.P.barrier()
        self.off = keep

    def alloc(self, name, shape, dtype, npart=128):
        esz = {F32: 4, BF16: 2, U8: 1, mybir.dt.uint32: 4, mybir.dt.int32: 4}[dtype]
        n = 1
        for s in shape[1:]:
            n *= s
        nbytes = (n * esz + 63) // 64 * 64
        assert self.off + nbytes <= self.arena_bytes, (name, self.off, nbytes, self.arena_bytes)
        ap = self.arena[0:shape[0], self.off:self.off + n * esz].bitcast(dtype)
        self.off += nbytes
        if len(shape) == 3:
            ap = ap.rearrange("p (a b) -> p a b", a=shape[1])
        elif len(shape) == 4:
            ap = ap.rearrange("p (a b c) -> p a b c", a=shape[1], b=shape[2])
        self.uid += 1
        return V(ap, (Buf(f"{name}_{self.uid}"),))

    def dma(self, q, out, in_, **kw):
        o, i = _ap(out), _ap(in_)
        return self.P.add(q, lambda h: h.dma_start(out=o, in_=i, **kw),
                          reads=_bufs(in_), writes=_bufs(out), dma=True)

    def mm(self, out, lhsT, rhs, start, stop, extra_reads=()):
        o, l, r = _ap(out), _ap(lhsT), _ap(rhs)
        return self.P.add("tensor", lambda h: h.matmul(o, l, r, start=start, stop=stop),
                          reads=_bufs(lhsT, rhs) + list(extra_reads), writes=_bufs(out))

    def transpose(self, out, in_, ident):
        o, i, d = _ap(out), _ap(in_), _ap(ident)
        return self.P.add("tensor", lambda h: h.transpose(o, i, d),
                          reads=_bufs(in_, ident), writes=_bufs(out))

    def act(self, out, in_, func, bias=None, scale=1.0, accum_out=None, eng="scalar"):
        o, i = _ap(out), _ap(in_)
        kw = {}
        if bias is not None:
            kw["bias"] = _ap(bias)
        if accum_out is not None:
            kw["accum_out"] = _ap(accum_out)
        sc = _ap(scale)
        return self.P.add(eng, lambda h: h.activation(out=o, in_=i, func=func, scale=sc, **kw),
                          reads=_bufs(in_, bias, scale), writes=_bufs(out, accum_out))

    def vop(self, name, out, *ins, eng="vector", accum_out=None, **kw):
        o = _ap(out)
        args = [_ap(x) for x in ins]
        kws = {k: _ap(v) for k, v in kw.items()}
        if accum_out is not None:
            kws["accum_out"] = _ap(accum_out)
        return self.P.add(eng, lambda h: getattr(h, name)(o, *args, **kws),
                          reads=_bufs(*ins, *kw.values()), writes=_bufs(out, accum_out))


def run_interleaved(gens, width=2):
    it = iter(gens)
    active = []
    done = False
    while True:
        while not done and len(active) < width:
            g = next(it, None)
            if g is None:
                done = True
                break
            active.append(g)
        if not active:
            break
        for g in list(active):
            try:
                next(g)
            except StopIteration:
                active.remove(g)


def load_bc(k, q, dst, src_ap):
    return k.dma(q, dst, src_ap.partition_broadcast(128))


def load_mod_tiles(k, dr, layer, src, which, norm_w_ap, need_gate=True):
    base = 0 if which == "m" else 3
    mods = dr["mods_d"]
    A = k.alloc("A", [128, D], F32)
    B = k.alloc("B", [128, D], F32)
    tmp = k.alloc("gn", [128, D], F32)
    load_bc(k, "sync", B, mods[src, layer, (base + 0) * D:(base + 1) * D])
    load_bc(k, "sync", A, mods[src, layer, (base + 1) * D:(base + 2) * D])
    load_bc(k, "sync", tmp, norm_w_ap)
    k.vop("scalar_tensor_tensor", A, A, 1.0, tmp, op0=ALU.add, op1=ALU.mult)
    G = None
    if need_gate:
        G = k.alloc("G", [128, D], F32)
        load_bc(k, "sync", G, mods[src, layer, (base + 2) * D:(base + 3) * D])
    return A, B, G


def norm_mod(k, xt, A, B, h_out, junk, st, t32, ss_eng="vector"):
    ss, ms, sq, rs = st[:, 0:1], st[:, 1:2], st[:, 2:3], st[:, 3:4]
    if ss_eng == "vector":
        k.vop("scalar_tensor_tensor", junk, xt, 1.0, xt, op0=ALU.mult, op1=ALU.mult, accum_out=ss)
    else:
        k.act(junk, xt, AF.Square, accum_out=ss)
    k.vop("tensor_scalar", ms, ss, 1.0 / D, EPS, op0=ALU.mult, op1=ALU.add)
    k.act(sq, ms, AF.Sqrt)
    k.vop("reciprocal", rs, sq)
    k.vop("scalar_tensor_tensor", t32, xt, rs, A, op0=ALU.mult, op1=ALU.mult)
    k.vop("tensor_tensor", h_out, t32, B, op=ALU.add)


def norm_mod_g(k, xt, A, B, h_out, junk, st, t32, ss_eng="vector"):
    ss, ms, sq, rs = st[:, 0:1], st[:, 1:2], st[:, 2:3], st[:, 3:4]
    if ss_eng == "vector":
        k.vop("scalar_tensor_tensor", junk, xt, 1.0, xt, op0=ALU.mult, op1=ALU.mult, accum_out=ss)
    else:
        k.act(junk, xt, AF.Square, accum_out=ss)
    yield
    k.vop("tensor_scalar", ms, ss, 1.0 / D, EPS, op0=ALU.mult, op1=ALU.add)
    k.act(sq, ms, AF.Sqrt)
    yield
    k.vop("reciprocal", rs, sq)
    k.vop("scalar_tensor_tensor", t32, xt, rs, A, op0=ALU.mult, op1=ALU.mult)
    k.vop("tensor_tensor", h_out, t32, B, op=ALU.add)
    yield


def transpose_tile(k, h, dst, bank_bf, ident, evac_eng="scalar"):
    for c in range(8):
        k.transpose(bank_bf[:, c * 128:(c + 1) * 128], h[:, c * 128:(c + 1) * 128], ident)
    src = bank_bf.rearrange("p (a b) -> p a b", a=8)
    if evac_eng == "scalar":
        k.act(dst, src, AF.Copy)
    else:
        k.vop("tensor_copy", dst, src)


def phase_adaln(k, dr):
    k.reset()
    cT = k.alloc("cT", [128, 2, 8], F32)
    sT = k.alloc("sT", [128, 2, 8], F32)
    wt = [k.alloc(f"adaw{i}", [128, 8, 512], F32) for i in range(3)]
    bias = k.alloc("adab", [2, 2, 6 * D], F32)
    mods = k.alloc("mods", [2, 2, 6 * D], F32)
    k.dma("sync", cT[:, 0, :], dr["c"][0].rearrange("(p k) -> p k", k=8))
    k.dma("sync", cT[:, 1, :], dr["c_ctx"].rearrange("(p k) -> p k", k=8))
    for j in range(2):
        k.dma("sync", bias[j:j + 1, :, :], dr["ada_b"].rearrange("(o l) n -> o l n", o=1))
    k.act(sT, cT, AF.Silu)
    n = 0
    for layer in range(2):
        wv = dr["ada_w"][layer].rearrange("(p k) n -> p k n", k=8)
        for nchunk in range(12):
            w = wt[n % 3]
            k.dma("sync" if n % 2 == 0 else "scalar", w, wv[:, :, nchunk * 512:(nchunk + 1) * 512])
            ps = k.banks[n % 2]
            for kk in range(8):
                k.mm(ps[0:2, :], sT[:, :, kk], w[:, kk, :], kk == 0, kk == 7)
            sl = slice(nchunk * 512, (nchunk + 1) * 512)
            k.vop("tensor_tensor", mods[0:2, layer, sl], ps[0:2, :], bias[0:2, layer, sl], op=ALU.add)
            n += 1
    k.dma("sync", dr["mods_d"], mods)


GELU = AF.Gelu_apprx_tanh
LRU_BLOCKS = [("ctx", 0, 2, 0)] + [("x", 4 * i, 4, C + 512 * i) for i in range(8)]


def slow_dma(k, q, out, in_):
    return k.dma(q, out, in_, allow_slow_non_contiguous=True)


def phase_lru_in(k, dr):
    k.reset()
    ident = k.alloc("ident", [128, 128], BF16)
    k.dma("gpsimd", ident, dr["ident"])
    wv = dr["lru_in_w"][0].rearrange("(k p) n -> p k n", p=128)
    in_w = []
    for kk in range(8):
        w = k.alloc(f"in_w{kk}", [128, 2048], BF16)
        k.dma("gpsimd", w, wv[:, kk, :], max_dma_last_dim=4096)
        in_w.append(w)
    Ax, Bx, _ = load_mod_tiles(k, dr, 0, 0, "m", dr["norm_mix_w"][0], need_gate=False)
    Ac, Bc, _ = load_mod_tiles(k, dr, 0, 1, "m", dr["norm_mix_w"][0], need_gate=False)
    xt = [k.alloc("xt", [128, D], F32) for _ in range(3)]
    junk = k.alloc("junk", [128, D], BF16)
    t32s = [k.alloc("t32", [128, D], F32) for _ in range(2)]
    hb = [[k.alloc("hb", [128, D], BF16) for _ in range(4)] for _ in range(2)]
    st = [k.alloc("st", [128, 4], F32) for _ in range(4)]
    hT = [k.alloc("hT", [128, 8, 512], BF16) for _ in range(2)]
    gg = [k.alloc("gg", [128, 8, 512], BF16) for _ in range(2)]
    uu = [k.alloc("uu", [128, 8, 512], F32) for _ in range(2)]
    bankbf = [k.banks[6].bitcast(BF16), k.banks[7].bitcast(BF16)]
    cnt = {"tn": 0, "tp": 0}

    def norms(bi):
        src, t0, nt, tok0 = LRU_BLOCKS[bi]
        srcap = dr["ctx"] if src == "ctx" else dr["x"]
        A_, B_ = (Ac, Bc) if src == "ctx" else (Ax, Bx)
        for j in range(nt):
            tn = cnt["tn"]
            cnt["tn"] += 1
            x_ = xt[tn % 3]
            k.dma("sync", x_, srcap[(t0 + j) * 128:(t0 + j + 1) * 128, :])
            norm_mod(k, x_, A_, B_, hb[bi % 2][j], junk, st[tn % 4], t32s[tn % 2], ss_eng="scalar")

    def transposes(bi):
        src, t0, nt, tok0 = LRU_BLOCKS[bi]
        for j in range(nt):
            tp = cnt["tp"]
            cnt["tp"] += 1
            transpose_tile(k, hb[bi % 2][j], hT[bi % 2][:, :, j * 128:(j + 1) * 128], bankbf[tp % 2], ident)

    norms(0)
    transposes(0)
    for bi, (src, t0, nt, tok0) in enumerate(LRU_BLOCKS):
        hTb = hT[bi % 2]
        if bi + 1 < len(LRU_BLOCKS):
            norms(bi + 1)
        ntok = nt * 128
        for oc in range(16):
            ps = k.banks[oc % 6]
            for kk in range(8):
                k.mm(ps[:, :ntok], in_w[kk][:, oc * 128:(oc + 1) * 128], hTb[:, kk, :ntok], kk == 0, kk == 7)
            if oc < 8:
                k.act(gg[bi % 2][:, oc, :ntok], ps[:, :ntok], GELU)
            else:
                k.vop("tensor_copy", uu[bi % 2][:, oc - 8, :ntok], ps[:, :ntok])
        if bi + 1 < len(LRU_BLOCKS):
            transposes(bi + 1)
        k.dma("gpsimd", dr["GG"][:, :, tok0:tok0 + ntok].rearrange("c p t -> p c t"), gg[bi % 2][:, :, :ntok])
        k.dma("gpsimd", dr["UU"][:, :, tok0:tok0 + ntok].rearrange("c p t -> p c t"), uu[bi % 2][:, :, :ntok])


def phase_lru_scan(k, dr):
    k.reset()
    cw = k.alloc("cw", [128, 4, 8], F32)
    cb = k.alloc("cb", [128, 8], F32)
    gab = k.alloc("gab", [128, 2, 8], F32)
    gxb = k.alloc("gxb", [128, 2, 8], F32)
    lam = k.alloc("lam", [128, 2, 8], F32)
    cp = k.alloc("cp", [128, 2, 8], F32)
    for tap in range(4):
        slow_dma(k, "sync", cw[:, tap, :], dr["lru_conv_w"][0, tap].rearrange("(c p) -> p c", p=128))
    slow_dma(k, "sync", cb, dr["lru_conv_b"][0].rearrange("(c p) -> p c", p=128))
    for d in range(2):
        slow_dma(k, "sync", gab[:, d, :], dr["lru_gate_a_b"][0, d].rearrange("(c p) -> p c", p=128))
        slow_dma(k, "sync", gxb[:, d, :], dr["lru_gate_x_b"][0, d].rearrange("(c p) -> p c", p=128))
        slow_dma(k, "sync", lam[:, d, :], dr["lru_lambda"][0, d].rearrange("(c p) -> p c", p=128))
    k.act(cp, lam, AF.Exp, scale=-1.0)
    k.act(cp, cp, AF.Ln, bias=1.0)
    k.vop("tensor_scalar", cp, cp, -8.0, None, op0=ALU.mult)
    gw = k.alloc("gw", [128, 32, 128], BF16)
    k.dma("gpsimd", gw[:, 0:16, :], dr["lru_gate_a_w"][0].rearrange("d n i e -> i (d n) e"))
    k.dma("gpsimd", gw[:, 16:32, :], dr["lru_gate_x_w"][0].rearrange("d n i e -> i (d n) e"))
    XB = 259
    up = [k.alloc("up", [128, TS + 6], F32) for _ in range(1)]
    for u_ in up:
        k.vop("memset", u_, 0.0)
    uconv = k.alloc("uconv", [128, TS], F32)
    ub = k.alloc("ub", [128, TS], BF16)
    rrs = [k.alloc("rr", [128, TS], F32) for _ in range(2)]
    igs = [k.alloc("ig", [128, TS], F32) for _ in range(2)]
    sbs = [k.alloc("sb", [128, TS], F32) for _ in range(2)]
    y = k.alloc("y", [128, TS], F32)
    ggc = [k.alloc("ggc", [128, TS], BF16) for _ in range(1)]
    z = [k.alloc("z", [128, TS], BF16) for _ in range(1)]
    pieces = [(0, 256)] + [(C + 512 * j, 512) for j in range(8)]
    segs = [(2, 0, C), (XB + 2, C, T)]
    nb = 0
    for c in range(8):
        u_ = up[0]
        k.dma("sync", u_[:, 2:2 + C], dr["UU"][c, :, 0:C])
        k.dma("sync", u_[:, XB + 2:XB + 2 + T], dr["UU"][c, :, C:TS])
        k.dma("sync", ggc[0], dr["GG"][c])
        for (sbase, dbase, L) in segs:
            dst = uconv[:, dbase:dbase + L]
            k.vop("tensor_scalar", dst, u_[:, sbase - 2:sbase - 2 + L], cw[:, 0, c:c + 1], cb[:, c:c + 1],
                  op0=ALU.mult, op1=ALU.add)
            for tap in range(1, 4):
                k.vop("scalar_tensor_tensor", dst, u_[:, sbase - 2 + tap:sbase - 2 + tap + L],
                      cw[:, tap, c:c + 1], dst, op0=ALU.mult, op1=ALU.add)
        k.act(ub, uconv, AF.Copy)
        def dir_gen(d, c=c):
            nonlocal nb
            rr, ig, sb = rrs[d], igs[d], sbs[d]
            for pi, (p0, pl) in enumerate(pieces):
                psr = k.banks[nb % 6]
                psi = k.banks[(nb + 1) % 6]
                nb += 2
                k.mm(psr[:, :pl], gw[:, d * 8 + c, :], ub[:, p0:p0 + pl], True, True)
                k.mm(psi[:, :pl], gw[:, 16 + d * 8 + c, :], ub[:, p0:p0 + pl], True, True)
                k.act(rr[:, p0:p0 + pl], psr[:, :pl], AF.Sigmoid, bias=gab[:, d, c:c + 1])
                k.act(ig[:, p0:p0 + pl], psi[:, :pl], AF.Sigmoid, bias=gxb[:, d, c:c + 1])
                if pi % 3 == 2:
                    yield
            yield
            k.act(rr, rr, AF.Exp, scale=cp[:, d, c:c + 1])
            k.vop("tensor_tensor", ig, ig, uconv, op=ALU.mult, eng="gpsimd")
            yield
            k.act(sb, rr, AF.Square)
            yield
            k.act(sb, sb, AF.Sqrt, bias=1.0, scale=-1.0)
            yield
            k.vop("tensor_tensor", ig, ig, sb, op=ALU.mult)
            yield
            hd = y if d == 0 else sb
            if d == 0:
                k.vop("tensor_tensor_scan", hd[:, 0:C], rr[:, 0:C], ig[:, 0:C], 0.0, op0=ALU.mult, op1=ALU.add)
                yield
                k.vop("tensor_tensor_scan", hd[:, C:TS], rr[:, C:TS], ig[:, C:TS], hd[:, C - 1:C],
                      op0=ALU.mult, op1=ALU.add)
            else:
                k.vop("tensor_tensor_scan", hd[:, 0:C][:, ::-1], rr[:, 0:C][:, ::-1], ig[:, 0:C][:, ::-1], 0.0,
                      op0=ALU.mult, op1=ALU.add)
                yield
                k.vop("tensor_tensor_scan", hd[:, C:TS][:, ::-1], rr[:, C:TS][:, ::-1], ig[:, C:TS][:, ::-1],
                      hd[:, 0:1], op0=ALU.mult, op1=ALU.add)

        run_interleaved([dir_gen(0), dir_gen(1)], width=2)
        k.vop("tensor_tensor", y, y, sbs[1], op=ALU.add)
        k.vop("tensor_tensor", z[0], y, ggc[0], op=ALU.mult)
        k.dma("gpsimd", dr["ZZ"][c], z[0])


def phase_lru_out(k, dr):
    k.reset()
    wv = dr["lru_out_w"][0].rearrange("(k p) n -> p k n", p=128)
    ow = []
    for kk in range(8):
        w = k.alloc(f"ow{kk}", [128, D], BF16)
        k.dma("gpsimd", w, wv[:, kk, :], max_dma_last_dim=4096)
        ow.append(w)
    Gx = k.alloc("Gx", [128, D], F32)
    Gc = k.alloc("Gc", [128, D], F32)
    load_bc(k, "sync", Gx, dr["mods_d"][0, 0, 2 * D:3 * D])
    load_bc(k, "sync", Gc, dr["mods_d"][1, 0, 2 * D:3 * D])
    zb = [k.alloc("zb", [128, 8, 512], BF16) for _ in range(2)]
    xt = [k.alloc("xt", [128, D], F32) for _ in range(3)]
    tmp = k.alloc("tmp", [128, D], F32)
    tn = 0
    nb = 0
    for bi, (src, t0, nt, tok0) in enumerate(LRU_BLOCKS):
        ntok = nt * 128
        zt = zb[bi % 2]
        k.dma("sync", zt[:, :, :ntok], dr["ZZ"][:, :, tok0:tok0 + ntok].rearrange("c p t -> p c t"))
        srcap = dr["ctx"] if src == "ctx" else dr["x"]
        dstap = dr["CR1"] if src == "ctx" else dr["XR1"]
        G = Gc if src == "ctx" else Gx
        for j in range(nt):
            x_ = xt[tn % 3]
            tn += 1
            rows = slice((t0 + j) * 128, (t0 + j + 1) * 128)
            k.dma("sync", x_, srcap[rows, :])
            for half in range(2):
                cs = slice(half * 512, (half + 1) * 512)
                ps = k.banks[nb % 4]
                nb += 1
                for kk in range(8):
                    k.mm(ps, zt[:, kk, j * 128:(j + 1) * 128], ow[kk][:, cs], kk == 0, kk == 7)
                k.vop("tensor_tensor", tmp[:, cs], ps, G[:, cs], op=ALU.mult)
                k.vop("tensor_tensor", x_[:, cs], x_[:, cs], tmp[:, cs], op=ALU.add)
            k.dma("gpsimd", dstap[rows, :], x_)


def bcast_last(v, n):
    a = v.ap
    dims = [list(d) for d in a.ap]
    return V(bass.AP(a.tensor, a.offset, dims + [[0, n]]), v.bufs)


def phase_moe(k, dr, layer, srcs, dsts, blocks):
    import os
    dbg_mode = os.environ.get("MOE_DBG", "")
    if dbg_mode:
        blocks = blocks[:1]
    n_exp = {"": NE, "pro": 0, "pro0": 0, "e1": 1, "e2": 2}[dbg_mode]
    k.reset()
    ident = k.alloc("ident32", [128, 128], F32)
    k.dma("sync", ident, dr["ident"])
    rw = k.alloc("rw", [128, 8, NE], F32)
    k.dma("sync", rw, dr["router_w"].rearrange("(k p) e -> p k e", p=128))
    rb = k.alloc("rb", [128, NE], F32)
    load_bc(k, "sync", rb, dr["router_b"])
    mod = {}
    for kind, si in (("x", 0), ("ctx", 1)):
        if kind in srcs:
            mod[kind] = load_mod_tiles(k, dr, layer, si, "f", dr["norm_ffn_w"][layer])
    maxnt = max(len(b) for b in blocks)
    xt = [k.alloc("xt", [128, D], F32) for _ in range(2)]
    h32 = [k.alloc("h32", [128, D], F32) for _ in range(2)]
    junk = k.alloc("junk", [128, D], BF16)
    t32 = k.alloc("t32", [128, D], F32)
    st = [k.alloc("st", [128, 4], F32) for _ in range(2)]
    hT32 = [k.alloc("hT32", [128, 8, 128], F32) for _ in range(2)]
    hTb = k.alloc("hTb", [128, 8, maxnt * 128], BF16)
    gates = k.alloc("gates", [128, maxnt, NE], F32)
    rs = [k.alloc("rs", [128, 160], F32) for _ in range(2)]
    w1 = [k.alloc("w1", [128, 8, FF], BF16) for _ in range(2)]
    w3 = [k.alloc("w3", [128, 8, FF], BF16) for _ in range(2)]
    w2 = [k.alloc("w2", [128, 4, D], BF16) for _ in range(2)]
    ssb = [k.alloc("ssb", [128, 512], F32) for _ in range(2)]
    actT = [k.alloc("actT", [128, 4, maxnt * 128], BF16) for _ in range(2)]
    yacc = k.alloc("yacc", [128, maxnt, D], F32)
    tmp = k.alloc("tmp", [128, D], F32)
    tn = 0
    ne = 0
    nh = 0
    ny = 0
    for blk in blocks:
        nt = len(blk)
        ntok = nt * 128
        for j, (kind, ti) in enumerate(blk):
            x_ = xt[tn % 2]
            h_ = h32[tn % 2]
            r_ = rs[tn % 2]
            A_, B_, _ = mod[kind]
            rows = slice(ti * 128, (ti + 1) * 128)
            k.dma("sync", x_, srcs[kind][rows, :])
            norm_mod(k, x_, A_, B_, h_, junk, st[tn % 2], t32)
            for hb_ in range(2):
                bank = k.banks[6 + hb_]
                for c4 in range(4):
                    c = hb_ * 4 + c4
                    k.transpose(bank[:, c4 * 128:(c4 + 1) * 128], h_[:, c * 128:(c + 1) * 128], ident)
                src = bank.rearrange("p (a b) -> p a b", a=4)
                k.vop("tensor_copy", hT32[tn % 2][:, hb_ * 4:hb_ * 4 + 4, :], src)
            k.act(hTb[:, :, j * 128:(j + 1) * 128], hT32[tn % 2], AF.Copy)
            lps = k.banks[5]
            for kk in range(8):
                k.mm(lps[:, 0:NE], hT32[tn % 2][:, kk, :], rw[:, kk, :], kk == 0, kk == 7)
            if dbg_mode == "pro0":
                k.vop("tensor_copy", gates[:, j, :], lps[:, 0:NE])
                tn += 1
                continue
            lg = r_[:, 0:16]
            pg = r_[:, 16:32]
            p6 = r_[:, 32:56].rearrange("p (a b) -> p a b", a=4)
            msk = r_[:, 56:72]
            eq1 = r_[:, 72:88]
            sel = r_[:, 88:104]
            gsel = r_[:, 104:120]
            gs = r_[:, 120:124]
            oh = r_[:, 124:128]
            ohm = r_[:, 128:132]
            mx, nmx, gm, v1, v2, den, rden = (r_[:, 132 + i:133 + i] for i in range(7))
            pgv = pg.rearrange("p (a b) -> p a b", a=4)
            mskv = msk.rearrange("p (a b) -> p a b", a=4)
            k.vop("tensor_tensor", lg, lps[:, 0:NE], rb, op=ALU.add)
            k.vop("tensor_reduce", mx, lg, axis=AX.X, op=ALU.max)
            k.vop("tensor_scalar", nmx, mx, -1.0, None, op0=ALU.mult)
            k.act(pg, lg, AF.Exp, bias=nmx)
            k.vop("tensor_tensor", p6[:, :, 0:3], pgv[:, :, 0:3], pgv[:, :, 1:4], op=ALU.add)
            k.vop("tensor_tensor", p6[:, :, 3:5], pgv[:, :, 0:2], pgv[:, :, 2:4], op=ALU.add)
            k.vop("tensor_tensor", p6[:, :, 5:6], pgv[:, :, 0:1], pgv[:, :, 3:4], op=ALU.add)
            k.vop("tensor_reduce", gs, p6, axis=AX.X, op=ALU.max)
            k.vop("tensor_reduce", gm, gs, axis=AX.X, op=ALU.max)
            k.vop("tensor_scalar", oh, gs, gm, None, op0=ALU.is_equal)
            k.vop("tensor_scalar", ohm, oh, -1.0, None, op0=ALU.add)
            k.vop("tensor_tensor", mskv, pgv, bcast_last(oh, 4), op=ALU.mult)
            k.vop("tensor_tensor", mskv, mskv, bcast_last(ohm, 4), op=ALU.add)
            k.vop("tensor_reduce", v1, msk, axis=AX.X, op=ALU.max)
            k.vop("tensor_scalar", eq1, msk, v1, None, op0=ALU.is_equal)
            k.vop("scalar_tensor_tensor", eq1, eq1, -2.0, msk, op0=ALU.mult, op1=ALU.add)
            k.vop("tensor_reduce", v2, eq1, axis=AX.X, op=ALU.max)
            k.vop("tensor_scalar", sel, msk, v2, None, op0=ALU.is_ge)
            k.vop("scalar_tensor_tensor", gsel, msk, 1.0, sel, op0=ALU.mult, op1=ALU.mult, accum_out=den)
            k.vop("reciprocal", rden, den)
            k.vop("tensor_scalar", gates[:, j, :], gsel, rden, None, op0=ALU.mult)
            tn += 1
        hs = ntok // 2
        for e in range(n_exp):
            wb = ne % 2
            ne += 1
            k.dma("gpsimd", w1[wb], dr["moe_w1"][layer, e].rearrange("(k p) f -> p k f", p=128))
            k.dma("gpsimd", w3[wb], dr["moe_w3"][layer, e].rearrange("(k p) f -> p k f", p=128))
            k.dma("gpsimd", w2[wb], dr["moe_w2"][layer, e].rearrange("(k p) n -> p k n", p=128),
                  max_dma_last_dim=4096)
            aT = actT[e % 2]
            for half in range(2):
                cs = slice(half * hs, (half + 1) * hs)
                for fc in range(4):
                    ps1 = k.banks[(nh % 2) * 2]
                    ps3 = k.banks[(nh % 2) * 2 + 1]
                    s_ = ssb[nh % 2]
                    nh += 1
                    for kk in range(8):
                        k.mm(ps1[:, :hs], w1[wb][:, kk, fc * 128:(fc + 1) * 128], hTb[:, kk, cs], kk == 0, kk == 7)
                    for kk in range(8):
                        k.mm(ps3[:, :hs], w3[wb][:, kk, fc * 128:(fc + 1) * 128], hTb[:, kk, cs], kk == 0, kk == 7)
                    k.act(s_[:, :hs], ps1[:, :hs], AF.Silu)
                    k.vop("tensor_tensor", aT[:, fc, cs], s_[:, :hs], ps3[:, :hs], op=ALU.mult)
            for j in range(nt):
                for dh in range(2):
                    psy = k.banks[4 + ny % 2]
                    ny += 1
                    ds_ = slice(dh * 512, (dh + 1) * 512)
                    for fc in range(4):
                        k.mm(psy, aT[:, fc, j * 128:(j + 1) * 128], w2[wb][:, fc, ds_], fc == 0, fc == 3)
                    if e == 0:
                        k.vop("tensor_scalar", yacc[:, j, ds_], psy, gates[:, j, e:e + 1], None, op0=ALU.mult)
                    else:
                        k.vop("scalar_tensor_tensor", yacc[:, j, ds_], psy, gates[:, j, e:e + 1], yacc[:, j, ds_],
                              op0=ALU.mult, op1=ALU.add)
        for j, (kind, ti) in enumerate(blk):
            x_ = xt[tn % 2]
            tn += 1
            rows = slice(ti * 128, (ti + 1) * 128)
            G = mod[kind][2]
            k.dma("sync", x_, srcs[kind][rows, :])
            if n_exp == 0:
                k.vop("tensor_copy", yacc[:, j, 0:NE], gates[:, j, :])
                k.vop("tensor_copy", yacc[:, j, NE:D], h32[0][:, NE:D])
            k.vop("tensor_tensor", tmp, yacc[:, j, :], G, op=ALU.mult)
            k.vop("tensor_tensor", x_, x_, tmp, op=ALU.add)
            k.dma("sync", dsts[kind][rows, :], x_)


NB = 12
NSLOT = NB * 512
U32 = mybir.dt.uint32
I32 = mybir.dt.int32


def phase_moe_sorted(k, dr, layer, srcs, dsts, tiles):
    nt = len(tiles)
    nblk = (nt * 128 + 4 * 511) // 512
    assert nblk <= NB
    k.reset()
    Hs, Ys, Gs = dr["Hs"], dr["Ys"], dr["Gs"]
    slot_u = k.alloc("slot_u", [128, nt], U32)
    g512 = k.alloc("g512", [128, NB], F32)
    g2048 = k.alloc("g2048", [128, NB], F32)
    mark = k.off
    zt = k.alloc("zt", [128, 4096], BF16)
    k.vop("memset", zt, 0.0, eng="gpsimd")
    zg = k.alloc("zg", [128, NSLOT * 16 // 128], F32)
    k.vop("memset", zg, 0.0, eng="gpsimd")
    for i in range(NB):
        k.dma("gpsimd", Hs[i * 512:(i + 1) * 512, :].rearrange("(p r) c -> p (r c)", p=128), zt)
    k.dma("gpsimd", Gs.rearrange("(p r) c -> p (r c)", p=128), zg)
    ident = k.alloc("ident32", [128, 128], F32)
    k.dma("sync", ident, dr["ident"])
    ltri = k.alloc("ltri", [128, 128], F32)
    k.dma("sync", ltri, dr["ltri"])
    ones = k.alloc("ones", [128, 128], F32)
    k.vop("memset", ones, 1.0)
    rw = k.alloc("rw", [128, 8, NE], F32)
    k.dma("sync", rw, dr["router_w"].rearrange("(k p) e -> p k e", p=128))
    rb = k.alloc("rb", [128, NE], F32)
    load_bc(k, "sync", rb, dr["router_b"])
    mod = {}
    for kind, si in (("x", 0), ("ctx", 1)):
        if kind in srcs:
            mod[kind] = load_mod_tiles(k, dr, layer, si, "f", dr["norm_ffn_w"][layer], need_gate=False)
    xt = [k.alloc("xt", [128, D], F32) for _ in range(3)]
    h32 = [k.alloc("h32", [128, D], F32) for _ in range(2)]
    junk = k.alloc("junk", [128, D], BF16)
    t32s = [k.alloc("t32", [128, D], F32) for _ in range(2)]
    st = [k.alloc("st", [128, 4], F32) for _ in range(3)]
    hT32 = [k.alloc("hT32", [128, 8, 128], F32) for _ in range(2)]
    hb_all = k.alloc("hb_all", [128, nt, D], BF16)
    lgall = k.alloc("lgall", [128, nt, NE], F32)
    junks = [junk, k.alloc("junk2", [128, D], BF16)]

    def m1_gen(j, kind, ti):
        x_ = xt[j % 3]
        h_ = h32[j % 2]
        A_, B_, _ = mod[kind]
        rows = slice(ti * 128, (ti + 1) * 128)
        k.dma("sync", x_, srcs[kind][rows, :])
        yield from norm_mod_g(k, x_, A_, B_, h_, junks[j % 2], st[j % 3], t32s[j % 2], ss_eng="scalar")
        k.act(hb_all[:, j, :], h_, AF.Copy)
        for hb_ in range(2):
            bank = k.banks[6 + hb_]
            for c4 in range(4):
                c = hb_ * 4 + c4
                k.transpose(bank[:, c4 * 128:(c4 + 1) * 128], h_[:, c * 128:(c + 1) * 128], ident)
            k.vop("tensor_copy", hT32[j % 2][:, hb_ * 4:hb_ * 4 + 4, :], bank.rearrange("p (a b) -> p a b", a=4))
        yield
        lps = k.banks[4 + j % 2]
        for kk in range(8):
            k.mm(lps[:, 0:NE], hT32[j % 2][:, kk, :], rw[:, kk, :], kk == 0, kk == 7)
        k.vop("tensor_tensor", lgall[:, j, :], lps[:, 0:NE], rb, op=ALU.add)

    run_interleaved((m1_gen(j, kind, ti) for j, (kind, ti) in enumerate(tiles)), width=2)
    def al(name, n):
        return k.alloc(name, [128, n], F32)
    pg = al("pg", nt * 16); p6 = al("p6", nt * 24); msk = al("msk", nt * 16); eq1 = al("eq1", nt * 16)
    sel = al("sel", nt * 16); gates = al("gates", nt * 16); glp = al("glp", nt * 16)
    gs = al("gs", nt * 4); oh = al("oh", nt * 4); ohm = al("ohm", nt * 4); t4 = al("t4", nt * 4)
    tot = al("tot", nt * 4); cum = al("cum", nt * 4)
    mx = al("mx", nt); gm = al("gm", nt); v1 = al("v1", nt); v2 = al("v2", nt); den = al("den", nt)
    slot_f = al("slot_f", nt); onesr = al("onesr", nt)
    ng = al("ng", 4); cnt = al("cnt", 4); base = al("base", 4); endg = al("endg", 4)
    thr = al("thr", 9); cmp_ = al("cmp", 36); blk0 = al("blk0", NB); gidf = al("gidf", NB); tmpb = al("tmpb", NB)

    def v3(v, a, b):
        return v.rearrange("p (a b) -> p a b", a=a)
    lg3 = lgall
    k.vop("tensor_reduce", mx, lg3, axis=AX.X, op=ALU.max)
    k.vop("tensor_tensor", v3(pg, nt, 16), lg3, bcast_last(mx, 16), op=ALU.subtract)
    k.act(pg, pg, AF.Exp)
    pgv = v3(pg, nt * 4, 4)
    p6v = v3(p6, nt * 4, 6)
    k.vop("tensor_tensor", p6v[:, :, 0:3], pgv[:, :, 0:3], pgv[:, :, 1:4], op=ALU.add)
    k.vop("tensor_tensor", p6v[:, :, 3:5], pgv[:, :, 0:2], pgv[:, :, 2:4], op=ALU.add)
    k.vop("tensor_tensor", p6v[:, :, 5:6], pgv[:, :, 0:1], pgv[:, :, 3:4], op=ALU.add)
    k.vop("tensor_reduce", gs, p6v, axis=AX.X, op=ALU.max)
    k.vop("tensor_reduce", gm, v3(gs, nt, 4), axis=AX.X, op=ALU.max)
    k.vop("tensor_tensor", v3(oh, nt, 4), v3(gs, nt, 4), bcast_last(gm, 4), op=ALU.is_equal)
    k.vop("tensor_scalar", ohm, oh, -1.0, None, op0=ALU.add)
    mskv = v3(msk, nt * 4, 4)
    k.vop("tensor_tensor", mskv, pgv, bcast_last(oh, 4), op=ALU.mult)
    k.vop("tensor_tensor", mskv, mskv, bcast_last(ohm, 4), op=ALU.add)
    msk3 = v3(msk, nt, 16)
    k.vop("tensor_reduce", v1, msk3, axis=AX.X, op=ALU.max)
    k.vop("tensor_tensor", v3(eq1, nt, 16), msk3, bcast_last(v1, 16), op=ALU.is_equal)
    k.vop("scalar_tensor_tensor", eq1, eq1, -2.0, msk, op0=ALU.mult, op1=ALU.add)
    k.vop("tensor_reduce", v2, v3(eq1, nt, 16), axis=AX.X, op=ALU.max)
    k.vop("tensor_tensor", v3(sel, nt, 16), msk3, bcast_last(v2, 16), op=ALU.is_ge)
    k.vop("tensor_tensor", sel, sel, msk, op=ALU.mult)
    k.vop("tensor_reduce", den, v3(sel, nt, 16), axis=AX.X, op=ALU.add)
    k.vop("reciprocal", den, den)
    k.vop("tensor_tensor", v3(gates, nt, 16), v3(sel, nt, 16), bcast_last(den, 16), op=ALU.mult)
    k.vop("memset", glp, 0.0)
    k.vop("tensor_reduce", v3(glp, nt, 16)[:, :, 0:4], apv(gates, [[16, nt], [1, 4], [4, 4]]), axis=AX.X, op=ALU.add)
    pre_ps = k.banks[4]
    tot_ps = k.banks[5]
    k.mm(pre_ps[:, 0:nt * 4], ltri, oh, True, True)
    k.mm(tot_ps[:, 0:nt * 4], ones, oh, True, True)
    k.vop("tensor_copy", tot, tot_ps[:, 0:nt * 4])
    k.vop("memset", onesr, 1.0)
    for g in range(4):
        k.vop("tensor_tensor_scan", apv(cum, [[4, nt]], extra_off=g), onesr, apv(tot, [[4, nt]], extra_off=g), 0.0,
              op0=ALU.mult, op1=ALU.add)
    k.vop("tensor_copy", ng, cum[:, (nt - 1) * 4:nt * 4])
    k.vop("tensor_tensor", cum, cum, tot, op=ALU.subtract)
    for m in range(9):
        k.vop("memset", thr[:, m:m + 1], float(512 * m))
    k.vop("tensor_tensor", v3(cmp_, 4, 9), bcast_last(ng, 9), apv(thr, [[0, 4], [1, 9]]), op=ALU.is_gt)
    k.vop("tensor_reduce", cnt, v3(cmp_, 4, 9), axis=AX.X, op=ALU.add)
    k.vop("tensor_scalar", cnt, cnt, 512.0, None, op0=ALU.mult)
    k.vop("memset", base[:, 0:1], 0.0)
    for g in range(1, 4):
        k.vop("tensor_tensor", base[:, g:g + 1], base[:, g - 1:g], cnt[:, g - 1:g], op=ALU.add)
    k.vop("tensor_tensor", endg, base, cnt, op=ALU.add)
    k.vop("tensor_tensor", t4, cum, pre_ps[:, 0:nt * 4], op=ALU.add)
    k.vop("tensor_tensor", v3(t4, nt, 4), v3(t4, nt, 4), apv(base, [[0, nt], [1, 4]]), op=ALU.add)
    k.vop("tensor_tensor", t4, t4, oh, op=ALU.mult)
    k.vop("tensor_reduce", slot_f, v3(t4, nt, 4), axis=AX.X, op=ALU.add)
    k.vop("tensor_copy", slot_u, slot_f)
    for i in range(NB):
        k.vop("memset", blk0[:, i:i + 1], float(512 * i))
    k.vop("tensor_scalar", gidf, blk0, endg[:, 0:1], None, op0=ALU.is_ge)
    for g in (1, 2):
        k.vop("tensor_scalar", tmpb, blk0, endg[:, g:g + 1], None, op0=ALU.is_ge)
        k.vop("tensor_tensor", gidf, gidf, tmpb, op=ALU.add)
    k.vop("tensor_scalar", g512, gidf, 512.0, None, op0=ALU.mult)
    k.vop("tensor_scalar", g2048, gidf, 2048.0, None, op0=ALU.mult)
    k.P.barrier()
    for j in range(nt):
        idx = slot_u[:, j:j + 1]
        src_h = hb_all[:, j, :]
        src_g = v3(glp, nt, 16)[:, j, :]

        def sc_h(h, idx=idx, src=src_h):
            return h.indirect_dma_start(out=Hs, out_offset=bass.IndirectOffsetOnAxis(ap=idx.ap, axis=0),
                                        in_=src.ap, in_offset=None)

        def sc_g(h, idx=idx, src=src_g):
            return h.indirect_dma_start(out=Gs, out_offset=bass.IndirectOffsetOnAxis(ap=idx.ap, axis=0),
                                        in_=src.ap, in_offset=None)
        k.P.add("gpsimd", sc_h, reads=list(slot_u.bufs) + list(hb_all.bufs), dma=True)
        k.P.add("gpsimd", sc_g, reads=list(slot_u.bufs) + list(glp.bufs), dma=True)
    k.P.barrier()
    k.off = mark
    identb = k.alloc("identb", [128, 128], BF16)
    k.dma("gpsimd", identb, dr["ident"])
    ht4 = [k.alloc("ht4", [128, 4, D], BF16) for _ in range(2)]
    gt4 = [k.alloc("gt4", [128, 4, 16], F32) for _ in range(2)]
    hTb = [k.alloc("hTb", [128, 8, 512], BF16) for _ in range(2)]
    w1 = [k.alloc("w1", [128, 8 * FF], BF16) for _ in range(2)]
    w3 = [k.alloc("w3", [128, 8 * FF], BF16) for _ in range(2)]
    wst = [k.alloc("wst", [128, 8 * FF], F32) for _ in range(4)]
    w2 = [[k.alloc("w2", [128, D], BF16) for _ in range(4)] for _ in range(2)]
    ssb = [k.alloc("ssb", [128, 512], F32) for _ in range(2)]
    actT = [k.alloc("actT", [128, 4, 512], BF16) for _ in range(2)]
    yacc = [k.alloc("yacc", [128, 4, D], F32) for _ in range(2)]
    bankbf = [k.banks[6].bitcast(BF16), k.banks[7].bitcast(BF16)]
    cA = k.alloc("cA", [128, 32], F32)
    cB = k.alloc("cB", [128, 16], F32)
    k.dma("sync", cA, dr["idxA"])
    k.dma("sync", cB, dr["idxB"])
    idxA = [k.alloc("idxA", [128, 4], U32) for _ in range(nblk)]
    idxB = [k.alloc("idxB", [128, 16], U32) for _ in range(nblk)]
    W1f = dr["moe_w1"].rearrange("l e (p k) c -> (l e p) (k c)", k=8)
    W3f = dr["moe_w3"].rearrange("l e (p k) c -> (l e p) (k c)", k=8)
    W2f = dr["moe_w2"].rearrange("l e f c -> (l e f) c")
    for i in range(nblk):
        k.vop("tensor_scalar", idxA[i], cA[:, 0:4], g512[:, i:i + 1], float(layer * NE * 128), op0=ALU.add, op1=ALU.add)
        k.vop("tensor_scalar", idxB[i], cB, g2048[:, i:i + 1], float(layer * NE * FF), op0=ALU.add, op1=ALU.add)
    ne = 0
    nh = 0
    ny = 0
    tpc = {"n": 0}

    def do_transposes(bi):
        h4_ = ht4[bi % 2]
        hTd = hTb[bi % 2]
        for a in range(4):
            bb = bankbf[tpc["n"] % 2]
            tpc["n"] += 1
            hv = h4_[:, a, :].rearrange("p (f k) -> p k f", k=8)
            for c in range(8):
                k.transpose(bb[:, c * 128:(c + 1) * 128], hv[:, c, :], identb)
            k.vop("tensor_copy", hTd[:, :, a * 128:(a + 1) * 128], bb.rearrange("p (a b) -> p a b", a=8))
    regs = {}
    for i in range(nblk):
        rows = slice(i * 512, (i + 1) * 512)
        h4 = ht4[i % 2]
        g4 = gt4[i % 2]
        hT_ = hTb[i % 2]
        ya = yacc[i % 2]
        if i == 0:
            k.dma("sync", h4, Hs[rows, :].rearrange("(a p) c -> p a c", p=128))
            k.dma("sync", g4, Gs[rows, :].rearrange("(a p) c -> p a c", p=128))
        if i + 1 < nblk:
            nrows = slice((i + 1) * 512, (i + 2) * 512)
            k.dma("sync", ht4[(i + 1) % 2], Hs[nrows, :].rearrange("(a p) c -> p a c", p=128))
            k.dma("sync", gt4[(i + 1) % 2], Gs[nrows, :].rearrange("(a p) c -> p a c", p=128))
        if i == 0:
            do_transposes(0)
        for el in range(4):
            wb = ne % 2
            ne += 1

            def gath(dst, srcW, idxv):
                def f(h):
                    return h.indirect_dma_start(out=dst.ap, out_offset=None, in_=srcW,
                                                in_offset=bass.IndirectOffsetOnAxis(ap=idxv.ap, axis=0))
                k.P.add("gpsimd", f, reads=list(idxv.bufs), writes=list(dst.bufs), dma=True)
            s1 = wst[(2 * ne) % 4]
            s3 = wst[(2 * ne + 1) % 4]
            gath(s1, W1f, idxA[i][:, el:el + 1])
            gath(s3, W3f, idxA[i][:, el:el + 1])
            k.act(w1[wb], s1, AF.Copy)
            k.act(w3[wb], s3, AF.Copy)
            for kk in range(4):
                gath(w2[wb][kk], W2f, idxB[i][:, el * 4 + kk:el * 4 + kk + 1])
            aT = actT[ne % 2]
            for fc in range(4):
                ps1 = k.banks[(nh % 2) * 2]
                ps3 = k.banks[(nh % 2) * 2 + 1]
                s_ = ssb[nh % 2]
                nh += 1
                for kk in range(8):
                    k.mm(ps1, w1[wb][:, kk * FF + fc * 128:kk * FF + (fc + 1) * 128], hT_[:, kk, :], kk == 0, kk == 7)
                for kk in range(8):
                    k.mm(ps3, w3[wb][:, kk * FF + fc * 128:kk * FF + (fc + 1) * 128], hT_[:, kk, :], kk == 0, kk == 7)
                k.act(s_, ps1, AF.Silu)
                k.vop("tensor_tensor", aT[:, fc, :], s_, ps3, op=ALU.mult)
            if el == 1 and i + 1 < nblk:
                do_transposes(i + 1)
            for a in range(4):
                for dh in range(2):
                    psy = k.banks[4 + ny % 2]
                    ny += 1
                    ds_ = slice(dh * 512, (dh + 1) * 512)
                    for fc in range(4):
                        k.mm(psy, aT[:, fc, a * 128:(a + 1) * 128], w2[wb][fc][:, ds_], fc == 0, fc == 3)
                    if el == 0:
                        k.vop("tensor_scalar", ya[:, a, ds_], psy, g4[:, a, el:el + 1], None, op0=ALU.mult)
                    else:
                        k.vop("scalar_tensor_tensor", ya[:, a, ds_], psy, g4[:, a, el:el + 1], ya[:, a, ds_],
                              op0=ALU.mult, op1=ALU.add)
        k.dma("sync", Ys[rows, :].rearrange("(a p) c -> p a c", p=128), ya)
    k.P.barrier()
    k.off = mark
    Gt = {}
    base_i = 3
    for kind, si in (("x", 0), ("ctx", 1)):
        if kind in srcs:
            Gt[kind] = k.alloc("G", [128, D], F32)
            load_bc(k, "sync", Gt[kind], dr["mods_d"][si, layer, (base_i + 2) * D:(base_i + 3) * D])
    xt = [k.alloc("xt", [128, D], F32) for _ in range(6)]
    yt = [k.alloc("yt", [128, D], F32) for _ in range(6)]
    for j, (kind, ti) in enumerate(tiles):
        x_ = xt[j % 6]
        y_ = yt[j % 6]
        rows = slice(ti * 128, (ti + 1) * 128)
        idx = slot_u[:, j:j + 1]

        def ga(h, idx=idx, dst=y_):
            return h.indirect_dma_start(out=dst.ap, out_offset=None, in_=Ys,
                                        in_offset=bass.IndirectOffsetOnAxis(ap=idx.ap, axis=0))
        k.P.add("gpsimd", ga, reads=list(slot_u.bufs), writes=list(y_.bufs), dma=True)
        k.dma("scalar", x_, srcs[kind][rows, :])
        k.vop("tensor_tensor", y_, y_, Gt[kind], op=ALU.mult)
        k.vop("tensor_tensor", x_, x_, y_, op=ALU.add)
        k.dma("sync", dsts[kind][rows, :], x_)


def phase_moe0(k, dr):
    tiles = [("ctx", 0), ("ctx", 1)] + [("x", i) for i in range(NXT)]
    blocks = [tiles[0:7], tiles[7:14], tiles[14:21], tiles[21:28], tiles[28:34]]
    import os
    if os.environ.get("MOE_SRC_X"):
        srcs = {"x": dr["x"], "ctx": dr["ctx"]}
    else:
        srcs = {"x": dr["XR1"], "ctx": dr["CR1"]}
    if os.environ.get("MOE_DENSE"):
        phase_moe(k, dr, 0, srcs, {"x": dr["XR2"], "ctx": dr["CR2"]}, blocks)
    else:
        phase_moe_sorted(k, dr, 0, srcs, {"x": dr["XR2"], "ctx": dr["CR2"]}, tiles)


def phase_moe1(k, dr):
    tiles = [("x", i) for i in range(NXT)]
    blocks = [tiles[8 * i:8 * i + 8] for i in range(4)]
    import os
    if os.environ.get("MOE_DENSE"):
        phase_moe(k, dr, 1, {"x": dr["XR3"]}, {"x": dr["out"]}, blocks)
    else:
        phase_moe_sorted(k, dr, 1, {"x": dr["XR3"]}, {"x": dr["out"]}, tiles)


QPAIRS = [(0, 4), (1, 5), (2, 6), (3, 7), (8, 12), (9, 13), (10, 14), (11, 15)]
NKT = TS // 128
ATT = {}


def apv(v, dims, extra_off=0):
    a = v.ap
    return V(bass.AP(a.tensor, a.offset + extra_off, [list(a.ap[0])] + [list(d) for d in dims]), v.bufs)


def phase_attn_proj(k, dr):
    k.reset()
    kT = k.alloc("kT", [128, 2, TS], BF16)
    Vaug = k.alloc("Vaug", [128, NKT * 4, 128], BF16)
    ATT["kT"], ATT["Vaug"], ATT["keep"] = kT, Vaug, k.off
    k.vop("memset", Vaug[:, :, 64:128], 1.0)
    ident = k.alloc("ident", [128, 128], BF16)
    k.dma("gpsimd", ident, dr["ident"])
    wv = dr["attn_qkv_w"][0].rearrange("(k p) n -> p k n", p=128)
    qkvw = []
    for kk in range(8):
        w = k.alloc(f"qkvw{kk}", [128, 1536], BF16)
        for G_ in range(2):
            for a_ in range(2):
                src = wv[:, kk, (8 * G_ + 4 * a_) * 64:(8 * G_ + 4 * a_ + 4) * 64].rearrange("p (i d) -> p i d", i=4)
                dst = w[:, 8 * G_ * 64:8 * G_ * 64 + 512].rearrange("p (i a d) -> p i a d", i=4, a=2)[:, :, a_, :]
                k.dma("gpsimd", dst, src)
        k.dma("gpsimd", w[:, 1024:1536], wv[:, kk, 1024:1536], max_dma_last_dim=2048)
        qkvw.append(w)
    Ax, Bx, _ = load_mod_tiles(k, dr, 1, 0, "m", dr["norm_mix_w"][1], need_gate=False)
    Ac, Bc, _ = load_mod_tiles(k, dr, 1, 1, "m", dr["norm_mix_w"][1], need_gate=False)
    gq = k.alloc("gq", [128, 20, 64], F32)
    g64 = k.alloc("g64", [128, 2, 64], F32)
    load_bc(k, "sync", g64[:, 0, :], dr["attn_q_norm_w"][0])
    load_bc(k, "sync", g64[:, 1, :], dr["attn_k_norm_w"][0])
    k.vop("tensor_copy", gq[:, 0:16, :], apv(g64[:, 0, :], [[0, 16], [1, 64]]))
    k.vop("tensor_copy", gq[:, 16:20, :], apv(g64[:, 1, :], [[0, 4], [1, 64]]))
    rp = k.alloc("rp", [128, NXT, 64], F32)
    k.dma("sync", rp, dr["rope"].rearrange("(n p) c -> p n c", p=128))
    xt = [k.alloc("xt", [128, D], F32) for _ in range(2)]
    junks = [k.alloc("junk", [128, D], BF16) for _ in range(2)]
    t32s = [k.alloc("t32", [128, D], F32) for _ in range(2)]
    hb = [k.alloc("hb", [128, D], BF16) for _ in range(2)]
    st = [k.alloc("st", [128, 4], F32) for _ in range(2)]
    hT = [k.alloc("hT", [128, 8, 128], BF16) for _ in range(2)]
    qk32 = [k.alloc("qk32", [128, 20, 64], F32) for _ in range(2)]
    sqs = [k.alloc("sq", [128, 20, 64], F32) for _ in range(2)]
    rst = [k.alloc("rst", [128, 64], F32) for _ in range(2)]
    tcs = [[k.alloc("tc", [128, 20, 32], F32) for _ in range(2)] for _ in range(2)]
    tss = [[k.alloc("ts", [128, 20, 32], F32) for _ in range(2)] for _ in range(2)]
    qr = [k.alloc("qr", [128, 20, 64], BF16) for _ in range(2)]
    qst = [k.alloc("qst", [128, 8, 512], BF16) for _ in range(2)]
    tiles = [("ctx", j) for j in range(NCT)] + [("x", i) for i in range(NXT)]

    def tile_gen(kind, ti, tn):
        isx = kind == "x"
        kt = ti if not isx else NCT + ti
        x_ = xt[tn % 2]
        A_, B_ = (Ax, Bx) if isx else (Ac, Bc)
        rows = slice(ti * 128, (ti + 1) * 128)
        k.dma("sync", x_, (dr["XR2"] if isx else dr["CR2"])[rows, :])
        yield from norm_mod_g(k, x_, A_, B_, hb[tn % 2], junks[tn % 2], st[tn % 2], t32s[tn % 2], ss_eng="scalar")
        hbank = k.banks[4 + tn % 2].bitcast(BF16)
        hT_ = hT[tn % 2]
        transpose_tile(k, hb[tn % 2], hT_, hbank, ident)
        yield
        sq = sqs[tn % 2]
        q32 = qk32[tn % 2]
        q32f = q32.rearrange("p a b -> p (a b)")
        if isx:
            for nbk in range(3):
                for kk in range(8):
                    k.mm(k.banks[nbk], hT_[:, kk, :], qkvw[kk][:, nbk * 512:(nbk + 1) * 512], kk == 0, kk == 7)
            k.act(q32f[:, 0:512], k.banks[0], AF.Copy)
            k.act(q32f[:, 512:1024], k.banks[1], AF.Copy)
            k.act(q32f[:, 1024:1280], k.banks[2][:, 0:256], AF.Copy)
            vsrc = k.banks[2][:, 256:512]
            h0, nh = 0, 20
        else:
            for kk in range(8):
                k.mm(k.banks[2], hT_[:, kk, :], qkvw[kk][:, 1024:1536], kk == 0, kk == 7)
            k.act(q32f[:, 1024:1280], k.banks[2][:, 0:256], AF.Copy)
            vsrc = k.banks[2][:, 256:512]
            h0, nh = 16, 4
        k.act(Vaug[:, kt * 4:kt * 4 + 4, 0:64], vsrc.rearrange("p (g d) -> p g d", g=4), AF.Copy)
        yield
        qh = q32[:, h0:h0 + nh, :]
        r_ = rst[tn % 2]
        k.vop("tensor_tensor", sq[:, h0:h0 + nh, :], qh, qh, op=ALU.mult)
        k.vop("tensor_reduce", r_[:, 0:nh], sq[:, h0:h0 + nh, :], axis=AX.X, op=ALU.add)
        k.vop("tensor_scalar", r_[:, 20:20 + nh], r_[:, 0:nh], 1.0 / 64, EPS, op0=ALU.mult, op1=ALU.add)
        k.act(r_[:, 40:40 + nh], r_[:, 20:20 + nh], AF.Sqrt)
        yield
        k.vop("reciprocal", r_[:, 0:nh], r_[:, 40:40 + nh])
        k.vop("tensor_tensor", qh, qh, bcast_last(r_[:, 0:nh], 64), op=ALU.mult)
        q_ = qr[tn % 2]
        if isx:
            k.vop("tensor_tensor", qh, qh, gq[:, h0:h0 + nh, :], op=ALU.mult)
            for rc in range(2):
                qv = apv(q32, [[64, 20], [16, 2], [1, 16]], extra_off=rc * 32)
                ov = apv(q_, [[64, 20], [16, 2], [1, 16]], extra_off=rc * 32)
                cosb = apv(rp[:, ti, rc * 32:rc * 32 + 16], [[0, 20], [0, 2], [1, 16]])
                sinb = apv(rp[:, ti, rc * 32 + 16:rc * 32 + 32], [[0, 20], [0, 2], [1, 16]])
                tcv = tcs[tn % 2][rc].rearrange("p h (t j) -> p h t j", t=2)
                tsv = tss[tn % 2][rc].rearrange("p h (t j) -> p h t j", t=2)
                k.vop("tensor_tensor", tcv, qv, cosb, op=ALU.mult)
                k.vop("tensor_tensor", tsv, qv, sinb, op=ALU.mult)
                k.vop("tensor_tensor", ov[:, :, 0, :], tcv[:, :, 0, :], tsv[:, :, 1, :], op=ALU.subtract)
                k.vop("tensor_tensor", ov[:, :, 1, :], tcv[:, :, 1, :], tsv[:, :, 0, :], op=ALU.add)
        else:
            k.vop("tensor_tensor", q_[:, h0:h0 + nh, :], qh, gq[:, h0:h0 + nh, :], op=ALU.mult)
        yield
        kbank = k.banks[3].bitcast(BF16)
        for m in range(2):
            k.transpose(kbank[:, m * 128:(m + 1) * 128], q_[:, 16 + 2 * m:18 + 2 * m, :].rearrange("p a b -> p (a b)"), ident)
        k.act(kT[:, :, kt * 128:(kt + 1) * 128], kbank[:, 0:256].rearrange("p (a b) -> p a b", a=2), AF.Copy)
        if isx:
            qbank = k.banks[6 + tn % 2].bitcast(BF16)
            qf = q_.rearrange("p a b -> p (a b)")
            for s_i in range(8):
                k.transpose(qbank[:, s_i * 128:(s_i + 1) * 128], qf[:, s_i * 128:(s_i + 1) * 128], ident)
            qs_ = qst[(ti // 4) % 2]
            k.vop("tensor_copy", qs_[:, :, (ti % 4) * 128:(ti % 4 + 1) * 128],
                  qbank.rearrange("p (a b) -> p a b", a=8))
            if ti % 4 == 3:
                t0 = (ti // 4) * 512
                k.dma("gpsimd", dr["QT"][:, :, t0:t0 + 512].rearrange("s p t -> p s t"), qs_)


    run_interleaved((tile_gen(kind, ti, n) for n, (kind, ti) in enumerate(tiles)), width=2)


def bank2(k, i):
    return V(k.psum[:, i:i + 2, :].rearrange("p a b -> p (a b)"), k.banks[i].bufs + k.banks[i + 1].bufs)


def phase_attn_core(k, dr):
    k.reset(keep=ATT["keep"])
    kT, Vaug = ATT["kT"], ATT["Vaug"]
    wv = dr["attn_o_w"][0].rearrange("(k p) n -> p k n", p=128)
    ow = []
    for kk in range(8):
        w = k.alloc(f"aow{kk}", [128, D], BF16)
        k.dma("gpsimd", w, wv[:, kk, :], max_dma_last_dim=4096)
        ow.append(w)
    G = k.alloc("G", [128, D], F32)
    load_bc(k, "sync", G, dr["mods_d"][0, 1, 2 * D:3 * D])
    qz = [k.alloc("qz", [128, 16, 512], BF16) for _ in range(2)]
    for q_ in qz:
        k.vop("memset", q_, 0.0)
    aT = [k.alloc("aT", [128, 8, 512], BF16) for _ in range(2)]
    NP = 4
    pT = [k.alloc("pT", [128, 1024], BF16) for _ in range(NP)]
    rl = [k.alloc("rl", [128, 512], F32) for _ in range(2)]
    rl0 = [k.alloc("rl0", [128, 512], F32) for _ in range(2)]
    osb = [k.alloc("osb", [128, 512], F32) for _ in range(2)]
    xt = [k.alloc("xt", [128, D], F32) for _ in range(3)]
    tmp = k.alloc("tmp", [128, D], F32)
    sb2 = [bank2(k, 2), bank2(k, 4), bank2(k, 6)]
    LOOK = 2
    nu = 0
    nr = 0
    tn = 0
    pending = []
    tnc = {"n": 0}

    def flush_oproj():
        while pending:
            qb_, a_o = pending.pop(0)
            for j in range(4):
                ti = qb_ * 4 + j
                x_ = xt[tnc["n"] % 3]
                tnc["n"] += 1
                rows = slice(ti * 128, (ti + 1) * 128)
                k.dma("sync", x_, dr["XR2"][rows, :])
                for half in range(2):
                    cs = slice(half * 512, (half + 1) * 512)
                    ps = k.banks[2 + half]
                    for c in range(8):
                        k.mm(ps, a_o[:, c, j * 128:(j + 1) * 128], ow[c][:, cs], c == 0, c == 7)
                    k.vop("tensor_tensor", tmp[:, cs], ps, G[:, cs], op=ALU.mult)
                    k.vop("tensor_tensor", x_[:, cs], x_[:, cs], tmp[:, cs], op=ALU.add)
                k.dma("gpsimd", dr["XR3"][rows, :], x_)

    for qb in range(T // 512):
        q_ = qz[qb % 2]
        a_ = aT[qb % 2]
        qsl = slice(qb * 512, (qb + 1) * 512)
        for G_ in range(2):
            k.dma("sync", q_[0:64, 8 * G_:8 * G_ + 4, :],
                  dr["QT"][4 * G_:4 * G_ + 4, 0:64, qsl].rearrange("s p t -> p s t"))
            k.dma("sync", q_[64:128, 8 * G_ + 4:8 * G_ + 8, :],
                  dr["QT"][4 * G_:4 * G_ + 4, 64:128, qsl].rearrange("s p t -> p s t"))
        for g in range(4):
            for pr in range(2):
                if g == 0 and pr == 1:
                    flush_oproj()
                heads = [4 * g + 2 * pr, 4 * g + 2 * pr + 1]
                sbk = {}

                def emit_S(n):
                    bank = sb2[(nu + n) % 3]
                    sbk[n] = bank
                    for i, h in enumerate(heads):
                        k.mm(bank[:, i * 512:(i + 1) * 512], kT[:, g // 2, n * 128:(n + 1) * 128],
                             q_[:, h, :], True, True)

                def emit_PV(n):
                    p_ = pT[(nu + n) % NP]
                    k.act(p_, sbk[n], AF.Exp, scale=0.125)
                    for i in range(2):
                        k.mm(k.banks[i], Vaug[:, n * 4 + g, :], p_[:, i * 512:(i + 1) * 512],
                             n == 0, n == NKT - 1)

                for n in range(LOOK):
                    emit_S(n)
                for n in range(NKT):
                    if n + LOOK < NKT:
                        emit_S(n + LOOK)
                    emit_PV(n)
                nu += NKT
                for i, h in enumerate(heads):
                    k.vop("tensor_copy", osb[i], k.banks[i])
                for i, h in enumerate(heads):
                    r_ = rl[nr % 2]
                    r0 = rl0[nr % 2]
                    nr += 1
                    ov_ = osb[i]
                    k.vop("reciprocal", r_[64:128, :], ov_[64:128, :])
                    k.vop("tensor_copy", r0[0:64, :], r_[64:128, :])
                    dp = (h % 2) * 64
                    k.vop("tensor_tensor", a_[dp:dp + 64, h // 2, :], ov_[0:64, :], r0[0:64, :], op=ALU.mult)
        pending.append((qb, a_))
    flush_oproj()


PHASES = [("adaln", phase_adaln), ("lru_in", phase_lru_in), ("lru_scan", phase_lru_scan),
          ("lru_out", phase_lru_out), ("moe0", phase_moe0),
          ("attn_proj", phase_attn_proj), ("attn_core", phase_attn_core), ("moe1", phase_moe1)]


IN_SPECS = {
    "x": [T, D], "c": [1, D], "ctx": [C, D], "c_ctx": [D],
    "ada_w": [2, D, 6 * D], "ada_b": [2, 6 * D], "norm_mix_w": [2, D], "norm_ffn_w": [2, D],
    "lru_in_w": [1, D, 2 * D], "lru_conv_w": [1, 4, D], "lru_conv_b": [1, D],
    "lru_gate_a_w": [1, 2, 8, 128, 128], "lru_gate_a_b": [1, 2, D],
    "lru_gate_x_w": [1, 2, 8, 128, 128], "lru_gate_x_b": [1, 2, D],
    "lru_lambda": [1, 2, D], "lru_out_w": [1, D, D],
    "attn_qkv_w": [1, D, 1536], "attn_q_norm_w": [1, 64], "attn_k_norm_w": [1, 64],
    "attn_o_w": [1, D, D], "router_w": [D, NE], "router_b": [NE],
    "moe_w1": [2, NE, D, FF], "moe_w3": [2, NE, D, FF], "moe_w2": [2, NE, FF, D],
    "ident": [128, 128], "rope": [T, 64], "ltri": [128, 128], "idxA": [128, 32], "idxB": [128, 16],
}
SCRATCH_SPECS = {
    "mods_d": ([2, 2, 6 * D], F32),
    "GG": ([8, 128, TS], BF16), "UU": ([8, 128, TS], F32), "ZZ": ([8, 128, TS], BF16),
    "XR1": ([T, D], F32), "CR1": ([C, D], F32), "XR2": ([T, D], F32), "CR2": ([C, D], F32),
    "XR3": ([T, D], F32), "QT": ([8, 128, T], BF16),
    "Hs": ([NSLOT, D], BF16), "Ys": ([NSLOT, D], F32), "Gs": ([NSLOT, 16], F32),
}


class DR(dict):
    def __init__(self, nc, dbg):
        super().__init__()
        self.nc = nc
        self.dbg = dbg
        self.used_inputs = []

    def __missing__(self, name):
        if not self.dbg:
            if name in ("XR1", "XR2", "XR3"):
                return self["out"]
            if name == "CR2":
                return self["CR1"]
            if name == "ZZ":
                return self["GG"]
            if name == "QT":
                return self["GG"][:, :, 0:T]
        if name in IN_SPECS:
            ap = self.nc.dram_tensor(name, list(IN_SPECS[name]), F32, kind="ExternalInput").ap()
            self.used_inputs.append(name)
        else:
            shape, dt = SCRATCH_SPECS[name]
            kind = "ExternalOutput" if self.dbg else "Internal"
            ap = self.nc.dram_tensor(name, list(shape), dt, kind=kind).ap()
        self[name] = ap
        return ap


def build(stop_after=None, dbg=False):
    nc = bass.Bass("TRN2", target_bir_lowering=False)
    dr = DR(nc, dbg)
    dr["out"] = nc.dram_tensor("out", [T, D], F32, kind="ExternalOutput").ap()
    with ExitStack() as stack:
        arena_bytes = 204 * 1024
        arena_t = stack.enter_context(nc.sbuf_tensor("arena", [128, arena_bytes], U8))
        pt = stack.enter_context(nc.psum_tensor("psum", [128, 8, 512], F32))
        banks = [V(pt[:, i, :], (Buf(f"bank{i}", excl=True),)) for i in range(8)]
        P = Prog(nc)
        k = K(nc, P, arena_t[:], arena_bytes, banks)
        k.psum = pt
        import os
        only = os.environ.get("PHASE_ONLY", "")
        for name, fn in PHASES:
            if only and name not in only.split(","):
                continue
            fn(k, dr)
            if stop_after == name:
                break
        P.barrier()
        P.add("sync", lambda h: h.nop(), dma=False)
        P.emit(stack)
    nc._used_inputs = list(dr.used_inputs)
    return nc


def make_in_maps(inputs, used=None):
    f = lambda a: np.ascontiguousarray(np.asarray(a, dtype=np.float32))
    shared = {n: f(inputs[n]) for n in (
        "c_ctx", "ada_w", "ada_b", "norm_mix_w", "norm_ffn_w", "lru_in_w", "lru_conv_w", "lru_conv_b",
        "lru_gate_a_w", "lru_gate_a_b", "lru_gate_x_w", "lru_gate_x_b", "lru_lambda", "lru_out_w",
        "attn_qkv_w", "attn_q_norm_w", "attn_k_norm_w", "attn_o_w", "router_w", "router_b",
        "moe_w1", "moe_w3", "moe_w2")}
    shared["ident"] = np.eye(128, dtype=np.float32)
    shared["ltri"] = np.triu(np.ones((128, 128), dtype=np.float32), k=1)
    p_ = np.arange(128, dtype=np.float32)[:, None, None]
    ia = np.zeros((128, 32), dtype=np.float32)
    ia[:, 0:4] = np.arange(4, dtype=np.float32)[None, :] * 128 + np.arange(128, dtype=np.float32)[:, None]
    shared["idxA"] = ia
    shared["idxB"] = np.ascontiguousarray((np.arange(4, dtype=np.float32)[None, :, None] * 512
                                           + np.arange(4, dtype=np.float32)[None, None, :] * 128 + p_).reshape(128, 16))
    inv = (np.float32(10000.0) ** (-np.arange(16, dtype=np.float32) / np.float32(16))).astype(np.float32)
    t = np.arange(T)
    pr = (t // 64).astype(np.float32)[:, None] * inv
    pc = (t % 64).astype(np.float32)[:, None] * inv
    rope = np.concatenate([np.cos(pr), np.sin(pr), np.cos(pc), np.sin(pc)], axis=1).astype(np.float32)
    shared["rope"] = np.ascontiguousarray(rope)
    x = f(inputs["x"]); c = f(inputs["c"]); ctx = f(inputs["ctx"])
    maps = []
    for b in range(8):
        m = dict(shared)
        m["x"] = x[b]; m["c"] = c[b:b + 1]; m["ctx"] = ctx[b]
        if used is not None:
            m = {n: v for n, v in m.items() if n in used}
        maps.append(m)
    return maps


def kernel(**inputs):
    nc = build()
    res = run_bass_kernel_spmd(nc, make_in_maps(inputs, nc._used_inputs), core_ids=list(range(8)))
    return np.stack([r["out"] for r in res.results], axis=0).astype(np.float32)
```

```python
import numpy as np
from contextlib import ExitStack
import concourse.bass as bass
import concourse.mybir as mybir
from concourse.bass_utils import run_bass_kernel_spmd

F32 = mybir.dt.float32
BF16 = mybir.dt.bfloat16
U8 = mybir.dt.uint8
AF = mybir.ActivationFunctionType
ALU = mybir.AluOpType
AX = mybir.AxisListType

D = 1024
T = 4096
C = 256
TS = T + C
NXT = T // 128
NCT = C // 128
NE = 16
FF = 512
EPS = 1e-6
ENGS = ("sync", "gpsimd", "scalar", "vector", "tensor")
NRING = {"sync": 28, "gpsimd": 24, "scalar": 8}


class Buf:
    __slots__ = ("name", "w", "r", "excl")

    def __init__(self, name="", excl=False):
        self.name = name
        self.w = None
        self.r = {}
        self.excl = excl


class Op:
    __slots__ = ("eng", "fn", "deps", "needed", "sig", "dma", "ring", "val", "prev")


class V:
    __slots__ = ("ap", "bufs")

    def __init__(self, ap, bufs):
        self.ap = ap
        self.bufs = tuple(bufs)

    def __getitem__(self, k):
        return V(self.ap[k], self.bufs)

    def rearrange(self, s, **kw):
        return V(self.ap.rearrange(s, **kw), self.bufs)

    def bitcast(self, dt):
        return V(self.ap.bitcast(dt), self.bufs)

    def with_bufs(self, *bufs):
        return V(self.ap, bufs)

    @property
    def shape(self):
        return self.ap.shape


def _ap(x):
    return x.ap if isinstance(x, V) else x


class Prog:
    def __init__(self, nc):
        self.nc = nc
        self.ops = {e: [] for e in ENGS}
        self.last_compute = {e: None for e in ENGS}
        self.dma_since = []
        self.pending = {e: None for e in ENGS}
        self.ring_pos = {e: 0 for e in ENGS}
        self.ring_last = {}
        self.ring_cnt = {}

    def add(self, eng, fn, reads=(), writes=(), dma=False):
        op = Op()
        op.eng = eng
        op.fn = fn
        op.dma = dma
        op.needed = False
        op.sig = 0
        op.deps = {}
        op.prev = None
        for b in reads:
            if b.w is not None:
                op.deps[b.w] = "raw"
            if b.excl:
                for key, o in b.r.items():
                    if key != eng:
                        op.deps.setdefault(o, "raw")
        for b in writes:
            if b.w is not None:
                op.deps.setdefault(b.w, "waw")
            for o in b.r.values():
                if o is not op:
                    op.deps.setdefault(o, "war")
        pend = self.pending[eng]
        if pend:
            for o in pend:
                op.deps[o] = "raw"
            self.pending[eng] = None
        for b in writes:
            b.w = op
            b.r = {}
        for b in reads:
            b.r[("dma", id(op)) if dma else eng] = op
        if dma:
            n = NRING[eng]
            key = (eng, self.ring_pos[eng] % n)
            self.ring_pos[eng] += 1
            op.ring = key
            self.ring_cnt[key] = self.ring_cnt.get(key, 0) + 1
            op.val = 16 * self.ring_cnt[key]
            op.prev = self.ring_last.get(key)
            self.ring_last[key] = op
            self.dma_since.append(op)
        else:
            self.last_compute[eng] = op
        self.ops[eng].append(op)
        return op

    def barrier(self):
        deps = [o for o in self.last_compute.values() if o is not None]
        latest = {}
        for o in self.dma_since:
            latest[o.ring] = o
        deps += list(latest.values())
        self.dma_since = list(latest.values())
        for e in ENGS:
            cur = self.pending[e] or []
            self.pending[e] = list(cur) + deps

    @staticmethod
    def _keep(op, d, kind):
        if d.dma:
            return True
        if d.eng != op.eng:
            return True
        if op.dma:
            return True
        if op.eng == "tensor":
            return False
        return True

    def emit(self, stack):
        nc = self.nc
        for e in ENGS:
            for op in self.ops[e]:
                for d, kind in op.deps.items():
                    if (not d.dma) and self._keep(op, d, kind):
                        d.needed = True
        for e in ENGS:
            cnt = 0
            for op in self.ops[e]:
                if (not op.dma) and op.needed:
                    cnt += 1
                    op.sig = cnt
        esem = {e: stack.enter_context(nc.semaphore("es_" + e)) for e in ENGS if e != "sync"}
        rsem = {}
        for e, n in NRING.items():
            for i in range(n):
                rsem[(e, i)] = stack.enter_context(nc.semaphore(f"rs_{e}_{i}"))
        block = stack.enter_context(nc.Block())
        prog = self

        def run_engine(e, h):
            waited = {}

            def wait(sem_key, sem, val):
                if waited.get(sem_key, 0) < val:
                    h.wait_ge(sem, val)
                    waited[sem_key] = val

            for op in prog.ops[e]:
                for d, kind in op.deps.items():
                    if not prog._keep(op, d, kind):
                        continue
                    if d.dma:
                        wait(d.ring, rsem[d.ring], d.val)
                    else:
                        wait(d.eng, esem[d.eng], d.sig)
                if op.dma and op.prev is not None:
                    wait(op.ring, rsem[op.ring], op.prev.val)
                ins = op.fn(h)
                if op.dma:
                    ins.then_inc(rsem[op.ring], 16)
                elif op.needed:
                    ins.then_inc(esem[e], 1)

        @block.sync
        def _(h):
            run_engine("sync", h)

        @block.gpsimd
        def _(h):
            run_engine("gpsimd", h)

        @block.scalar
        def _(h):
            run_engine("scalar", h)

        @block.vector
        def _(h):
            run_engine("vector", h)

        @block.tensor
        def _(h):
            run_engine("tensor", h)


def _bufs(*xs):
    out = []
    for x in xs:
        if isinstance(x, V):
            out.extend(x.bufs)
    return out


class K:
    def __init__(self, nc, P, arena, arena_bytes, banks):
        self.nc = nc
        self.P = P
        self.arena = arena
        self.arena_bytes = arena_bytes
        self.off = 0
        self.banks = banks
        self.uid = 0

    def reset(self, keep=0):
        self.P.barrier()
        self.off = keep

    def alloc(self, name, shape, dtype, npart=128):
        esz = {F32: 4, BF16: 2, U8: 1, mybir.dt.uint32: 4, mybir.dt.int32: 4}[dtype]
        n = 1
        for s in shape[1:]:
            n *= s
        nbytes = (n * esz + 63) // 64 * 64
        assert self.off + nbytes <= self.arena_bytes, (name, self.off, nbytes, self.arena_bytes)
        ap = self.arena[0:shape[0], self.off:self.off + n * esz].bitcast(dtype)
        self.off += nbytes
        if len(shape) == 3:
            ap = ap.rearrange("p (a b) -> p a b", a=shape[1])
        elif len(shape) == 4:
            ap = ap.rearrange("p (a b c) -> p a b c", a=shape[1], b=shape[2])
        self.uid += 1
        return V(ap, (Buf(f"{name}_{self.uid}"),))

    def dma(self, q, out, in_, **kw):
        o, i = _ap(out), _ap(in_)
        return self.P.add(q, lambda h: h.dma_start(out=o, in_=i, **kw),
                          reads=_bufs(in_), writes=_bufs(out), dma=True)

    def mm(self, out, lhsT, rhs, start, stop, extra_reads=()):
        o, l, r = _ap(out), _ap(lhsT), _ap(rhs)
        return self.P.add("tensor", lambda h: h.matmul(o, l, r, start=start, stop=stop),
                          reads=_bufs(lhsT, rhs) + list(extra_reads), writes=_bufs(out))

    def transpose(self, out, in_, ident):
        o, i, d = _ap(out), _ap(in_), _ap(ident)
        return self.P.add("tensor", lambda h: h.transpose(o, i, d),
                          reads=_bufs(in_, ident), writes=_bufs(out))

    def act(self, out, in_, func, bias=None, scale=1.0, accum_out=None, eng="scalar"):
        o, i = _ap(out), _ap(in_)
        kw = {}
        if bias is not None:
            kw["bias"] = _ap(bias)
        if accum_out is not None:
            kw["accum_out"] = _ap(accum_out)
        sc = _ap(scale)
        return self.P.add(eng, lambda h: h.activation(out=o, in_=i, func=func, scale=sc, **kw),
                          reads=_bufs(in_, bias, scale), writes=_bufs(out, accum_out))

    def vop(self, name, out, *ins, eng="vector", accum_out=None, **kw):
        o = _ap(out)
        args = [_ap(x) for x in ins]
        kws = {k: _ap(v) for k, v in kw.items()}
        if accum_out is not None:
            kws["accum_out"] = _ap(accum_out)
        return self.P.add(eng, lambda h: getattr(h, name)(o, *args, **kws),
                          reads=_bufs(*ins, *kw.values()), writes=_bufs(out, accum_out))


def run_interleaved(gens, width=2):
    it = iter(gens)
    active = []
    done = False
    while True:
        while not done and len(active) < width:
            g = next(it, None)
            if g is None:
                done = True
                break
            active.append(g)
        if not active:
            break
        for g in list(active):
            try:
                next(g)
            except StopIteration:
                active.remove(g)


def load_bc(k, q, dst, src_ap):
    return k.dma(q, dst, src_ap.partition_broadcast(128))


def load_mod_tiles(k, dr, layer, src, which, norm_w_ap, need_gate=True):
    base = 0 if which == "m" else 3
    mods = dr["mods_d"]
    A = k.alloc("A", [128, D], F32)
    B = k.alloc("B", [128, D], F32)
    tmp = k.alloc("gn", [128, D], F32)
    load_bc(k, "sync", B, mods[src, layer, (base + 0) * D:(base + 1) * D])
    load_bc(k, "sync", A, mods[src, layer, (base + 1) * D:(base + 2) * D])
    load_bc(k, "sync", tmp, norm_w_ap)
    k.vop("scalar_tensor_tensor", A, A, 1.0, tmp, op0=ALU.add, op1=ALU.mult)
    G = None
    if need_gate:
        G = k.alloc("G", [128, D], F32)
        load_bc(k, "sync", G, mods[src, layer, (base + 2) * D:(base + 3) * D])
    return A, B, G


def norm_mod(k, xt, A, B, h_out, junk, st, t32, ss_eng="vector"):
    ss, ms, sq, rs = st[:, 0:1], st[:, 1:2], st[:, 2:3], st[:, 3:4]
    if ss_eng == "vector":
        k.vop("scalar_tensor_tensor", junk, xt, 1.0, xt, op0=ALU.mult, op1=ALU.mult, accum_out=ss)
    else:
        k.act(junk, xt, AF.Square, accum_out=ss)
    k.vop("tensor_scalar", ms, ss, 1.0 / D, EPS, op0=ALU.mult, op1=ALU.add)
    k.act(sq, ms, AF.Sqrt)
    k.vop("reciprocal", rs, sq)
    k.vop("scalar_tensor_tensor", t32, xt, rs, A, op0=ALU.mult, op1=ALU.mult)
    k.vop("tensor_tensor", h_out, t32, B, op=ALU.add)


def norm_mod_g(k, xt, A, B, h_out, junk, st, t32, ss_eng="vector"):
    ss, ms, sq, rs = st[:, 0:1], st[:, 1:2], st[:, 2:3], st[:, 3:4]
    if ss_eng == "vector":
        k.vop("scalar_tensor_tensor", junk, xt, 1.0, xt, op0=ALU.mult, op1=ALU.mult, accum_out=ss)
    else:
        k.act(junk, xt, AF.Square, accum_out=ss)
    yield
    k.vop("tensor_scalar", ms, ss, 1.0 / D, EPS, op0=ALU.mult, op1=ALU.add)
    k.act(sq, ms, AF.Sqrt)
    yield
    k.vop("reciprocal", rs, sq)
    k.vop("scalar_tensor_tensor", t32, xt, rs, A, op0=ALU.mult, op1=ALU.mult)
    k.vop("tensor_tensor", h_out, t32, B, op=ALU.add)
    yield


def transpose_tile(k, h, dst, bank_bf, ident, evac_eng="scalar"):
    for c in range(8):
        k.transpose(bank_bf[:, c * 128:(c + 1) * 128], h[:, c * 128:(c + 1) * 128], ident)
    src = bank_bf.rearrange("p (a b) -> p a b", a=8)
    if evac_eng == "scalar":
        k.act(dst, src, AF.Copy)
    else:
        k.vop("tensor_copy", dst, src)


def phase_adaln(k, dr):
    k.reset()
    cT = k.alloc("cT", [128, 2, 8], F32)
    sT = k.alloc("sT", [128, 2, 8], F32)
    wt = [k.alloc(f"adaw{i}", [128, 8, 512], F32) for i in range(3)]
    bias = k.alloc("adab", [2, 2, 6 * D], F32)
    mods = k.alloc("mods", [2, 2, 6 * D], F32)
    k.dma("sync", cT[:, 0, :], dr["c"][0].rearrange("(p k) -> p k", k=8))
    k.dma("sync", cT[:, 1, :], dr["c_ctx"].rearrange("(p k) -> p k", k=8))
    for j in range(2):
        k.dma("sync", bias[j:j + 1, :, :], dr["ada_b"].rearrange("(o l) n -> o l n", o=1))
    k.act(sT, cT, AF.Silu)
    n = 0
    for layer in range(2):
        wv = dr["ada_w"][layer].rearrange("(p k) n -> p k n", k=8)
        for nchunk in range(12):
            w = wt[n % 3]
            k.dma("sync" if n % 2 == 0 else "scalar", w, wv[:, :, nchunk * 512:(nchunk + 1) * 512])
            ps = k.banks[n % 2]
            for kk in range(8):
                k.mm(ps[0:2, :], sT[:, :, kk], w[:, kk, :], kk == 0, kk == 7)
            sl = slice(nchunk * 512, (nchunk + 1) * 512)
            k.vop("tensor_tensor", mods[0:2, layer, sl], ps[0:2, :], bias[0:2, layer, sl], op=ALU.add)
            n += 1
    k.dma("sync", dr["mods_d"], mods)


GELU = AF.Gelu_apprx_tanh
LRU_BLOCKS = [("ctx", 0, 2, 0)] + [("x", 4 * i, 4, C + 512 * i) for i in range(8)]


def slow_dma(k, q, out, in_):
    return k.dma(q, out, in_, allow_slow_non_contiguous=True)


def phase_lru_in(k, dr):
    k.reset()
    ident = k.alloc("ident", [128, 128], BF16)
    k.dma("gpsimd", ident, dr["ident"])
    wv = dr["lru_in_w"][0].rearrange("(k p) n -> p k n", p=128)
    in_w = []
    for kk in range(8):
        w = k.alloc(f"in_w{kk}", [128, 2048], BF16)
        k.dma("gpsimd", w, wv[:, kk, :], max_dma_last_dim=4096)
        in_w.append(w)
    Ax, Bx, _ = load_mod_tiles(k, dr, 0, 0, "m", dr["norm_mix_w"][0], need_gate=False)
    Ac, Bc, _ = load_mod_tiles(k, dr, 0, 1, "m", dr["norm_mix_w"][0], need_gate=False)
    xt = [k.alloc("xt", [128, D], F32) for _ in range(3)]
    junk = k.alloc("junk", [128, D], BF16)
    t32s = [k.alloc("t32", [128, D], F32) for _ in range(2)]
    hb = [[k.alloc("hb", [128, D], BF16) for _ in range(4)] for _ in range(2)]
    st = [k.alloc("st", [128, 4], F32) for _ in range(4)]
    hT = [k.alloc("hT", [128, 8, 512], BF16) for _ in range(2)]
    gg = [k.alloc("gg", [128, 8, 512], BF16) for _ in range(2)]
    uu = [k.alloc("uu", [128, 8, 512], F32) for _ in range(2)]
    bankbf = [k.banks[6].bitcast(BF16), k.banks[7].bitcast(BF16)]
    cnt = {"tn": 0, "tp": 0}

    def norms(bi):
        src, t0, nt, tok0 = LRU_BLOCKS[bi]
        srcap = dr["ctx"] if src == "ctx" else dr["x"]
        A_, B_ = (Ac, Bc) if src == "ctx" else (Ax, Bx)
        for j in range(nt):
            tn = cnt["tn"]
            cnt["tn"] += 1
            x_ = xt[tn % 3]
            k.dma("sync", x_, srcap[(t0 + j) * 128:(t0 + j + 1) * 128, :])
            norm_mod(k, x_, A_, B_, hb[bi % 2][j], junk, st[tn % 4], t32s[tn % 2], ss_eng="scalar")

    def transposes(bi):
        src, t0, nt, tok0 = LRU_BLOCKS[bi]
        for j in range(nt):
            tp = cnt["tp"]
            cnt["tp"] += 1
            transpose_tile(k, hb[bi % 2][j], hT[bi % 2][:, :, j * 128:(j + 1) * 128], bankbf[tp % 2], ident)

    norms(0)
    transposes(0)
    for bi, (src, t0, nt, tok0) in enumerate(LRU_BLOCKS):
        hTb = hT[bi % 2]
        if bi + 1 < len(LRU_BLOCKS):
            norms(bi + 1)
        ntok = nt * 128
        for oc in range(16):
            ps = k.banks[oc % 6]
            for kk in range(8):
                k.mm(ps[:, :ntok], in_w[kk][:, oc * 128:(oc + 1) * 128], hTb[:, kk, :ntok], kk == 0, kk == 7)
            if oc < 8:
                k.act(gg[bi % 2][:, oc, :ntok], ps[:, :ntok], GELU)
            else:
                k.vop("tensor_copy", uu[bi % 2][:, oc - 8, :ntok], ps[:, :ntok])
        if bi + 1 < len(LRU_BLOCKS):
            transposes(bi + 1)
        k.dma("gpsimd", dr["GG"][:, :, tok0:tok0 + ntok].rearrange("c p t -> p c t"), gg[bi % 2][:, :, :ntok])
        k.dma("gpsimd", dr["UU"][:, :, tok0:tok0 + ntok].rearrange("c p t -> p c t"), uu[bi % 2][:, :, :ntok])


def phase_lru_scan(k, dr):
    k.reset()
    cw = k.alloc("cw", [128, 4, 8], F32)
    cb = k.alloc("cb", [128, 8], F32)
    gab = k.alloc("gab", [128, 2, 8], F32)
    gxb = k.alloc("gxb", [128, 2, 8], F32)
    lam = k.alloc("lam", [128, 2, 8], F32)
    cp = k.alloc("cp", [128, 2, 8], F32)
    for tap in range(4):
        slow_dma(k, "sync", cw[:, tap, :], dr["lru_conv_w"][0, tap].rearrange("(c p) -> p c", p=128))
    slow_dma(k, "sync", cb, dr["lru_conv_b"][0].rearrange("(c p) -> p c", p=128))
    for d in range(2):
        slow_dma(k, "sync", gab[:, d, :], dr["lru_gate_a_b"][0, d].rearrange("(c p) -> p c", p=128))
        slow_dma(k, "sync", gxb[:, d, :], dr["lru_gate_x_b"][0, d].rearrange("(c p) -> p c", p=128))
        slow_dma(k, "sync", lam[:, d, :], dr["lru_lambda"][0, d].rearrange("(c p) -> p c", p=128))
    k.act(cp, lam, AF.Exp, scale=-1.0)
    k.act(cp, cp, AF.Ln, bias=1.0)
    k.vop("tensor_scalar", cp, cp, -8.0, None, op0=ALU.mult)
    gw = k.alloc("gw", [128, 32, 128], BF16)
    k.dma("gpsimd", gw[:, 0:16, :], dr["lru_gate_a_w"][0].rearrange("d n i e -> i (d n) e"))
    k.dma("gpsimd", gw[:, 16:32, :], dr["lru_gate_x_w"][0].rearrange("d n i e -> i (d n) e"))
    XB = 259
    up = [k.alloc("up", [128, TS + 6], F32) for _ in range(1)]
    for u_ in up:
        k.vop("memset", u_, 0.0)
    uconv = k.alloc("uconv", [128, TS], F32)
    ub = k.alloc("ub", [128, TS], BF16)
    rrs = [k.alloc("rr", [128, TS], F32) for _ in range(2)]
    igs = [k.alloc("ig", [128, TS], F32) for _ in range(2)]
    sbs = [k.alloc("sb", [128, TS], F32) for _ in range(2)]
    y = k.alloc("y", [128, TS], F32)
    ggc = [k.alloc("ggc", [128, TS], BF16) for _ in range(1)]
    z = [k.alloc("z", [128, TS], BF16) for _ in range(1)]
    pieces = [(0, 256)] + [(C + 512 * j, 512) for j in range(8)]
    segs = [(2, 0, C), (XB + 2, C, T)]
    nb = 0
    for c in range(8):
        u_ = up[0]
        k.dma("sync", u_[:, 2:2 + C], dr["UU"][c, :, 0:C])
        k.dma("sync", u_[:, XB + 2:XB + 2 + T], dr["UU"][c, :, C:TS])
        k.dma("sync", ggc[0], dr["GG"][c])
        for (sbase, dbase, L) in segs:
            dst = uconv[:, dbase:dbase + L]
            k.vop("tensor_scalar", dst, u_[:, sbase - 2:sbase - 2 + L], cw[:, 0, c:c + 1], cb[:, c:c + 1],
                  op0=ALU.mult, op1=ALU.add)
            for tap in range(1, 4):
                k.vop("scalar_tensor_tensor", dst, u_[:, sbase - 2 + tap:sbase - 2 + tap + L],
                      cw[:, tap, c:c + 1], dst, op0=ALU.mult, op1=ALU.add)
        k.act(ub, uconv, AF.Copy)
        def dir_gen(d, c=c):
            nonlocal nb
            rr, ig, sb = rrs[d], igs[d], sbs[d]
            for pi, (p0, pl) in enumerate(pieces):
                psr = k.banks[nb % 6]
                psi = k.banks[(nb + 1) % 6]
                nb += 2
                k.mm(psr[:, :pl], gw[:, d * 8 + c, :], ub[:, p0:p0 + pl], True, True)
                k.mm(psi[:, :pl], gw[:, 16 + d * 8 + c, :], ub[:, p0:p0 + pl], True, True)
                k.act(rr[:, p0:p0 + pl], psr[:, :pl], AF.Sigmoid, bias=gab[:, d, c:c + 1])
                k.act(ig[:, p0:p0 + pl], psi[:, :pl], AF.Sigmoid, bias=gxb[:, d, c:c + 1])
                if pi % 3 == 2:
                    yield
            yield
            k.act(rr, rr, AF.Exp, scale=cp[:, d, c:c + 1])
            k.vop("tensor_tensor", ig, ig, uconv, op=ALU.mult, eng="gpsimd")
            yield
            k.act(sb, rr, AF.Square)
            yield
            k.act(sb, sb, AF.Sqrt, bias=1.0, scale=-1.0)
            yield
            k.vop("tensor_tensor", ig, ig, sb, op=ALU.mult)
            yield
            hd = y if d == 0 else sb
            if d == 0:
                k.vop("tensor_tensor_scan", hd[:, 0:C], rr[:, 0:C], ig[:, 0:C], 0.0, op0=ALU.mult, op1=ALU.add)
                yield
                k.vop("tensor_tensor_scan", hd[:, C:TS], rr[:, C:TS], ig[:, C:TS], hd[:, C - 1:C],
                      op0=ALU.mult, op1=ALU.add)
            else:
                k.vop("tensor_tensor_scan", hd[:, 0:C][:, ::-1], rr[:, 0:C][:, ::-1], ig[:, 0:C][:, ::-1], 0.0,
                      op0=ALU.mult, op1=ALU.add)
                yield
                k.vop("tensor_tensor_scan", hd[:, C:TS][:, ::-1], rr[:, C:TS][:, ::-1], ig[:, C:TS][:, ::-1],
                      hd[:, 0:1], op0=ALU.mult, op1=ALU.add)

        run_interleaved([dir_gen(0), dir_gen(1)], width=2)
        k.vop("tensor_tensor", y, y, sbs[1], op=ALU.add)
        k.vop("tensor_tensor", z[0], y, ggc[0], op=ALU.mult)
        k.dma("gpsimd", dr["ZZ"][c], z[0])


def phase_lru_out(k, dr):
    k.reset()
    wv = dr["lru_out_w"][0].rearrange("(k p) n -> p k n", p=128)
    ow = []
    for kk in range(8):
        w = k.alloc(f"ow{kk}", [128, D], BF16)
        k.dma("gpsimd", w, wv[:, kk, :], max_dma_last_dim=4096)
        ow.append(w)
    Gx = k.alloc("Gx", [128, D], F32)
    Gc = k.alloc("Gc", [128, D], F32)
    load_bc(k, "sync", Gx, dr["mods_d"][0, 0, 2 * D:3 * D])
    load_bc(k, "sync", Gc, dr["mods_d"][1, 0, 2 * D:3 * D])
    zb = [k.alloc("zb", [128, 8, 512], BF16) for _ in range(2)]
    xt = [k.alloc("xt", [128, D], F32) for _ in range(3)]
    tmp = k.alloc("tmp", [128, D], F32)
    tn = 0
    nb = 0
    for bi, (src, t0, nt, tok0) in enumerate(LRU_BLOCKS):
        ntok = nt * 128
        zt = zb[bi % 2]
        k.dma("sync", zt[:, :, :ntok], dr["ZZ"][:, :, tok0:tok0 + ntok].rearrange("c p t -> p c t"))
        srcap = dr["ctx"] if src == "ctx" else dr["x"]
        dstap = dr["CR1"] if src == "ctx" else dr["XR1"]
        G = Gc if src == "ctx" else Gx
        for j in range(nt):
            x_ = xt[tn % 3]
            tn += 1
            rows = slice((t0 + j) * 128, (t0 + j + 1) * 128)
            k.dma("sync", x_, srcap[rows, :])
            for half in range(2):
                cs = slice(half * 512, (half + 1) * 512)
                ps = k.banks[nb % 4]
                nb += 1
                for kk in range(8):
                    k.mm(ps, zt[:, kk, j * 128:(j + 1) * 128], ow[kk][:, cs], kk == 0, kk == 7)
                k.vop("tensor_tensor", tmp[:, cs], ps, G[:, cs], op=ALU.mult)
                k.vop("tensor_tensor", x_[:, cs], x_[:, cs], tmp[:, cs], op=ALU.add)
            k.dma("gpsimd", dstap[rows, :], x_)


def bcast_last(v, n):
    a = v.ap
    dims = [list(d) for d in a.ap]
    return V(bass.AP(a.tensor, a.offset, dims + [[0, n]]), v.bufs)


def phase_moe(k, dr, layer, srcs, dsts, blocks):
    import os
    dbg_mode = os.environ.get("MOE_DBG", "")
    if dbg_mode:
        blocks = blocks[:1]
    n_exp = {"": NE, "pro": 0, "pro0": 0, "e1": 1, "e2": 2}[dbg_mode]
    k.reset()
    ident = k.alloc("ident32", [128, 128], F32)
    k.dma("sync", ident, dr["ident"])
    rw = k.alloc("rw", [128, 8, NE], F32)
    k.dma("sync", rw, dr["router_w"].rearrange("(k p) e -> p k e", p=128))
    rb = k.alloc("rb", [128, NE], F32)
    load_bc(k, "sync", rb, dr["router_b"])
    mod = {}
    for kind, si in (("x", 0), ("ctx", 1)):
        if kind in srcs:
            mod[kind] = load_mod_tiles(k, dr, layer, si, "f", dr["norm_ffn_w"][layer])
    maxnt = max(len(b) for b in blocks)
    xt = [k.alloc("xt", [128, D], F32) for _ in range(2)]
    h32 = [k.alloc("h32", [128, D], F32) for _ in range(2)]
    junk = k.alloc("junk", [128, D], BF16)
    t32 = k.alloc("t32", [128, D], F32)
    st = [k.alloc("st", [128, 4], F32) for _ in range(2)]
    hT32 = [k.alloc("hT32", [128, 8, 128], F32) for _ in range(2)]
    hTb = k.alloc("hTb", [128, 8, maxnt * 128], BF16)
    gates = k.alloc("gates", [128, maxnt, NE], F32)
    rs = [k.alloc("rs", [128, 160], F32) for _ in range(2)]
    w1 = [k.alloc("w1", [128, 8, FF], BF16) for _ in range(2)]
    w3 = [k.alloc("w3", [128, 8, FF], BF16) for _ in range(2)]
    w2 = [k.alloc("w2", [128, 4, D], BF16) for _ in range(2)]
    ssb = [k.alloc("ssb", [128, 512], F32) for _ in range(2)]
    actT = [k.alloc("actT", [128, 4, maxnt * 128], BF16) for _ in range(2)]
    yacc = k.alloc("yacc", [128, maxnt, D], F32)
    tmp = k.alloc("tmp", [128, D], F32)
    tn = 0
    ne = 0
    nh = 0
    ny = 0
    for blk in blocks:
        nt = len(blk)
        ntok = nt * 128
        for j, (kind, ti) in enumerate(blk):
            x_ = xt[tn % 2]
            h_ = h32[tn % 2]
            r_ = rs[tn % 2]
            A_, B_, _ = mod[kind]
            rows = slice(ti * 128, (ti + 1) * 128)
            k.dma("sync", x_, srcs[kind][rows, :])
            norm_mod(k, x_, A_, B_, h_, junk, st[tn % 2], t32)
            for hb_ in range(2):
                bank = k.banks[6 + hb_]
                for c4 in range(4):
                    c = hb_ * 4 + c4
                    k.transpose(bank[:, c4 * 128:(c4 + 1) * 128], h_[:, c * 128:(c + 1) * 128], ident)
                src = bank.rearrange("p (a b) -> p a b", a=4)
                k.vop("tensor_copy", hT32[tn % 2][:, hb_ * 4:hb_ * 4 + 4, :], src)
            k.act(hTb[:, :, j * 128:(j + 1) * 128], hT32[tn % 2], AF.Copy)
            lps = k.banks[5]
            for kk in range(8):
                k.mm(lps[:, 0:NE], hT32[tn % 2][:, kk, :], rw[:, kk, :], kk == 0, kk == 7)
            if dbg_mode == "pro0":
                k.vop("tensor_copy", gates[:, j, :], lps[:, 0:NE])
                tn += 1
                continue
            lg = r_[:, 0:16]
            pg = r_[:, 16:32]
            p6 = r_[:, 32:56].rearrange("p (a b) -> p a b", a=4)
            msk = r_[:, 56:72]
            eq1 = r_[:, 72:88]
            sel = r_[:, 88:104]
            gsel = r_[:, 104:120]
            gs = r_[:, 120:124]
            oh = r_[:, 124:128]
            ohm = r_[:, 128:132]
            mx, nmx, gm, v1, v2, den, rden = (r_[:, 132 + i:133 + i] for i in range(7))
            pgv = pg.rearrange("p (a b) -> p a b", a=4)
            mskv = msk.rearrange("p (a b) -> p a b", a=4)
            k.vop("tensor_tensor", lg, lps[:, 0:NE], rb, op=ALU.add)
            k.vop("tensor_reduce", mx, lg, axis=AX.X, op=ALU.max)
            k.vop("tensor_scalar", nmx, mx, -1.0, None, op0=ALU.mult)
            k.act(pg, lg, AF.Exp, bias=nmx)
            k.vop("tensor_tensor", p6[:, :, 0:3], pgv[:, :, 0:3], pgv[:, :, 1:4], op=ALU.add)
            k.vop("tensor_tensor", p6[:, :, 3:5], pgv[:, :, 0:2], pgv[:, :, 2:4], op=ALU.add)
            k.vop("tensor_tensor", p6[:, :, 5:6], pgv[:, :, 0:1], pgv[:, :, 3:4], op=ALU.add)
            k.vop("tensor_reduce", gs, p6, axis=AX.X, op=ALU.max)
            k.vop("tensor_reduce", gm, gs, axis=AX.X, op=ALU.max)
            k.vop("tensor_scalar", oh, gs, gm, None, op0=ALU.is_equal)
            k.vop("tensor_scalar", ohm, oh, -1.0, None, op0=ALU.add)
            k.vop("tensor_tensor", mskv, pgv, bcast_last(oh, 4), op=ALU.mult)
            k.vop("tensor_tensor", mskv, mskv, bcast_last(ohm, 4), op=ALU.add)
            k.vop("tensor_reduce", v1, msk, axis=AX.X, op=ALU.max)
            k.vop("tensor_scalar", eq1, msk, v1, None, op0=ALU.is_equal)
            k.vop("scalar_tensor_tensor", eq1, eq1, -2.0, msk, op0=ALU.mult, op1=ALU.add)
            k.vop("tensor_reduce", v2, eq1, axis=AX.X, op=ALU.max)
            k.vop("tensor_scalar", sel, msk, v2, None, op0=ALU.is_ge)
            k.vop("scalar_tensor_tensor", gsel, msk, 1.0, sel, op0=ALU.mult, op1=ALU.mult, accum_out=den)
            k.vop("reciprocal", rden, den)
            k.vop("tensor_scalar", gates[:, j, :], gsel, rden, None, op0=ALU.mult)
            tn += 1
        hs = ntok // 2
        for e in range(n_exp):
            wb = ne % 2
            ne += 1
            k.dma("gpsimd", w1[wb], dr["moe_w1"][layer, e].rearrange("(k p) f -> p k f", p=128))
            k.dma("gpsimd", w3[wb], dr["moe_w3"][layer, e].rearrange("(k p) f -> p k f", p=128))
            k.dma("gpsimd", w2[wb], dr["moe_w2"][layer, e].rearrange("(k p) n -> p k n", p=128),
                  max_dma_last_dim=4096)
            aT = actT[e % 2]
            for half in range(2):
                cs = slice(half * hs, (half + 1) * hs)
                for fc in range(4):
                    ps1 = k.banks[(nh % 2) * 2]
                    ps3 = k.banks[(nh % 2) * 2 + 1]
                    s_ = ssb[nh % 2]
                    nh += 1
                    for kk in range(8):
                        k.mm(ps1[:, :hs], w1[wb][:, kk, fc * 128:(fc + 1) * 128], hTb[:, kk, cs], kk == 0, kk == 7)
                    for kk in range(8):
                        k.mm(ps3[:, :hs], w3[wb][:, kk, fc * 128:(fc + 1) * 128], hTb[:, kk, cs], kk == 0, kk == 7)
                    k.act(s_[:, :hs], ps1[:, :hs], AF.Silu)
                    k.vop("tensor_tensor", aT[:, fc, cs], s_[:, :hs], ps3[:, :hs], op=ALU.mult)
            for j in range(nt):
                for dh in range(2):
                    psy = k.banks[4 + ny % 2]
                    ny += 1
                    ds_ = slice(dh * 512, (dh + 1) * 512)
                    for fc in range(4):
                        k.mm(psy, aT[:, fc, j * 128:(j + 1) * 128], w2[wb][:, fc, ds_], fc == 0, fc == 3)
                    if e == 0:
                        k.vop("tensor_scalar", yacc[:, j, ds_], psy, gates[:, j, e:e + 1], None, op0=ALU.mult)
                    else:
                        k.vop("scalar_tensor_tensor", yacc[:, j, ds_], psy, gates[:, j, e:e + 1], yacc[:, j, ds_],
                              op0=ALU.mult, op1=ALU.add)
        for j, (kind, ti) in enumerate(blk):
            x_ = xt[tn % 2]
            tn += 1
            rows = slice(ti * 128, (ti + 1) * 128)
            G = mod[kind][2]
            k.dma("sync", x_, srcs[kind][rows, :])
            if n_exp == 0:
                k.vop("tensor_copy", yacc[:, j, 0:NE], gates[:, j, :])
                k.vop("tensor_copy", yacc[:, j, NE:D], h32[0][:, NE:D])
            k.vop("tensor_tensor", tmp, yacc[:, j, :], G, op=ALU.mult)
            k.vop("tensor_tensor", x_, x_, tmp, op=ALU.add)
            k.dma("sync", dsts[kind][rows, :], x_)


NB = 12
NSLOT = NB * 512
U32 = mybir.dt.uint32
I32 = mybir.dt.int32


def phase_moe_sorted(k, dr, layer, srcs, dsts, tiles):
    nt = len(tiles)
    nblk = (nt * 128 + 4 * 511) // 512
    assert nblk <= NB
    k.reset()
    Hs, Ys, Gs = dr["Hs"], dr["Ys"], dr["Gs"]
    slot_u = k.alloc("slot_u", [128, nt], U32)
    g512 = k.alloc("g512", [128, NB], F32)
    g2048 = k.alloc("g2048", [128, NB], F32)
    mark = k.off
    zt = k.alloc("zt", [128, 4096], BF16)
    k.vop("memset", zt, 0.0)
    zg = k.alloc("zg", [128, NSLOT * 16 // 128], F32)
    k.vop("memset", zg, 0.0)
    for i in range(NB):
        k.dma("scalar", Hs[i * 512:(i + 1) * 512, :].rearrange("(p r) c -> p (r c)", p=128), zt)
    k.dma("scalar", Gs.rearrange("(p r) c -> p (r c)", p=128), zg)
    ident = k.alloc("ident32", [128, 128], F32)
    k.dma("sync", ident, dr["ident"])
    ltri = k.alloc("ltri", [128, 128], F32)
    k.dma("sync", ltri, dr["ltri"])
    ones = k.alloc("ones", [128, 128], F32)
    k.vop("memset", ones, 1.0)
    rw = k.alloc("rw", [128, 8, NE], F32)
    k.dma("sync", rw, dr["router_w"].rearrange("(k p) e -> p k e", p=128))
    rb = k.alloc("rb", [128, NE], F32)
    load_bc(k, "sync", rb, dr["router_b"])
    mod = {}
    for kind, si in (("x", 0), ("ctx", 1)):
        if kind in srcs:
            mod[kind] = load_mod_tiles(k, dr, layer, si, "f", dr["norm_ffn_w"][layer], need_gate=False)
    xt = [k.alloc("xt", [128, D], F32) for _ in range(3)]
    h32 = [k.alloc("h32", [128, D], F32) for _ in range(2)]
    junk = k.alloc("junk", [128, D], BF16)
    t32s = [k.alloc("t32", [128, D], F32) for _ in range(2)]
    st = [k.alloc("st", [128, 4], F32) for _ in range(3)]
    hT32 = [k.alloc("hT32", [128, 8, 128], F32) for _ in range(2)]
    hb_all = k.alloc("hb_all", [128, nt, D], BF16)
    lgall = k.alloc("lgall", [128, nt, NE], F32)
    junks = [junk, k.alloc("junk2", [128, D], BF16)]

    def m1_gen(j, kind, ti):
        x_ = xt[j % 3]
        h_ = h32[j % 2]
        A_, B_, _ = mod[kind]
        rows = slice(ti * 128, (ti + 1) * 128)
        k.dma("sync", x_, srcs[kind][rows, :])
        yield from norm_mod_g(k, x_, A_, B_, h_, junks[j % 2], st[j % 3], t32s[j % 2], ss_eng="scalar")
        k.act(hb_all[:, j, :], h_, AF.Copy)
        for hb_ in range(2):
            bank = k.banks[6 + hb_]
            for c4 in range(4):
                c = hb_ * 4 + c4
                k.transpose(bank[:, c4 * 128:(c4 + 1) * 128], h_[:, c * 128:(c + 1) * 128], ident)
            k.vop("tensor_copy", hT32[j % 2][:, hb_ * 4:hb_ * 4 + 4, :], bank.rearrange("p (a b) -> p a b", a=4))
        yield
        lps = k.banks[4 + j % 2]
        for kk in range(8):
            k.mm(lps[:, 0:NE], hT32[j % 2][:, kk, :], rw[:, kk, :], kk == 0, kk == 7)
        k.vop("tensor_tensor", lgall[:, j, :], lps[:, 0:NE], rb, op=ALU.add)

    run_interleaved((m1_gen(j, kind, ti) for j, (kind, ti) in enumerate(tiles)), width=2)
    def al(name, n):
        return k.alloc(name, [128, n], F32)
    pg = al("pg", nt * 16); p6 = al("p6", nt * 24); msk = al("msk", nt * 16); eq1 = al("eq1", nt * 16)
    sel = al("sel", nt * 16); gates = al("gates", nt * 16); glp = al("glp", nt * 16)
    gs = al("gs", nt * 4); oh = al("oh", nt * 4); ohm = al("ohm", nt * 4); t4 = al("t4", nt * 4)
    tot = al("tot", nt * 4); cum = al("cum", nt * 4)
    mx = al("mx", nt); gm = al("gm", nt); v1 = al("v1", nt); v2 = al("v2", nt); den = al("den", nt)
    slot_f = al("slot_f", nt); onesr = al("onesr", nt)
    ng = al("ng", 4); cnt = al("cnt", 4); base = al("base", 4); endg = al("endg", 4)
    thr = al("thr", 9); cmp_ = al("cmp", 36); blk0 = al("blk0", NB); gidf = al("gidf", NB); tmpb = al("tmpb", NB)

    def v3(v, a, b):
        return v.rearrange("p (a b) -> p a b", a=a)
    lg3 = lgall
    k.vop("tensor_reduce", mx, lg3, axis=AX.X, op=ALU.max)
    k.vop("tensor_tensor", v3(pg, nt, 16), lg3, bcast_last(mx, 16), op=ALU.subtract)
    k.act(pg, pg, AF.Exp)
    pgv = v3(pg, nt * 4, 4)
    p6v = v3(p6, nt * 4, 6)
    k.vop("tensor_tensor", p6v[:, :, 0:3], pgv[:, :, 0:3], pgv[:, :, 1:4], op=ALU.add)
    k.vop("tensor_tensor", p6v[:, :, 3:5], pgv[:, :, 0:2], pgv[:, :, 2:4], op=ALU.add)
    k.vop("tensor_tensor", p6v[:, :, 5:6], pgv[:, :, 0:1], pgv[:, :, 3:4], op=ALU.add)
    k.vop("tensor_reduce", gs, p6v, axis=AX.X, op=ALU.max)
    k.vop("tensor_reduce", gm, v3(gs, nt, 4), axis=AX.X, op=ALU.max)
    k.vop("tensor_tensor", v3(oh, nt, 4), v3(gs, nt, 4), bcast_last(gm, 4), op=ALU.is_equal)
    k.vop("tensor_scalar", ohm, oh, -1.0, None, op0=ALU.add)
    mskv = v3(msk, nt * 4, 4)
    k.vop("tensor_tensor", mskv, pgv, bcast_last(oh, 4), op=ALU.mult)
    k.vop("tensor_tensor", mskv, mskv, bcast_last(ohm, 4), op=ALU.add)
    msk3 = v3(msk, nt, 16)
    k.vop("tensor_reduce", v1, msk3, axis=AX.X, op=ALU.max)
    k.vop("tensor_tensor", v3(eq1, nt, 16), msk3, bcast_last(v1, 16), op=ALU.is_equal)
    k.vop("scalar_tensor_tensor", eq1, eq1, -2.0, msk, op0=ALU.mult, op1=ALU.add)
    k.vop("tensor_reduce", v2, v3(eq1, nt, 16), axis=AX.X, op=ALU.max)
    k.vop("tensor_tensor", v3(sel, nt, 16), msk3, bcast_last(v2, 16), op=ALU.is_ge)
    k.vop("tensor_tensor", sel, sel, msk, op=ALU.mult)
    k.vop("tensor_reduce", den, v3(sel, nt, 16), axis=AX.X, op=ALU.add)
    k.vop("reciprocal", den, den)
    k.vop("tensor_tensor", v3(gates, nt, 16), v3(sel, nt, 16), bcast_last(den, 16), op=ALU.mult)
    k.vop("memset", glp, 0.0)
    k.vop("tensor_reduce", v3(glp, nt, 16)[:, :, 0:4], apv(gates, [[16, nt], [1, 4], [4, 4]]), axis=AX.X, op=ALU.add)
    pre_ps = k.banks[4]
    tot_ps = k.banks[5]
    k.mm(pre_ps[:, 0:nt * 4], ltri, oh, True, True)
    k.mm(tot_ps[:, 0:nt * 4], ones, oh, True, True)
    k.vop("tensor_copy", tot, tot_ps[:, 0:nt * 4])
    k.vop("memset", onesr, 1.0)
    for g in range(4):
        k.vop("tensor_tensor_scan", apv(cum, [[4, nt]], extra_off=g), onesr, apv(tot, [[4, nt]], extra_off=g), 0.0,
              op0=ALU.mult, op1=ALU.add)
    k.vop("tensor_copy", ng, cum[:, (nt - 1) * 4:nt * 4])
    k.vop("tensor_tensor", cum, cum, tot, op=ALU.subtract)
    for m in range(9):
        k.vop("memset", thr[:, m:m + 1], float(512 * m))
    k.vop("tensor_tensor", v3(cmp_, 4, 9), bcast_last(ng, 9), apv(thr, [[0, 4], [1, 9]]), op=ALU.is_gt)
    k.vop("tensor_reduce", cnt, v3(cmp_, 4, 9), axis=AX.X, op=ALU.add)
    k.vop("tensor_scalar", cnt, cnt, 512.0, None, op0=ALU.mult)
    k.vop("memset", base[:, 0:1], 0.0)
    for g in range(1, 4):
        k.vop("tensor_tensor", base[:, g:g + 1], base[:, g - 1:g], cnt[:, g - 1:g], op=ALU.add)
    k.vop("tensor_tensor", endg, base, cnt, op=ALU.add)
    k.vop("tensor_tensor", t4, cum, pre_ps[:, 0:nt * 4], op=ALU.add)
    k.vop("tensor_tensor", v3(t4, nt, 4), v3(t4, nt, 4), apv(base, [[0, nt], [1, 4]]), op=ALU.add)
    k.vop("tensor_tensor", t4, t4, oh, op=ALU.mult)
    k.vop("tensor_reduce", slot_f, v3(t4, nt, 4), axis=AX.X, op=ALU.add)
    k.vop("tensor_copy", slot_u, slot_f)
    for i in range(NB):
        k.vop("memset", blk0[:, i:i + 1], float(512 * i))
    k.vop("tensor_scalar", gidf, blk0, endg[:, 0:1], None, op0=ALU.is_ge)
    for g in (1, 2):
        k.vop("tensor_scalar", tmpb, blk0, endg[:, g:g + 1], None, op0=ALU.is_ge)
        k.vop("tensor_tensor", gidf, gidf, tmpb, op=ALU.add)
    k.vop("tensor_scalar", g512, gidf, 512.0, None, op0=ALU.mult)
    k.vop("tensor_scalar", g2048, gidf, 2048.0, None, op0=ALU.mult)
    k.P.barrier()
    for j in range(nt):
        idx = slot_u[:, j:j + 1]
        src_h = hb_all[:, j, :]
        src_g = v3(glp, nt, 16)[:, j, :]

        def sc_h(h, idx=idx, src=src_h):
            return h.indirect_dma_start(out=Hs, out_offset=bass.IndirectOffsetOnAxis(ap=idx.ap, axis=0),
                                        in_=src.ap, in_offset=None)

        def sc_g(h, idx=idx, src=src_g):
            return h.indirect_dma_start(out=Gs, out_offset=bass.IndirectOffsetOnAxis(ap=idx.ap, axis=0),
                                        in_=src.ap, in_offset=None)
        k.P.add("gpsimd", sc_h, reads=list(slot_u.bufs) + list(hb_all.bufs), dma=True)
        k.P.add("gpsimd", sc_g, reads=list(slot_u.bufs) + list(glp.bufs), dma=True)
    k.P.barrier()
    k.off = mark
    identb = k.alloc("identb", [128, 128], BF16)
    k.dma("gpsimd", identb, dr["ident"])
    ht4 = [k.alloc("ht4", [128, 4, D], BF16) for _ in range(2)]
    gt4 = [k.alloc("gt4", [128, 4, 16], F32) for _ in range(2)]
    hTb = [k.alloc("hTb", [128, 8, 512], BF16) for _ in range(2)]
    w1 = [k.alloc("w1", [128, 8 * FF], BF16) for _ in range(2)]
    w3 = [k.alloc("w3", [128, 8 * FF], BF16) for _ in range(2)]
    wst = [k.alloc("wst", [128, 8 * FF], F32) for _ in range(4)]
    w2 = [[k.alloc("w2", [128, D], BF16) for _ in range(4)] for _ in range(2)]
    ssb = [k.alloc("ssb", [128, 512], F32) for _ in range(2)]
    actT = [k.alloc("actT", [128, 4, 512], BF16) for _ in range(2)]
    yacc = [k.alloc("yacc", [128, 4, D], F32) for _ in range(2)]
    bankbf = [k.banks[6].bitcast(BF16), k.banks[7].bitcast(BF16)]
    cA = k.alloc("cA", [128, 32], F32)
    cB = k.alloc("cB", [128, 16], F32)
    k.dma("sync", cA, dr["idxA"])
    k.dma("sync", cB, dr["idxB"])
    idxA = [k.alloc("idxA", [128, 4], U32) for _ in range(nblk)]
    idxB = [k.alloc("idxB", [128, 16], U32) for _ in range(nblk)]
    W1f = dr["moe_w1"].rearrange("l e (p k) c -> (l e p) (k c)", k=8)
    W3f = dr["moe_w3"].rearrange("l e (p k) c -> (l e p) (k c)", k=8)
    W2f = dr["moe_w2"].rearrange("l e f c -> (l e f) c")
    for i in range(nblk):
        k.vop("tensor_scalar", idxA[i], cA[:, 0:4], g512[:, i:i + 1], float(layer * NE * 128), op0=ALU.add, op1=ALU.add)
        k.vop("tensor_scalar", idxB[i], cB, g2048[:, i:i + 1], float(layer * NE * FF), op0=ALU.add, op1=ALU.add)
    ne = 0
    nh = 0
    ny = 0
    tpc = {"n": 0}

    def do_transposes(bi):
        h4_ = ht4[bi % 2]
        hTd = hTb[bi % 2]
        for a in range(4):
            bb = bankbf[tpc["n"] % 2]
            tpc["n"] += 1
            hv = h4_[:, a, :].rearrange("p (f k) -> p k f", k=8)
            for c in range(8):
                k.transpose(bb[:, c * 128:(c + 1) * 128], hv[:, c, :], identb)
            k.vop("tensor_copy", hTd[:, :, a * 128:(a + 1) * 128], bb.rearrange("p (a b) -> p a b", a=8))
    regs = {}
    for i in range(nblk):
        rows = slice(i * 512, (i + 1) * 512)
        h4 = ht4[i % 2]
        g4 = gt4[i % 2]
        hT_ = hTb[i % 2]
        ya = yacc[i % 2]
        if i == 0:
            k.dma("sync", h4, Hs[rows, :].rearrange("(a p) c -> p a c", p=128))
            k.dma("sync", g4, Gs[rows, :].rearrange("(a p) c -> p a c", p=128))
        if i + 1 < nblk:
            nrows = slice((i + 1) * 512, (i + 2) * 512)
            k.dma("sync", ht4[(i + 1) % 2], Hs[nrows, :].rearrange("(a p) c -> p a c", p=128))
            k.dma("sync", gt4[(i + 1) % 2], Gs[nrows, :].rearrange("(a p) c -> p a c", p=128))
        if i == 0:
            do_transposes(0)
        for el in range(4):
            wb = ne % 2
            ne += 1

            def gath(dst, srcW, idxv):
                def f(h):
                    return h.indirect_dma_start(out=dst.ap, out_offset=None, in_=srcW,
                                                in_offset=bass.IndirectOffsetOnAxis(ap=idxv.ap, axis=0))
                k.P.add("gpsimd", f, reads=list(idxv.bufs), writes=list(dst.bufs), dma=True)
            s1 = wst[(2 * ne) % 4]
            s3 = wst[(2 * ne + 1) % 4]
            gath(s1, W1f, idxA[i][:, el:el + 1])
            gath(s3, W3f, idxA[i][:, el:el + 1])
            k.act(w1[wb], s1, AF.Copy)
            k.act(w3[wb], s3, AF.Copy)
            for kk in range(4):
                gath(w2[wb][kk], W2f, idxB[i][:, el * 4 + kk:el * 4 + kk + 1])
            aT = actT[ne % 2]
            for fc in range(4):
                ps1 = k.banks[(nh % 2) * 2]
                ps3 = k.banks[(nh % 2) * 2 + 1]
                s_ = ssb[nh % 2]
                nh += 1
                for kk in range(8):
                    k.mm(ps1, w1[wb][:, kk * FF + fc * 128:kk * FF + (fc + 1) * 128], hT_[:, kk, :], kk == 0, kk == 7)
                for kk in range(8):
                    k.mm(ps3, w3[wb][:, kk * FF + fc * 128:kk * FF + (fc + 1) * 128], hT_[:, kk, :], kk == 0, kk == 7)
                k.act(s_, ps1, AF.Silu)
                k.vop("tensor_tensor", aT[:, fc, :], s_, ps3, op=ALU.mult)
            if el == 1 and i + 1 < nblk:
                do_transposes(i + 1)
            for a in range(4):
                for dh in range(2):
                    psy = k.banks[4 + ny % 2]
                    ny += 1
                    ds_ = slice(dh * 512, (dh + 1) * 512)
                    for fc in range(4):
                        k.mm(psy, aT[:, fc, a * 128:(a + 1) * 128], w2[wb][fc][:, ds_], fc == 0, fc == 3)
                    if el == 0:
                        k.vop("tensor_scalar", ya[:, a, ds_], psy, g4[:, a, el:el + 1], None, op0=ALU.mult)
                    else:
                        k.vop("scalar_tensor_tensor", ya[:, a, ds_], psy, g4[:, a, el:el + 1], ya[:, a, ds_],
                              op0=ALU.mult, op1=ALU.add)
        k.dma("sync", Ys[rows, :].rearrange("(a p) c -> p a c", p=128), ya)
    k.P.barrier()
    k.off = mark
    Gt = {}
    base_i = 3
    for kind, si in (("x", 0), ("ctx", 1)):
        if kind in srcs:
            Gt[kind] = k.alloc("G", [128, D], F32)
            load_bc(k, "sync", Gt[kind], dr["mods_d"][si, layer, (base_i + 2) * D:(base_i + 3) * D])
    xt = [k.alloc("xt", [128, D], F32) for _ in range(6)]
    yt = [k.alloc("yt", [128, D], F32) for _ in range(6)]
    for j, (kind, ti) in enumerate(tiles):
        x_ = xt[j % 6]
        y_ = yt[j % 6]
        rows = slice(ti * 128, (ti + 1) * 128)
        idx = slot_u[:, j:j + 1]

        def ga(h, idx=idx, dst=y_):
            return h.indirect_dma_start(out=dst.ap, out_offset=None, in_=Ys,
                                        in_offset=bass.IndirectOffsetOnAxis(ap=idx.ap, axis=0))
        k.P.add("gpsimd", ga, reads=list(slot_u.bufs), writes=list(y_.bufs), dma=True)
        k.dma("scalar", x_, srcs[kind][rows, :])
        k.vop("tensor_tensor", y_, y_, Gt[kind], op=ALU.mult)
        k.vop("tensor_tensor", x_, x_, y_, op=ALU.add)
        k.dma("sync", dsts[kind][rows, :], x_)


def phase_moe0(k, dr):
    tiles = [("ctx", 0), ("ctx", 1)] + [("x", i) for i in range(NXT)]
    blocks = [tiles[0:7], tiles[7:14], tiles[14:21], tiles[21:28], tiles[28:34]]
    import os
    if os.environ.get("MOE_SRC_X"):
        srcs = {"x": dr["x"], "ctx": dr["ctx"]}
    else:
        srcs = {"x": dr["XR1"], "ctx": dr["CR1"]}
    if os.environ.get("MOE_DENSE"):
        phase_moe(k, dr, 0, srcs, {"x": dr["XR2"], "ctx": dr["CR2"]}, blocks)
    else:
        phase_moe_sorted(k, dr, 0, srcs, {"x": dr["XR2"], "ctx": dr["CR2"]}, tiles)


def phase_moe1(k, dr):
    tiles = [("x", i) for i in range(NXT)]
    blocks = [tiles[8 * i:8 * i + 8] for i in range(4)]
    import os
    if os.environ.get("MOE_DENSE"):
        phase_moe(k, dr, 1, {"x": dr["XR3"]}, {"x": dr["out"]}, blocks)
    else:
        phase_moe_sorted(k, dr, 1, {"x": dr["XR3"]}, {"x": dr["out"]}, tiles)


QPAIRS = [(0, 4), (1, 5), (2, 6), (3, 7), (8, 12), (9, 13), (10, 14), (11, 15)]
NKT = TS // 128
ATT = {}


def apv(v, dims, extra_off=0):
    a = v.ap
    return V(bass.AP(a.tensor, a.offset + extra_off, [list(a.ap[0])] + [list(d) for d in dims]), v.bufs)


def phase_attn_proj(k, dr):
    k.reset()
    kT = k.alloc("kT", [128, 2, TS], BF16)
    Vaug = k.alloc("Vaug", [128, NKT * 4, 128], BF16)
    ATT["kT"], ATT["Vaug"], ATT["keep"] = kT, Vaug, k.off
    k.vop("memset", Vaug[:, :, 64:128], 1.0)
    ident = k.alloc("ident", [128, 128], BF16)
    k.dma("gpsimd", ident, dr["ident"])
    wv = dr["attn_qkv_w"][0].rearrange("(k p) n -> p k n", p=128)
    qkvw = []
    for kk in range(8):
        w = k.alloc(f"qkvw{kk}", [128, 1536], BF16)
        for G_ in range(2):
            for a_ in range(2):
                src = wv[:, kk, (8 * G_ + 4 * a_) * 64:(8 * G_ + 4 * a_ + 4) * 64].rearrange("p (i d) -> p i d", i=4)
                dst = w[:, 8 * G_ * 64:8 * G_ * 64 + 512].rearrange("p (i a d) -> p i a d", i=4, a=2)[:, :, a_, :]
                k.dma("gpsimd", dst, src)
        k.dma("gpsimd", w[:, 1024:1536], wv[:, kk, 1024:1536], max_dma_last_dim=2048)
        qkvw.append(w)
    Ax, Bx, _ = load_mod_tiles(k, dr, 1, 0, "m", dr["norm_mix_w"][1], need_gate=False)
    Ac, Bc, _ = load_mod_tiles(k, dr, 1, 1, "m", dr["norm_mix_w"][1], need_gate=False)
    gq = k.alloc("gq", [128, 20, 64], F32)
    g64 = k.alloc("g64", [128, 2, 64], F32)
    load_bc(k, "sync", g64[:, 0, :], dr["attn_q_norm_w"][0])
    load_bc(k, "sync", g64[:, 1, :], dr["attn_k_norm_w"][0])
    k.vop("tensor_copy", gq[:, 0:16, :], apv(g64[:, 0, :], [[0, 16], [1, 64]]))
    k.vop("tensor_copy", gq[:, 16:20, :], apv(g64[:, 1, :], [[0, 4], [1, 64]]))
    rp = k.alloc("rp", [128, NXT, 64], F32)
    k.dma("sync", rp, dr["rope"].rearrange("(n p) c -> p n c", p=128))
    xt = [k.alloc("xt", [128, D], F32) for _ in range(2)]
    junks = [k.alloc("junk", [128, D], BF16) for _ in range(2)]
    t32s = [k.alloc("t32", [128, D], F32) for _ in range(2)]
    hb = [k.alloc("hb", [128, D], BF16) for _ in range(2)]
    st = [k.alloc("st", [128, 4], F32) for _ in range(2)]
    hT = [k.alloc("hT", [128, 8, 128], BF16) for _ in range(2)]
    qk32 = [k.alloc("qk32", [128, 20, 64], F32) for _ in range(2)]
    sqs = [k.alloc("sq", [128, 20, 64], F32) for _ in range(2)]
    rst = [k.alloc("rst", [128, 64], F32) for _ in range(2)]
    tcs = [[k.alloc("tc", [128, 20, 32], F32) for _ in range(2)] for _ in range(2)]
    tss = [[k.alloc("ts", [128, 20, 32], F32) for _ in range(2)] for _ in range(2)]
    qr = [k.alloc("qr", [128, 20, 64], BF16) for _ in range(2)]
    qst = [k.alloc("qst", [128, 8, 512], BF16) for _ in range(2)]
    tiles = [("ctx", j) for j in range(NCT)] + [("x", i) for i in range(NXT)]

    def tile_gen(kind, ti, tn):
        isx = kind == "x"
        kt = ti if not isx else NCT + ti
        x_ = xt[tn % 2]
        A_, B_ = (Ax, Bx) if isx else (Ac, Bc)
        rows = slice(ti * 128, (ti + 1) * 128)
        k.dma("sync", x_, (dr["XR2"] if isx else dr["CR2"])[rows, :])
        yield from norm_mod_g(k, x_, A_, B_, hb[tn % 2], junks[tn % 2], st[tn % 2], t32s[tn % 2], ss_eng="scalar")
        hbank = k.banks[4 + tn % 2].bitcast(BF16)
        hT_ = hT[tn % 2]
        transpose_tile(k, hb[tn % 2], hT_, hbank, ident)
        yield
        sq = sqs[tn % 2]
        q32 = qk32[tn % 2]
        q32f = q32.rearrange("p a b -> p (a b)")
        if isx:
            for nbk in range(3):
                for kk in range(8):
                    k.mm(k.banks[nbk], hT_[:, kk, :], qkvw[kk][:, nbk * 512:(nbk + 1) * 512], kk == 0, kk == 7)
            k.act(q32f[:, 0:512], k.banks[0], AF.Copy)
            k.act(q32f[:, 512:1024], k.banks[1], AF.Copy)
            k.act(q32f[:, 1024:1280], k.banks[2][:, 0:256], AF.Copy)
            vsrc = k.banks[2][:, 256:512]
            h0, nh = 0, 20
        else:
            for kk in range(8):
                k.mm(k.banks[2], hT_[:, kk, :], qkvw[kk][:, 1024:1536], kk == 0, kk == 7)
            k.act(q32f[:, 1024:1280], k.banks[2][:, 0:256], AF.Copy)
            vsrc = k.banks[2][:, 256:512]
            h0, nh = 16, 4
        k.act(Vaug[:, kt * 4:kt * 4 + 4, 0:64], vsrc.rearrange("p (g d) -> p g d", g=4), AF.Copy)
        yield
        qh = q32[:, h0:h0 + nh, :]
        r_ = rst[tn % 2]
        k.vop("tensor_tensor", sq[:, h0:h0 + nh, :], qh, qh, op=ALU.mult)
        k.vop("tensor_reduce", r_[:, 0:nh], sq[:, h0:h0 + nh, :], axis=AX.X, op=ALU.add)
        k.vop("tensor_scalar", r_[:, 20:20 + nh], r_[:, 0:nh], 1.0 / 64, EPS, op0=ALU.mult, op1=ALU.add)
        k.act(r_[:, 40:40 + nh], r_[:, 20:20 + nh], AF.Sqrt)
        yield
        k.vop("reciprocal", r_[:, 0:nh], r_[:, 40:40 + nh])
        k.vop("tensor_tensor", qh, qh, bcast_last(r_[:, 0:nh], 64), op=ALU.mult)
        q_ = qr[tn % 2]
        if isx:
            k.vop("tensor_tensor", qh, qh, gq[:, h0:h0 + nh, :], op=ALU.mult)
            for rc in range(2):
                qv = apv(q32, [[64, 20], [16, 2], [1, 16]], extra_off=rc * 32)
                ov = apv(q_, [[64, 20], [16, 2], [1, 16]], extra_off=rc * 32)
                cosb = apv(rp[:, ti, rc * 32:rc * 32 + 16], [[0, 20], [0, 2], [1, 16]])
                sinb = apv(rp[:, ti, rc * 32 + 16:rc * 32 + 32], [[0, 20], [0, 2], [1, 16]])
                tcv = tcs[tn % 2][rc].rearrange("p h (t j) -> p h t j", t=2)
                tsv = tss[tn % 2][rc].rearrange("p h (t j) -> p h t j", t=2)
                k.vop("tensor_tensor", tcv, qv, cosb, op=ALU.mult)
                k.vop("tensor_tensor", tsv, qv, sinb, op=ALU.mult)
                k.vop("tensor_tensor", ov[:, :, 0, :], tcv[:, :, 0, :], tsv[:, :, 1, :], op=ALU.subtract)
                k.vop("tensor_tensor", ov[:, :, 1, :], tcv[:, :, 1, :], tsv[:, :, 0, :], op=ALU.add)
        else:
            k.vop("tensor_tensor", q_[:, h0:h0 + nh, :], qh, gq[:, h0:h0 + nh, :], op=ALU.mult)
        yield
        kbank = k.banks[3].bitcast(BF16)
        for m in range(2):
            k.transpose(kbank[:, m * 128:(m + 1) * 128], q_[:, 16 + 2 * m:18 + 2 * m, :].rearrange("p a b -> p (a b)"), ident)
        k.act(kT[:, :, kt * 128:(kt + 1) * 128], kbank[:, 0:256].rearrange("p (a b) -> p a b", a=2), AF.Copy)
        if isx:
            qbank = k.banks[6 + tn % 2].bitcast(BF16)
            qf = q_.rearrange("p a b -> p (a b)")
            for s_i in range(8):
                k.transpose(qbank[:, s_i * 128:(s_i + 1) * 128], qf[:, s_i * 128:(s_i + 1) * 128], ident)
            qs_ = qst[(ti // 4) % 2]
            k.vop("tensor_copy", qs_[:, :, (ti % 4) * 128:(ti % 4 + 1) * 128],
                  qbank.rearrange("p (a b) -> p a b", a=8))
            if ti % 4 == 3:
                t0 = (ti // 4) * 512
                k.dma("gpsimd", dr["QT"][:, :, t0:t0 + 512].rearrange("s p t -> p s t"), qs_)


    run_interleaved((tile_gen(kind, ti, n) for n, (kind, ti) in enumerate(tiles)), width=2)


def bank2(k, i):
    return V(k.psum[:, i:i + 2, :].rearrange("p a b -> p (a b)"), k.banks[i].bufs + k.banks[i + 1].bufs)


def phase_attn_core(k, dr):
    k.reset(keep=ATT["keep"])
    kT, Vaug = ATT["kT"], ATT["Vaug"]
    wv = dr["attn_o_w"][0].rearrange("(k p) n -> p k n", p=128)
    ow = []
    for kk in range(8):
        w = k.alloc(f"aow{kk}", [128, D], BF16)
        k.dma("gpsimd", w, wv[:, kk, :], max_dma_last_dim=4096)
        ow.append(w)
    G = k.alloc("G", [128, D], F32)
    load_bc(k, "sync", G, dr["mods_d"][0, 1, 2 * D:3 * D])
    qz = [k.alloc("qz", [128, 16, 512], BF16) for _ in range(2)]
    for q_ in qz:
        k.vop("memset", q_, 0.0)
    aT = [k.alloc("aT", [128, 8, 512], BF16) for _ in range(2)]
    NP = 4
    pT = [k.alloc("pT", [128, 1024], BF16) for _ in range(NP)]
    rl = [k.alloc("rl", [128, 512], F32) for _ in range(2)]
    rl0 = [k.alloc("rl0", [128, 512], F32) for _ in range(2)]
    osb = [k.alloc("osb", [128, 512], F32) for _ in range(2)]
    xt = [k.alloc("xt", [128, D], F32) for _ in range(3)]
    tmp = k.alloc("tmp", [128, D], F32)
    sb2 = [bank2(k, 2), bank2(k, 4), bank2(k, 6)]
    LOOK = 2
    nu = 0
    nr = 0
    tn = 0
    pending = []
    tnc = {"n": 0}

    def flush_oproj():
        while pending:
            qb_, a_o = pending.pop(0)
            for j in range(4):
                ti = qb_ * 4 + j
                x_ = xt[tnc["n"] % 3]
                tnc["n"] += 1
                rows = slice(ti * 128, (ti + 1) * 128)
                k.dma("sync", x_, dr["XR2"][rows, :])
                for half in range(2):
                    cs = slice(half * 512, (half + 1) * 512)
                    ps = k.banks[2 + half]
                    for c in range(8):
                        k.mm(ps, a_o[:, c, j * 128:(j + 1) * 128], ow[c][:, cs], c == 0, c == 7)
                    k.vop("tensor_tensor", tmp[:, cs], ps, G[:, cs], op=ALU.mult)
                    k.vop("tensor_tensor", x_[:, cs], x_[:, cs], tmp[:, cs], op=ALU.add)
                k.dma("gpsimd", dr["XR3"][rows, :], x_)

    for qb in range(T // 512):
        q_ = qz[qb % 2]
        a_ = aT[qb % 2]
        qsl = slice(qb * 512, (qb + 1) * 512)
        for G_ in range(2):
            k.dma("sync", q_[0:64, 8 * G_:8 * G_ + 4, :],
                  dr["QT"][4 * G_:4 * G_ + 4, 0:64, qsl].rearrange("s p t -> p s t"))
            k.dma("sync", q_[64:128, 8 * G_ + 4:8 * G_ + 8, :],
                  dr["QT"][4 * G_:4 * G_ + 4, 64:128, qsl].rearrange("s p t -> p s t"))
        for g in range(4):
            for pr in range(2):
                if g == 0 and pr == 1:
                    flush_oproj()
                heads = [4 * g + 2 * pr, 4 * g + 2 * pr + 1]
                sbk = {}

                def emit_S(n):
                    bank = sb2[(nu + n) % 3]
                    sbk[n] = bank
                    for i, h in enumerate(heads):
                        k.mm(bank[:, i * 512:(i + 1) * 512], kT[:, g // 2, n * 128:(n + 1) * 128],
                             q_[:, h, :], True, True)

                def emit_PV(n):
                    p_ = pT[(nu + n) % NP]
                    k.act(p_, sbk[n], AF.Exp, scale=0.125)
                    for i in range(2):
                        k.mm(k.banks[i], Vaug[:, n * 4 + g, :], p_[:, i * 512:(i + 1) * 512],
                             n == 0, n == NKT - 1)

                for n in range(LOOK):
                    emit_S(n)
                for n in range(NKT):
                    if n + LOOK < NKT:
                        emit_S(n + LOOK)
                    emit_PV(n)
                nu += NKT
                for i, h in enumerate(heads):
                    k.vop("tensor_copy", osb[i], k.banks[i])
                for i, h in enumerate(heads):
                    r_ = rl[nr % 2]
                    r0 = rl0[nr % 2]
                    nr += 1
                    ov_ = osb[i]
                    k.vop("reciprocal", r_[64:128, :], ov_[64:128, :])
                    k.vop("tensor_copy", r0[0:64, :], r_[64:128, :])
                    dp = (h % 2) * 64
                    k.vop("tensor_tensor", a_[dp:dp + 64, h // 2, :], ov_[0:64, :], r0[0:64, :], op=ALU.mult)
        pending.append((qb, a_))
    flush_oproj()


PHASES = [("adaln", phase_adaln), ("lru_in", phase_lru_in), ("lru_scan", phase_lru_scan),
          ("lru_out", phase_lru_out), ("moe0", phase_moe0),
          ("attn_proj", phase_attn_proj), ("attn_core", phase_attn_core), ("moe1", phase_moe1)]


IN_SPECS = {
    "x": [T, D], "c": [1, D], "ctx": [C, D], "c_ctx": [D],
    "ada_w": [2, D, 6 * D], "ada_b": [2, 6 * D], "norm_mix_w": [2, D], "norm_ffn_w": [2, D],
    "lru_in_w": [1, D, 2 * D], "lru_conv_w": [1, 4, D], "lru_conv_b": [1, D],
    "lru_gate_a_w": [1, 2, 8, 128, 128], "lru_gate_a_b": [1, 2, D],
    "lru_gate_x_w": [1, 2, 8, 128, 128], "lru_gate_x_b": [1, 2, D],
    "lru_lambda": [1, 2, D], "lru_out_w": [1, D, D],
    "attn_qkv_w": [1, D, 1536], "attn_q_norm_w": [1, 64], "attn_k_norm_w": [1, 64],
    "attn_o_w": [1, D, D], "router_w": [D, NE], "router_b": [NE],
    "moe_w1": [2, NE, D, FF], "moe_w3": [2, NE, D, FF], "moe_w2": [2, NE, FF, D],
    "ident": [128, 128], "rope": [T, 64], "ltri": [128, 128], "idxA": [128, 32], "idxB": [128, 16],
}
SCRATCH_SPECS = {
    "mods_d": ([2, 2, 6 * D], F32),
    "GG": ([8, 128, TS], BF16), "UU": ([8, 128, TS], F32), "ZZ": ([8, 128, TS], BF16),
    "XR1": ([T, D], F32), "CR1": ([C, D], F32), "XR2": ([T, D], F32), "CR2": ([C, D], F32),
    "XR3": ([T, D], F32), "QT": ([8, 128, T], BF16),
    "Hs": ([NSLOT, D], BF16), "Ys": ([NSLOT, D], F32), "Gs": ([NSLOT, 16], F32),
}


class DR(dict):
    def __init__(self, nc, dbg):
        super().__init__()
        self.nc = nc
        self.dbg = dbg
        self.used_inputs = []

    def __missing__(self, name):
        if not self.dbg:
            if name in ("XR1", "XR2", "XR3"):
                return self["out"]
            if name == "CR2":
                return self["CR1"]
            if name == "ZZ":
                return self["GG"]
            if name == "QT":
                return self["GG"][:, :, 0:T]
        if name in IN_SPECS:
            ap = self.nc.dram_tensor(name, list(IN_SPECS[name]), F32, kind="ExternalInput").ap()
            self.used_inputs.append(name)
        else:
            shape, dt = SCRATCH_SPECS[name]
            kind = "ExternalOutput" if self.dbg else "Internal"
            ap = self.nc.dram_tensor(name, list(shape), dt, kind=kind).ap()
        self[name] = ap
        return ap


def build(stop_after=None, dbg=False):
    nc = bass.Bass("TRN2", target_bir_lowering=False)
    dr = DR(nc, dbg)
    dr["out"] = nc.dram_tensor("out", [T, D], F32, kind="ExternalOutput").ap()
    with ExitStack() as stack:
        arena_bytes = 204 * 1024
        arena_t = stack.enter_context(nc.sbuf_tensor("arena", [128, arena_bytes], U8))
        pt = stack.enter_context(nc.psum_tensor("psum", [128, 8, 512], F32))
        banks = [V(pt[:, i, :], (Buf(f"bank{i}", excl=True),)) for i in range(8)]
        P = Prog(nc)
        k = K(nc, P, arena_t[:], arena_bytes, banks)
        k.psum = pt
        import os
        only = os.environ.get("PHASE_ONLY", "")
        for name, fn in PHASES:
            if only and name not in only.split(","):
                continue
            fn(k, dr)
            if stop_after == name:
                break
        P.barrier()
        P.add("sync", lambda h: h.nop(), dma=False)
        P.emit(stack)
    nc._used_inputs = list(dr.used_inputs)
    return nc


def make_in_maps(inputs, used=None):
    f = lambda a: np.ascontiguousarray(np.asarray(a, dtype=np.float32))
    shared = {n: f(inputs[n]) for n in (
        "c_ctx", "ada_w", "ada_b", "norm_mix_w", "norm_ffn_w", "lru_in_w", "lru_conv_w", "lru_conv_b",
        "lru_gate_a_w", "lru_gate_a_b", "lru_gate_x_w", "lru_gate_x_b", "lru_lambda", "lru_out_w",
        "attn_qkv_w", "attn_q_norm_w", "attn_k_norm_w", "attn_o_w", "router_w", "router_b",
        "moe_w1", "moe_w3", "moe_w2")}
    shared["ident"] = np.eye(128, dtype=np.float32)
    shared["ltri"] = np.triu(np.ones((128, 128), dtype=np.float32), k=1)
    p_ = np.arange(128, dtype=np.float32)[:, None, None]
    ia = np.zeros((128, 32), dtype=np.float32)
    ia[:, 0:4] = np.arange(4, dtype=np.float32)[None, :] * 128 + np.arange(128, dtype=np.float32)[:, None]
    shared["idxA"] = ia
    shared["idxB"] = np.ascontiguousarray((np.arange(4, dtype=np.float32)[None, :, None] * 512
                                           + np.arange(4, dtype=np.float32)[None, None, :] * 128 + p_).reshape(128, 16))
    inv = (np.float32(10000.0) ** (-np.arange(16, dtype=np.float32) / np.float32(16))).astype(np.float32)
    t = np.arange(T)
    pr = (t // 64).astype(np.float32)[:, None] * inv
    pc = (t % 64).astype(np.float32)[:, None] * inv
    rope = np.concatenate([np.cos(pr), np.sin(pr), np.cos(pc), np.sin(pc)], axis=1).astype(np.float32)
    shared["rope"] = np.ascontiguousarray(rope)
    x = f(inputs["x"]); c = f(inputs["c"]); ctx = f(inputs["ctx"])
    maps = []
    for b in range(8):
        m = dict(shared)
        m["x"] = x[b]; m["c"] = c[b:b + 1]; m["ctx"] = ctx[b]
        if used is not None:
            m = {n: v for n, v in m.items() if n in used}
        maps.append(m)
    return maps


def kernel(**inputs):
    nc = build()
    res = run_bass_kernel_spmd(nc, make_in_maps(inputs, nc._used_inputs), core_ids=list(range(8)))
    return np.stack([r["out"] for r in res.results], axis=0).astype(np.float32)
```
